# Optimizing a Trainium2 kernel written in Bass

```python
import jax
import jax.numpy as jnp
from jax import lax
import numpy as np

D_MODEL = 1024
BATCH = 4
SEQ = 8192
DEPTH = 1

PLE_DIM = 256
D_MIX = D_MODEL
RET_WIDTH = D_MIX // 2
RET_HEADS = 8
RET_HEAD_DIM = RET_WIDTH // RET_HEADS
ML_WIDTH = D_MIX - RET_WIDTH
ML_HEADS = 4
ML_HEAD_DIM = ML_WIDTH // ML_HEADS
N_PROJ = 4 * RET_WIDTH + 4 * ML_WIDTH + 2 * ML_HEADS
CHUNK = 128
CONV_W = 4
ROPE_BASE = 10000.0
N_GROUPS = 4
EXPERTS_PER_GROUP = 8
N_EXPERTS = N_GROUPS * EXPERTS_PER_GROUP
TOP_K = 2
D_EXPERT = D_MODEL // 2
MOE_BLOCK = 128
EPS = 1e-6

kernel_name = 'hymba_style_retention_mlstm_hmoe_ple'


def rms_norm(x, g):
    xf = x.astype(jnp.float32)
    y = xf * lax.rsqrt(jnp.mean(xf * xf, axis=-1, keepdims=True) + EPS)
    return (y * g.astype(jnp.float32)).astype(x.dtype)


def rotary(x, positions):
    half = x.shape[-1] // 2
    freqs = ROPE_BASE ** (-jnp.arange(half, dtype=jnp.float32) / half)
    ang = positions.astype(jnp.float32)[..., None] * freqs
    cos = jnp.cos(ang)[:, :, None, :]
    sin = jnp.sin(ang)[:, :, None, :]
    x1, x2 = x[..., :half], x[..., half:]
    return jnp.concatenate([x1 * cos - x2 * sin, x1 * sin + x2 * cos], axis=-1)


def _to_chunks(t):
    B, S, H, d = t.shape
    return t.reshape(B, S // CHUNK, CHUNK, H, d).transpose(0, 3, 1, 2, 4)


def _from_chunks(t):
    B, H, N, C, d = t.shape
    return t.transpose(0, 2, 3, 1, 4).reshape(B, N * C, H, d)


def retention(q, k, v, positions):
    B, S, H, dh = q.shape
    q = rotary(q, positions)
    k = rotary(k, positions) * (dh ** -0.5)
    qc, kc, vc = _to_chunks(q), _to_chunks(k), _to_chunks(v)
    log_gamma = jnp.log1p(-(2.0 ** (-5.0 - jnp.arange(H, dtype=jnp.float32))))
    idx = jnp.arange(CHUNK, dtype=jnp.float32)
    rel = idx[:, None] - idx[None, :]
    causal = rel >= 0
    decay = jnp.where(causal, jnp.exp(log_gamma[:, None, None] * jnp.where(causal, rel, 0.0)), 0.0)
    scores = jnp.einsum('bhncd,bhnmd->bhncm', qc, kc) * decay[:, None]
    intra = jnp.einsum('bhncm,bhnmd->bhncd', scores, vc)
    w_state = jnp.exp(log_gamma[:, None] * (CHUNK - 1 - idx))
    chunk_kv = jnp.einsum('bhncd,bhnce->bhnde', kc * w_state[:, None, :, None], vc)
    chunk_decay = jnp.exp(log_gamma * CHUNK)[:, None, None]

    def step(R, s):
        return chunk_decay * R + s, R

    R0 = jnp.zeros((B, H, dh, dh), jnp.float32)
    _, R_prev = lax.scan(step, R0, chunk_kv.transpose(2, 0, 1, 3, 4))
    R_prev = R_prev.transpose(1, 2, 0, 3, 4)
    w_query = jnp.exp(log_gamma[:, None] * (idx + 1.0))
    inter = jnp.einsum('bhncd,bhnde->bhnce', qc, R_prev) * w_query[:, None, :, None]
    return _from_chunks(intra + inter)


def mlstm(q, k, v, i_pre, f_pre):
    B, S, H, d = q.shape
    N = S // CHUNK
    k = k * (d ** -0.5)
    qc, kc, vc = _to_chunks(q), _to_chunks(k), _to_chunks(v)
    ig = i_pre.reshape(B, N, CHUNK, H).transpose(0, 3, 1, 2)
    logf = jax.nn.log_sigmoid(f_pre).reshape(B, N, CHUNK, H).transpose(0, 3, 1, 2)
    b = jnp.cumsum(logf, axis=-1)
    bL = b[..., -1]
    a = bL[..., None] - b + ig

    def step(carry, inp):
        S_, n_, m_ = carry
        k_, v_, a_, bL_ = inp
        m_new = jnp.maximum(bL_ + m_, jnp.max(a_, axis=-1))
        w = jnp.exp(a_ - m_new[..., None])
        dec = jnp.exp(bL_ + m_ - m_new)
        S_new = dec[..., None, None] * S_ + jnp.einsum('bhcd,bhce->bhde', k_ * w[..., None], v_)
        n_new = dec[..., None] * n_ + jnp.einsum('bhc,bhcd->bhd', w, k_)
        return (S_new, n_new, m_new), (S_, n_, m_)

    init = (jnp.zeros((B, H, d, d), jnp.float32), jnp.zeros((B, H, d), jnp.float32),
            jnp.zeros((B, H), jnp.float32))
    xs = (kc.transpose(2, 0, 1, 3, 4), vc.transpose(2, 0, 1, 3, 4),
          a.transpose(2, 0, 1, 3), bL.transpose(2, 0, 1))
    _, (S_prev, n_prev, m_prev) = lax.scan(step, init, xs)
    S_prev = S_prev.transpose(1, 2, 0, 3, 4)
    n_prev = n_prev.transpose(1, 2, 0, 3)
    m_prev = m_prev.transpose(1, 2, 0)

    causal = jnp.arange(CHUNK)[:, None] >= jnp.arange(CHUNK)[None, :]
    Dlog = jnp.where(causal, b[..., :, None] - b[..., None, :] + ig[..., None, :], -jnp.inf)
    inter_log = b + m_prev[..., None]
    m_row = jnp.maximum(jnp.max(Dlog, axis=-1), inter_log)
    P = jnp.exp(Dlog - m_row[..., None]) * jnp.einsum('bhncd,bhnmd->bhncm', qc, kc)
    w_inter = jnp.exp(inter_log - m_row)
    num = (jnp.einsum('bhncm,bhnmd->bhncd', P, vc)
           + w_inter[..., None] * jnp.einsum('bhncd,bhnde->bhnce', qc, S_prev))
    den = jnp.sum(P, axis=-1) + w_inter * jnp.einsum('bhncd,bhnd->bhnc', qc, n_prev)
    h = num / jnp.maximum(jnp.abs(den), jnp.exp(-m_row))[..., None]
    return _from_chunks(h)


def causal_conv(x, w, bias):
    C = x.shape[-1]
    y = lax.conv_general_dilated(x, w[:, None, :], window_strides=(1,),
                                 padding=((CONV_W - 1, 0),),
                                 dimension_numbers=('NWC', 'WIO', 'NWC'),
                                 feature_group_count=C)
    return y + bias


def mixer(xn, positions, w_in, conv_w, conv_b, b_igate, b_fgate, ret_gn, ml_gn, w_out):
    B, S, _ = xn.shape
    proj = xn @ w_in
    sizes = [RET_WIDTH] * 4 + [ML_WIDTH] * 4 + [ML_HEADS] * 2
    offs = np.cumsum(sizes)[:-1].tolist()
    rq, rk, rv, rg, mq, mk, mv, mo, mi, mf = jnp.split(proj, offs, axis=-1)
    f32 = jnp.float32
    rsh = (B, S, RET_HEADS, RET_HEAD_DIM)
    r = retention(rq.reshape(rsh).astype(f32), rk.reshape(rsh).astype(f32),
                  rv.reshape(rsh).astype(f32), positions)
    r = rms_norm(r, ret_gn.reshape(RET_HEADS, RET_HEAD_DIM)).reshape(B, S, RET_WIDTH)
    r = jax.nn.silu(rg.astype(f32)) * r
    qk = jax.nn.silu(causal_conv(jnp.concatenate([mq, mk], axis=-1), conv_w, conv_b))
    mq2, mk2 = jnp.split(qk, 2, axis=-1)
    msh = (B, S, ML_HEADS, ML_HEAD_DIM)
    i_pre = (mi + b_igate).astype(f32)
    f_pre = (mf + b_fgate).astype(f32)
    hm = mlstm(mq2.reshape(msh).astype(f32), mk2.reshape(msh).astype(f32),
               mv.reshape(msh).astype(f32), i_pre, f_pre)
    hm = jax.nn.sigmoid(mo.astype(f32)).reshape(msh) * hm
    hm = rms_norm(hm, ml_gn.reshape(ML_HEADS, ML_HEAD_DIM)).reshape(B, S, ML_WIDTH)
    y = jnp.concatenate([r, hm], axis=-1).astype(xn.dtype)
    return y @ w_out


def hier_moe(xn, w_group, b_group, w_router, b_router, w1, w3, w2):
    B, S, D = xn.shape
    T = B * S
    xf = xn.reshape(T, D)
    g_logits = (xf @ w_group).astype(jnp.float32) + b_group.astype(jnp.float32)
    g_prob = jax.nn.softmax(g_logits, axis=-1)
    g_sel = jnp.argmax(g_logits, axis=-1)
    p_g = jnp.take_along_axis(g_prob, g_sel[:, None], axis=-1)
    e_logits = ((xf @ w_router).astype(jnp.float32) + b_router.astype(jnp.float32)
                ).reshape(T, N_GROUPS, EXPERTS_PER_GROUP)
    e_in = jnp.take_along_axis(e_logits, g_sel[:, None, None], axis=1)[:, 0]
    top_p, top_i = lax.top_k(jax.nn.softmax(e_in, axis=-1), TOP_K)
    gates = p_g * top_p / jnp.sum(top_p, axis=-1, keepdims=True)
    expert_id = (g_sel[:, None] * EXPERTS_PER_GROUP + top_i).astype(jnp.int32)
    TK = T * TOP_K
    flat_e = expert_id.reshape(-1)
    order = jnp.argsort(flat_e).astype(jnp.int32)
    sorted_e = flat_e[order]
    counts = jnp.bincount(flat_e, length=N_EXPERTS).astype(jnp.int32)
    padded = (counts + MOE_BLOCK - 1) // MOE_BLOCK * MOE_BLOCK
    start = jnp.cumsum(counts) - counts
    pad_end = jnp.cumsum(padded)
    pad_start = pad_end - padded
    dest = pad_start[sorted_e] + jnp.arange(TK, dtype=jnp.int32) - start[sorted_e]
    n_blocks = TK // MOE_BLOCK + N_EXPERTS
    P = n_blocks * MOE_BLOCK
    row_token = jnp.full((P,), T, jnp.int32).at[dest].set(order // TOP_K)
    x_pad = jnp.concatenate([xf, jnp.zeros((1, D), xf.dtype)], axis=0)
    x_buf = x_pad[row_token].reshape(n_blocks, MOE_BLOCK, D)
    block_start = jnp.arange(n_blocks, dtype=jnp.int32) * MOE_BLOCK
    block_expert = jnp.clip(jnp.searchsorted(pad_end, block_start, side='right'),
                            0, N_EXPERTS - 1).astype(jnp.int32)

    def expert_block(args):
        xb, e = args
        hmid = jax.nn.silu(xb @ w1[e]) * (xb @ w3[e])
        return hmid @ w2[e]

    y_buf = lax.map(expert_block, (x_buf, block_expert)).reshape(P, D)
    slot_pos = jnp.zeros((TK,), jnp.int32).at[order].set(dest)
    y = y_buf[slot_pos].reshape(T, TOP_K, D)
    out = jnp.einsum('tk,tkd->td', gates.astype(y.dtype), y)
    return out.reshape(B, S, D)


def setup_inputs(seed: int = 0) -> dict:
    key = jax.random.key(seed)
    ks = jax.random.split(key, 26)
    f32 = jnp.float32
    nrm = lambda k, shape, scale: jax.random.normal(k, shape, f32) * scale
    L, D = DEPTH, D_MODEL
    x = jax.random.normal(ks[0], (BATCH, SEQ, D), f32)
    p = jax.random.normal(ks[1], (DEPTH, BATCH, SEQ, PLE_DIM), f32)
    positions = jnp.broadcast_to(jnp.arange(SEQ, dtype=jnp.int32)[None, :], (BATCH, SEQ))
    return {
        'x': x,
        'p': p,
        'positions': positions,
        'attn_norm': 1.0 + nrm(ks[2], (L, D), 0.02),
        'w_in': nrm(ks[3], (L, D, N_PROJ), D ** -0.5),
        'conv_w': nrm(ks[4], (L, CONV_W, 2 * ML_WIDTH), CONV_W ** -0.5),
        'conv_b': nrm(ks[5], (L, 2 * ML_WIDTH), 0.02),
        'b_igate': nrm(ks[6], (L, ML_HEADS), 0.1),
        'b_fgate': jnp.linspace(3.0, 6.0, ML_HEADS, dtype=f32)[None, :] + nrm(ks[7], (L, ML_HEADS), 0.02),
        'ret_gn': 1.0 + nrm(ks[8], (L, RET_WIDTH), 0.02),
        'ml_gn': 1.0 + nrm(ks[9], (L, ML_WIDTH), 0.02),
        'w_out': nrm(ks[10], (L, D_MIX, D), D_MIX ** -0.5),
        'moe_norm': 1.0 + nrm(ks[11], (L, D), 0.02),
        'w_group': nrm(ks[12], (L, D, N_GROUPS), D ** -0.5),
        'b_group': nrm(ks[13], (L, N_GROUPS), 0.01),
        'w_router': nrm(ks[14], (L, D, N_EXPERTS), D ** -0.5),
        'b_router': nrm(ks[15], (L, N_EXPERTS), 0.01),
        'w1': nrm(ks[16], (L, N_EXPERTS, D, D_EXPERT), D ** -0.5),
        'w3': nrm(ks[17], (L, N_EXPERTS, D, D_EXPERT), D ** -0.5),
        'w2': nrm(ks[18], (L, N_EXPERTS, D_EXPERT, D), D_EXPERT ** -0.5),
        'w_ple_up': nrm(ks[19], (L, PLE_DIM, D), PLE_DIM ** -0.5),
        'ple_norm': 1.0 + nrm(ks[20], (L, D), 0.02),
        'ple_gate_norm': 1.0 + nrm(ks[21], (L, D), 0.02),
        'w_ple_gate': nrm(ks[22], (L, D, D), D ** -0.5),
        'final_norm': 1.0 + nrm(ks[23], (D,), 0.02),
    }


def reference(x, p, positions, attn_norm, w_in, conv_w, conv_b, b_igate, b_fgate,
              ret_gn, ml_gn, w_out, moe_norm, w_group, b_group, w_router, b_router,
              w1, w3, w2, w_ple_up, ple_norm, ple_gate_norm, w_ple_gate, final_norm):
    h = x
    for l in range(DEPTH):
        xn = rms_norm(h, attn_norm[l])
        h = h + mixer(xn, positions, w_in[l], conv_w[l], conv_b[l], b_igate[l], b_fgate[l],
                      ret_gn[l], ml_gn[l], w_out[l])
        xn = rms_norm(h, moe_norm[l])
        h = h + hier_moe(xn, w_group[l], b_group[l], w_router[l], b_router[l],
                         w1[l], w3[l], w2[l])
        e = rms_norm(p[l] @ w_ple_up[l], ple_norm[l])
        gate = jax.nn.sigmoid(rms_norm(h, ple_gate_norm[l]) @ w_ple_gate[l])
        h = h + gate * e
    return rms_norm(h, final_norm)
```

```python
import math
from contextlib import ExitStack
import numpy as np
import concourse.bass as bass
import concourse.mybir as mybir
from concourse.bass_utils import run_bass_kernel_spmd

F32 = mybir.dt.float32
BF16 = mybir.dt.bfloat16
I32 = mybir.dt.int32
U32 = mybir.dt.uint32
ALU = mybir.AluOpType
AF = mybir.ActivationFunctionType
AX = mybir.AxisListType

P = 128
D = 1024
NPROJ = 4104
NE = 32
DEXP = 512
PLE = 256
EPS = 1e-6
TWO_PI = 2.0 * math.pi

C_ID, C_MASK, C_USTR, C_ONES = 0, 128, 256, 384
C_GQ, C_GK = 512, 520
C_GC = 528
C_FREQ = 536
C_IOTA32 = 568
C_IOTA4 = 600
C_CARRY = 604
CST_W = 608


MODEL_A = {'pa': 30.0, 'pb': 0.3, 'lat': 0.0, 'ga': 100.0, 'gb': 2.0, 'va': 50.0, 'vb': 1.0}
MODEL_BC = {'pa': 64.0, 'pb': 0.45, 'lat': 120.0, 'ga': 300.0, 'gb': 3.0, 'va': 150.0, 'vb': 1.2}
MODEL = dict(MODEL_A)


class Res:
    __slots__ = ("name", "w", "r", "sem", "cnt", "t", "tw", "tr", "fsz")

    def __init__(self, name, t=None):
        self.name, self.t = name, t
        self.w, self.r = {}, {}
        self.sem, self.cnt = None, 0
        self.tw = self.tr = 0.0
        try:
            sh = list(t.shape)
            f = 1
            for d_ in sh[1:]:
                f *= d_
            self.fsz = f
        except Exception:
            self.fsz = 512

    def __getitem__(self, key):
        return self.t[key]


class KB:
    SAME_ENGINE_SYNC = True

    def __init__(self, nc, stack):
        self.nc, self.stack = nc, stack
        self.eng = {"pe": nc.tensor, "act": nc.scalar, "dve": nc.vector, "pool": nc.gpsimd, "sp": nc.sync}
        self.sem, self.cnt, self.waited = {}, {}, {}
        for e in self.eng:
            self.sem[e] = stack.enter_context(nc.semaphore("s_" + e))
            self.cnt[e] = 0
            self.waited[e] = {}
        self.all_res = []
        self.n_inst = self.n_wait = 0
        self.rec = None
        self.tE = {e: 0.0 for e in self.eng}

    def sb(self, st, name, shape, dt):
        r = Res(name, st.enter_context(self.nc.sbuf_tensor(name, list(shape), dt)))
        self.all_res.append(r)
        return r

    def ps(self, st, name, shape, dt):
        r = Res(name, st.enter_context(self.nc.psum_tensor(name, list(shape), dt)))
        self.all_res.append(r)
        return r

    def view(self, name, t):
        r = Res(name, t)
        self.all_res.append(r)
        return r

    def _waits(self, e, reads, writes):
        need = {}
        for r in reads:
            for k, sv in r.w.items():
                if need.get(k, (None, 0))[1] < sv[1]:
                    need[k] = sv
        for w in writes:
            for d in (w.w, w.r):
                for k, sv in d.items():
                    if need.get(k, (None, 0))[1] < sv[1]:
                        need[k] = sv
        wd = self.waited[e]
        for k, (s, v) in need.items():
            if k == e and (e == "pe" or not self.SAME_ENGINE_SYNC):
                continue
            if wd.get(k, 0) >= v:
                continue
            self.eng[e].wait_ge(s, v)
            self.n_wait += 1
            wd[k] = v

    def _dur(self, kind, e, writes, n):
        if kind == "dma":
            return 100.0
        if e == "pe":
            return MODEL['pa'] + MODEL['pb'] * (n if n is not None else 128)
        f = min(writes[0].fsz, 1024) if n is None else n
        if e == "pool":
            return MODEL['ga'] + MODEL['gb'] * f
        return MODEL['va'] + MODEL['vb'] * f

    def _model(self, kind, e, reads, writes, n, commit):
        t = self.tE[e]
        for r in reads:
            if r.tw > t:
                t = r.tw
        for w in writes:
            if w.tw > t:
                t = w.tw
            if w.tr > t:
                t = w.tr
        if commit:
            d = self._dur(kind, e, writes, n)
            self.tE[e] = t + d
            end = t + (2500.0 if kind == "dma" else d + MODEL['lat'])
            for r in reads:
                if end > r.tr:
                    r.tr = end
            for w in writes:
                w.tw = end
        return t

    def lock(self, res):
        if self.rec is not None:
            self.rec.append(("lock", res))

    def unlock(self, res):
        if self.rec is not None:
            self.rec.append(("unlock", res))

    def mark(self):
        if self.rec is not None:
            self.rec.append(("mark",))

    def lock_engine(self, e):
        if self.rec is not None:
            self.rec.append(("elock", e))

    def unlock_engine(self, e):
        if self.rec is not None:
            self.rec.append(("eunlock", e))

    def record(self, gen):
        assert self.rec is None
        self.rec = []
        for _ in gen:
            pass
        out, self.rec = self.rec, None
        return out

    def merge(self, streams):
        streams = [st for st in streams if st]
        idx = [0] * len(streams)
        locks = {}
        elocks = {}
        while True:
            best = None
            for i, st in enumerate(streams):
                if idx[i] >= len(st):
                    continue
                it = st[idx[i]]
                if it[0] == "lock":
                    if locks.get(it[1].name, i) != i:
                        continue
                    t = -2.0
                elif it[0] == "unlock" or it[0] == "eunlock":
                    t = -3.0
                elif it[0] == "elock":
                    if elocks.get(it[1], i) != i:
                        continue
                    t = -2.0
                else:
                    kind, e, fn, reads, writes, n = it
                    blocked = elocks.get(e, i) != i
                    for r in reads + writes:
                        if locks.get(r.name, i) != i:
                            blocked = True
                            break
                    if blocked:
                        continue
                    t = self._model(kind, e, reads, writes, n, False)
                if best is None or t < best[0]:
                    best = (t, i)
            if best is None:
                assert all(idx[i] >= len(st) for i, st in enumerate(streams)), "merge deadlock"
                return
            i = best[1]
            it = streams[i][idx[i]]
            idx[i] += 1
            if it[0] == "lock":
                locks[it[1].name] = i
            elif it[0] == "unlock":
                locks.pop(it[1].name, None)
            elif it[0] == "elock":
                elocks[it[1]] = i
            elif it[0] == "eunlock":
                elocks.pop(it[1], None)
            elif it[0] == "op":
                self.op(it[1], it[2], it[3], it[4], it[5])
            else:
                self.dma(it[1], it[2], it[3], it[4])

    def op(self, e, fn, reads=(), writes=(), n=None):
        if self.rec is not None:
            self.rec.append(("op", e, fn, tuple(reads), tuple(writes), n))
            return None
        self._model("op", e, reads, writes, n, True)
        self._waits(e, reads, writes)
        ins = fn()
        self.cnt[e] += 1
        tok = (self.sem[e], self.cnt[e])
        ins.then_inc(self.sem[e], 1)
        self.n_inst += 1
        for r in reads:
            r.r[e] = tok
        for w in writes:
            w.w[e] = tok
        return ins

    def dma(self, e, fn, reads=(), writes=()):
        if self.rec is not None:
            self.rec.append(("dma", e, fn, tuple(reads), tuple(writes), None))
            return None
        self._model("dma", e, reads, writes, None, True)
        self._waits(e, reads, writes)
        tgt = writes[0]
        if tgt.sem is None:
            tgt.sem = self.stack.enter_context(self.nc.semaphore("d_" + tgt.name))
        ins = fn()
        tgt.cnt += 16
        ins.then_inc(tgt.sem, 16)
        key = "dma:" + tgt.name
        tok = (tgt.sem, tgt.cnt)
        self.n_inst += 1
        for r in reads:
            r.r[key] = tok
        for w in writes:
            w.w[key] = tok
        return ins

    def barrier(self):
        for e in self.eng:
            wd = self.waited[e]
            for e2 in self.eng:
                if self.cnt[e2] == 0 or (e2 == e and (e == "pe" or not self.SAME_ENGINE_SYNC)):
                    continue
                if wd.get(e2, 0) < self.cnt[e2]:
                    self.eng[e].wait_ge(self.sem[e2], self.cnt[e2])
                    wd[e2] = self.cnt[e2]
            for r in self.all_res:
                if r.sem is not None and r.cnt > 0:
                    k = "dma:" + r.name
                    if wd.get(k, 0) < r.cnt:
                        self.eng[e].wait_ge(r.sem, r.cnt)
                        wd[k] = r.cnt


def build_program(NT, PT, CAP=512, stop=9, scatter=True, sub=99):
    nc = bass.Bass("TRN2", target_bir_lowering=False)
    NB = CAP // P
    NROWS = NE * CAP
    dscale = 128.0 ** -0.5

    def din(name, shape, dt=F32):
        return Res(name, nc.dram_tensor(name, list(shape), dt, kind="ExternalInput"))

    x_main = din("x_main", [NT * P, D])
    x_pre = din("x_pre", [max(PT, 1) * P, D])
    pos_main = din("pos_main", [P, NT], I32)
    pos_pre = din("pos_pre", [P, max(PT, 1)], I32)
    p_main = din("p_main", [NT * P, PLE])
    w_in = din("w_in", [D, NPROJ])
    w_out = din("w_out", [D, D])
    w1 = din("w1", [NE, D, DEXP])
    w3 = din("w3", [NE, D, DEXP])
    w2 = din("w2", [NE, DEXP, D])
    w_up = din("w_up", [PLE, D])
    w_gate = din("w_gate", [D, D])
    wr = din("wr", [D, 36])
    gk = din("gk", [P, 24])
    bc_moe = din("bc_moe", [P, D])
    bc_ple = din("bc_ple", [P, D])
    bc_fin = din("bc_fin", [P, D])
    bc_small = din("bc_small", [P, 44])
    convw = din("convw", [P, 40])
    cst_d = din("cst", [P, CST_W])
    zsrc = din("zsrc", [1024, 512])
    convb_row = din("convb_row", [1, 1024])
    out_d = Res("out", nc.dram_tensor("out", [NT * P, D], F32, kind="ExternalOutput"))
    h1s = Res("h1s", nc.dram_tensor("h1s", [NT * P, D], F32, kind="Internal"))
    xbuf = Res("xbuf", nc.dram_tensor("xbuf", [NROWS, D], BF16, kind="Internal"))
    ybuf = Res("ybuf", nc.dram_tensor("ybuf", [NROWS, D], BF16, kind="Internal"))

    with ExitStack() as gst:
        kb = KB(nc, gst)
        kb.all_res += [out_d, h1s, xbuf, ybuf]
        V = lambda fn, r=(), w=(), n=None: kb.op("dve", fn, r, w, n)
        A = lambda fn, r=(), w=(), n=None: kb.op("act", fn, r, w, n)
        G = lambda fn, r=(), w=(), n=None: kb.op("pool", fn, r, w, n)
        T = lambda fn, r=(), w=(), n=None: kb.op("pe", fn, r, w, n)
        SP = lambda fn, r=(), w=(): kb.dma("sp", fn, r, w)
        GD = lambda fn, r=(), w=(): kb.dma("pool", fn, r, w)

        bc_reg = nc.gpsimd.to_reg(NROWS - 1)
        cst = kb.sb(gst, "cstt", [P, CST_W], F32)
        identb = kb.sb(gst, "identb", [P, P], BF16)
        maskb = kb.sb(gst, "maskb", [P, P], BF16)
        ustrb = kb.sb(gst, "ustrb", [P, P], BF16)
        onesb = kb.sb(gst, "onesb", [P, P], BF16)
        dest_i = kb.sb(gst, "dest_i", [P, NT, 2], I32)
        gates = kb.sb(gst, "gates", [P, NT, 2], F32)
        bcs = kb.sb(gst, "bcs", [P, 44], F32)
        bk = [kb.ps(gst, "bk%d" % i, [P, 512], F32) for i in range(4)]
        bkT = kb.ps(gst, "bkT", [P, 1024], BF16)
        bk += [kb.ps(gst, "bk%d" % i, [P, 512], F32) for i in (5, 6)]
        bk7t = gst.enter_context(nc.psum_tensor("bk7", [P, 512], F32))
        ps_g = kb.view("bk7", bk7t)
        ps_cs = ps_lg = ps_pos = ps_tot = ps_g
        b0, b1, b2, b3, b5, b6 = bk

        SP(lambda: nc.sync.dma_start(out=cst[:, :], in_=cst_d[:, :]), [cst_d], [cst])
        xbuf_z = kb.view("xbuf_z", xbuf.t)
        zpending = list(range(NROWS // 1024))

        def zfill():
            if zpending:
                zi = zpending.pop(0)
                SP(lambda: nc.sync.dma_start(out=xbuf.t[zi * 1024:(zi + 1) * 1024, :], in_=zsrc.t[:, :].bitcast(BF16)), [zsrc], [xbuf_z])
        SP(lambda: nc.sync.dma_start(out=bcs[:, :], in_=bc_small[:, :]), [bc_small], [bcs])
        G(lambda: nc.gpsimd.tensor_copy(out=identb[:, :], in_=cst[:, C_ID:C_ID + P]), [cst], [identb])
        G(lambda: nc.gpsimd.tensor_copy(out=ustrb[:, :], in_=cst[:, C_USTR:C_USTR + P]), [cst], [ustrb])
        G(lambda: nc.gpsimd.tensor_copy(out=onesb[:, :], in_=cst[:, C_ONES:C_ONES + P]), [cst], [onesb])
        G(lambda: nc.gpsimd.memset(dest_i[:, :, :], 0), [], [dest_i])
        G(lambda: nc.gpsimd.memset(gates[:, :, :], 0.0), [], [gates])
        ident_f = cst
        mask_ap = lambda: cst[:, C_MASK:C_MASK + P]

        def rsqrt_chain(dst_ap, src_ap, n, res_list_r, res_list_w, tmp):
            A(lambda: nc.scalar.activation(out=tmp_ap(tmp, src_ap), in_=src_ap, func=AF.Ln, bias=EPS, scale=1.0 / n),
              res_list_r, [tmp])
            A(lambda: nc.scalar.activation(out=dst_ap, in_=tmp_ap(tmp, src_ap), func=AF.Exp, scale=-0.5),
              [tmp], res_list_w)

        def tmp_ap(tmp, like):
            w = like.shape[-1] if len(like.shape) == 2 else None
            return tmp[:, 0:w]

        def load_weight(st, dst, src_ap_fn, nk, ncols, gscale, qname, blk=512):
            stg = [kb.sb(st, qname + "_stg%d" % i, [P, nk, blk], F32) for i in range(2)]
            i = 0
            for c0 in range(0, ncols, blk):
                cw = min(blk, ncols - c0)
                s_ = stg[i % 2]
                SP(lambda s_=s_, c0=c0, cw=cw: nc.sync.dma_start(out=s_[:, :, 0:cw], in_=src_ap_fn(c0, cw)), [], [s_])
                for k in range(nk):
                    eng = ("dve", "act")[(i * nk + k) % 2]
                    o_ = dst[:, k, c0:c0 + cw]
                    i_ = s_[:, k, 0:cw]
                    if gscale is None:
                        if eng == "act":
                            A(lambda o_=o_, i_=i_: nc.scalar.copy(out=o_, in_=i_), [s_], [dst])
                        elif eng == "dve":
                            V(lambda o_=o_, i_=i_: nc.vector.tensor_copy(out=o_, in_=i_), [s_], [dst])
                        else:
                            G(lambda o_=o_, i_=i_: nc.gpsimd.tensor_copy(out=o_, in_=i_), [s_], [dst])
                    else:
                        gs, gres = gscale
                        sc = gs(k)
                        if eng == "act":
                            A(lambda o_=o_, i_=i_, sc=sc: nc.scalar.activation(out=o_, in_=i_, func=AF.Copy, scale=sc), [s_, gres], [dst])
                        elif eng == "dve":
                            V(lambda o_=o_, i_=i_, sc=sc: nc.vector.tensor_scalar(out=o_, in0=i_, scalar1=sc, scalar2=None, op0=ALU.mult), [s_, gres], [dst])
                        else:
                            G(lambda o_=o_, i_=i_, sc=sc: nc.gpsimd.tensor_scalar(out=o_, in0=i_, scalar1=sc, scalar2=None, op0=ALU.mult), [s_, gres], [dst])
                i += 1

        MODEL.update(MODEL_A)
        with ExitStack() as ast:
            Win = kb.sb(ast, "Win", [P, 8, NPROJ], BF16)
            Wout = kb.sb(ast, "Wout", [P, 8, D], BF16)
            Wr = kb.sb(ast, "Wr", [P, 8, 36], BF16)
            gkt = kb.sb(ast, "gkt", [P, 24], F32)
            cvw = kb.sb(ast, "cvw", [P, 40], F32)
            bmoe = kb.sb(ast, "bmoe", [P, D], F32)
            SP(lambda: nc.sync.dma_start(out=gkt[:, :], in_=gk[:, :]), [gk], [gkt])
            SP(lambda: nc.sync.dma_start(out=cvw[:, :], in_=convw[:, :]), [convw], [cvw])
            SP(lambda: nc.sync.dma_start(out=bmoe[:, :], in_=bc_moe[:, :]), [bc_moe], [bmoe])
            with ExitStack() as lst:
                load_weight(lst, Win, lambda c0, cw: w_in.t[:, c0:c0 + cw].rearrange("(k p) n -> p k n", p=P), 8, NPROJ,
                            (lambda k: gkt[:, k:k + 1], gkt), "win")
                load_weight(lst, Wout, lambda c0, cw: w_out.t[:, c0:c0 + cw].rearrange("(k p) n -> p k n", p=P), 8, D,
                            (lambda k: gkt[:, 8 + k:9 + k], gkt), "wout")
                wrs = kb.sb(lst, "wrs", [P, 8, 36], F32)
                SP(lambda: nc.sync.dma_start(out=wrs[:, :, :], in_=wr.t[:, :].rearrange("(k p) n -> p k n", p=P)), [wr], [wrs])
                V(lambda: nc.vector.tensor_copy(out=Wr[:, :, :], in_=wrs[:, :, :]), [wrs], [Wr])
                kb.barrier()
                if stop <= 1:
                    return nc

            xs = [kb.sb(ast, "xs%d" % i, [P, D], F32) for i in range(3)]
            junk = kb.sb(ast, "junk", [P, D], BF16)
            sm = [kb.sb(ast, "sm_%d" % i, [P, 64], F32) for i in range(2)]
            sm2 = kb.sb(ast, "sm2", [P, 64], F32)
            xb = kb.sb(ast, "xb", [P, D], BF16)
            xT = [kb.sb(ast, "xT%d" % i, [P, 8, P], BF16) for i in range(2)]
            qs = [kb.sb(ast, "qs%d" % i, [P, 512], F32) for i in range(2)]
            ks = [kb.sb(ast, "ks%d" % i, [P, 512], F32) for i in range(2)]
            rt = [kb.sb(ast, "rt%d" % i, [P, 8, 32], F32) for i in range(4)]
            qr = kb.sb(ast, "qr", [P, 512], BF16)
            kr = [kb.sb(ast, "kr%d" % i, [P, 512], BF16) for i in range(2)]
            qkTr = [kb.sb(ast, "qkTr%d" % i, [P, 16, P], BF16) for i in range(2)]
            vr = [kb.sb(ast, "vr%d" % i, [P, 512], BF16) for i in range(2)]
            gr = [kb.sb(ast, "gr%d" % i, [P, 512], F32) for i in range(2)]
            STt = kb.sb(ast, "STt", [P, 8, P], BF16)
            sqr = kb.sb(ast, "sqr", [P, 512], F32)
            rn = kb.sb(ast, "rn", [P, 512], F32)
            y = kb.sb(ast, "y", [P, D], BF16)
            yT = kb.sb(ast, "yT", [P, 8, P], BF16)
            cv = [kb.sb(ast, "cv%d" % i, [P, 8, 131], BF16) for i in range(2)]
            Dg = kb.sb(ast, "Dg", [P, 32, P], BF16)
            brow = kb.sb(ast, "brow", [1, 1024], BF16)
            onesr = kb.sb(ast, "onesr", [1, P], BF16)
            qka = [kb.sb(ast, "qka%d" % i, [P, 8, P], BF16) for i in range(2)]
            vm1 = [kb.sb(ast, "vm1_%d" % i, [P, 4, 129], BF16) for i in range(2)]
            go = [kb.sb(ast, "go%d" % i, [P, 512], F32) for i in range(2)]
            PTt = kb.sb(ast, "PTt", [P, 4, P], BF16)
            khat = [kb.sb(ast, "khat%d" % i, [P, 4, P], BF16) for i in range(2)]
            hg = kb.sb(ast, "hg", [P, 512], F32)
            Tst = kb.sb(ast, "Tst", [P, 4, 129], F32)
            Sbf = kb.sb(ast, "Sbf", [P, 4, 129], BF16)
            dec = [kb.sb(ast, "dec%d" % i, [P, 4], F32) for i in range(3)]
            Rst = kb.sb(ast, "Rst", [P, 8, 64], F32)
            Rbf = kb.sb(ast, "Rbf", [P, 8, 64], BF16)
            h1t2 = [kb.sb(ast, "h1t%d" % i, [P, D], F32) for i in range(2)]
            h1t = h1t2[0]
            xn2 = kb.sb(ast, "xn2", [P, D], BF16)
            xn2T = kb.sb(ast, "xn2T", [P, 8, P], BF16)
            cosT = kb.sb(ast, "cosT", [P, max(NT, PT), 32], F32)
            sinT = kb.sb(ast, "sinT", [P, max(NT, PT), 32], F32)
            posi = kb.sb(ast, "posi", [P, max(NT, PT)], I32)
            posf = kb.sb(ast, "posf", [P, max(NT, PT)], F32)
            rl = kb.sb(ast, "rl", [P, 64], F32)
            rl2 = kb.sb(ast, "rl2", [P, 64], F32)
            m8 = kb.sb(ast, "m8", [P, 8], F32)
            i8 = kb.sb(ast, "i8", [P, 8], U32)
            oh = kb.sb(ast, "oh", [P, 3, 32], F32)
            ohb = kb.sb(ast, "ohb", [P, 32], BF16)
            CNT = kb.sb(ast, "CNT", [P, 32], F32)
            pos_s = kb.sb(ast, "pos_s", [P, 32], F32)

            G(lambda: nc.gpsimd.memset(Tst[:, :, :], 0.0), [], [Tst])
            G(lambda: nc.gpsimd.memset(Sbf[:, :, :], 0.0), [], [Sbf])
            G(lambda: nc.gpsimd.memset(Rst[:, :, :], 0.0), [], [Rst])
            G(lambda: nc.gpsimd.memset(Rbf[:, :, :], 0.0), [], [Rbf])
            G(lambda: nc.gpsimd.memset(CNT[:, :], 0.0), [], [CNT])
            for i in range(2):
                G(lambda i=i: nc.gpsimd.memset(vm1[i][:, :, :], 1.0), [], [vm1[i]])
            for i in range(3):
                G(lambda i=i: nc.gpsimd.memset(dec[i][:, :], 1.0), [], [dec[i]])
            for i in range(2):
                G(lambda i=i: nc.gpsimd.memset(cv[i][:, :, :], 0.0), [], [cv[i]])

            for c in range(8):
                for jj in range(4):
                    V(lambda c=c, jj=jj: nc.vector.tensor_scalar(out=Dg[:, c * 4 + jj, :], in0=cst[:, C_ID:C_ID + P], scalar1=cvw[:, c * 4 + jj:c * 4 + jj + 1],
                                                                 scalar2=None, op0=ALU.mult), [cst, cvw], [Dg], 128)
            SP(lambda: nc.sync.dma_start(out=h1t2[1][0:1, :], in_=convb_row.t[:, :]), [convb_row], [h1t2[1]])
            V(lambda: nc.vector.tensor_copy(out=brow[:, :], in_=h1t2[1][0:1, :]), [h1t2[1]], [brow])
            V(lambda: nc.vector.memset(onesr[:, :], 1.0), [], [onesr])

            def make_trig(pos_res, n, ibuf=None):
                SP(lambda: nc.sync.dma_start(out=posi[:, 0:n], in_=pos_res.t[:, 0:n]), [pos_res], [posi])
                V(lambda: nc.vector.tensor_copy(out=posf[:, 0:n], in_=posi[:, 0:n]), [posi], [posf])
                for n0 in range(0, n, 16):
                    m_ = min(16, n - n0)
                    rA, rB, rI = h1t2[0], h1t2[1], sqr
                    aA = rA[:, 0:m_ * 32].rearrange("p (n j) -> p n j", j=32)
                    aB = rB[:, 0:m_ * 32].rearrange("p (n j) -> p n j", j=32)
                    aI = rI[:, 0:m_ * 32].bitcast(I32).rearrange("p (n j) -> p n j", j=32)
                    V(lambda: nc.vector.tensor_tensor(out=aA, in0=posf[:, n0:n0 + m_].unsqueeze(2).broadcast_to([P, m_, 32]),
                                                      in1=cst[:, C_FREQ:C_FREQ + 32].unsqueeze(1).broadcast_to([P, m_, 32]), op=ALU.mult),
                      [posf, cst], [rA])
                    for (shift, dstT) in ((0.0, sinT), (math.pi / 2, cosT)):
                        V(lambda shift=shift: nc.vector.tensor_scalar(out=aB, in0=aA, scalar1=shift, scalar2=1.0 / TWO_PI,
                                                                      op0=ALU.add, op1=ALU.mult), [rA], [rB])
                        V(lambda: nc.vector.tensor_copy(out=aI, in_=aB), [rB], [rI])
                        V(lambda: nc.vector.tensor_copy(out=aB, in_=aI), [rI], [rB])
                        V(lambda: nc.vector.scalar_tensor_tensor(out=aB, in0=aB, scalar=-TWO_PI, in1=aA,
                                                                 op0=ALU.mult, op1=ALU.add), [rB, rA], [rB])
                        V(lambda shift=shift: nc.vector.tensor_scalar(out=aB, in0=aB, scalar1=shift, scalar2=math.pi,
                                                                      op0=ALU.add, op1=ALU.min), [rB], [rB])
                        V(lambda: nc.vector.tensor_scalar(out=aB, in0=aB, scalar1=-math.pi, scalar2=None,
                                                          op0=ALU.max), [rB], [rB])
                        A(lambda dstT=dstT: nc.scalar.activation(out=dstT[:, n0:n0 + m_, :], in_=aB, func=AF.Sin), [rB], [dstT])

            def rotary(src, dst, n):
                sv = src[:, :].rearrange("p (h t j) -> p h t j", h=8, t=2)
                dv = dst[:, :].rearrange("p (h t j) -> p h t j", h=8, t=2)
                cb = cosT[:, n, :].unsqueeze(1).broadcast_to([P, 8, 32])
                sb_ = sinT[:, n, :].unsqueeze(1).broadcast_to([P, 8, 32])
                q1, q2 = sv[:, :, 0, :], sv[:, :, 1, :]
                G(lambda: nc.gpsimd.tensor_tensor(out=rt[0][:, :, :], in0=q1, in1=cb, op=ALU.mult), [src, cosT], [rt[0]])
                G(lambda: nc.gpsimd.tensor_tensor(out=rt[1][:, :, :], in0=q2, in1=sb_, op=ALU.mult), [src, sinT], [rt[1]])
                G(lambda: nc.gpsimd.tensor_tensor(out=rt[2][:, :, :], in0=q1, in1=sb_, op=ALU.mult), [src, sinT], [rt[2]])
                G(lambda: nc.gpsimd.tensor_tensor(out=rt[3][:, :, :], in0=q2, in1=cb, op=ALU.mult), [src, cosT], [rt[3]])
                G(lambda: nc.gpsimd.tensor_tensor(out=dv[:, :, 0, :], in0=rt[0][:, :, :], in1=rt[1][:, :, :], op=ALU.subtract), [rt[0], rt[1]], [dst])
                G(lambda: nc.gpsimd.tensor_tensor(out=dv[:, :, 1, :], in0=rt[2][:, :, :], in1=rt[3][:, :, :], op=ALU.add), [rt[2], rt[3]], [dst])

            def inproj(bank, c0, ncols, base=0):
                for k in range(8):
                    T(lambda k=k: nc.tensor.matmul(bank[:, base:base + ncols], lhsT=xT[:, k, :], rhs=Win[:, k, c0:c0 + ncols],
                                                   start=(k == 0), stop=(k == 7)), [xT, Win], [bank])

            def transpose8(src, dstT, nk=8):
                kb.lock(bkT)
                for k in range(nk):
                    T(lambda k=k: nc.tensor.transpose(out=bkT[:, k * P:(k + 1) * P], in_=src[:, k * P:(k + 1) * P], identity=identb[:, :]),
                      [src, identb], [bkT])
                A(lambda: nc.scalar.copy(out=dstT[:, 0:nk, :], in_=bkT[:, 0:nk * P].rearrange("p (k t) -> p k t", k=nk)), [bkT], [dstT])
                kb.unlock(bkT)

            def load_x(gi_):
                xsrc_, n_, _ = seq[gi_]
                xt_ = xs[gi_ % 3]
                SP(lambda: nc.sync.dma_start(out=xt_[:, :], in_=xsrc_.t[n_ * P:(n_ + 1) * P, :]), [xsrc_], [xt_])

            def proj(bank, c0, xTt):
                for k in range(8):
                    T(lambda k=k: nc.tensor.matmul(bank[:, :], lhsT=xTt[:, k, :], rhs=Win[:, k, c0:c0 + 512],
                                                   start=(k == 0), stop=(k == 7)), [xTt, Win], [bank], 512)

            def projT(bank, c0, xTt):
                for cc in range(4):
                    for k in range(8):
                        T(lambda k=k, cc=cc: nc.tensor.matmul(bank[:, cc * P:(cc + 1) * P], lhsT=Win[:, k, c0 + cc * P:c0 + (cc + 1) * P], rhs=xTt[:, k, :],
                                                              start=(k == 0), stop=(k == 7)), [xTt, Win], [bank])

            def pre(gi):
                xt = xs[gi % 3]
                smj = sm[gi % 2]
                A(lambda: nc.scalar.activation(out=junk[:, :], in_=xt[:, :], func=AF.Square, accum_out=smj[:, 0:1]), [xt], [junk, smj])
                A(lambda: nc.scalar.activation(out=smj[:, 1:2], in_=smj[:, 0:1], func=AF.Ln, bias=EPS, scale=1.0 / D), [smj], [smj], 1)
                A(lambda: nc.scalar.activation(out=smj[:, 2:3], in_=smj[:, 1:2], func=AF.Exp, scale=-0.5), [smj], [smj], 1)
                A(lambda: nc.scalar.activation(out=xb[:, :], in_=xt[:, :], func=AF.Copy, scale=smj[:, 2:3]), [xt, smj], [xb])
                transpose8(xb, xT[gi % 2])
                yield

            def front(gi):
                xsrc, n, main = seq[gi]
                j = gi % 2
                xt = xs[gi % 3]
                smj, qsj, ksj, vrj, grj, qkaj, krj, qkTj, vm1j, goj, khj = sm[j], qs[j], ks[j], vr[j], gr[j], qka[j], kr[j], qkTr[j], vm1[j], go[j], khat[j]
                qhalo = main or (gi + 1 < len(seq) and seq[gi + 1][2])
                xTt = xT[j]
                cvc, cvn = cv[j], cv[1 - j]
                c_lo = 0 if main else 4
                h_lo = 0 if qhalo else 4

                def conv_mm(bank, c0):
                    for cc in range(4):
                        c = c0 + cc
                        for jj in range(4):
                            T(lambda c=c, cc=cc, jj=jj: nc.tensor.matmul(bank[:, cc * P:(cc + 1) * P], lhsT=Dg[:, c * 4 + jj, :], rhs=cvc[:, c, jj:jj + 128],
                                                                         start=(jj == 0), stop=False), [Dg, cvc], [bank])
                        T(lambda c=c, cc=cc: nc.tensor.matmul(bank[:, cc * P:(cc + 1) * P], lhsT=brow[0:1, c * P:(c + 1) * P], rhs=onesr[0:1, :],
                                                              start=False, stop=True), [brow, onesr], [bank])

                projT(b1, 2560, xTt)
                A(lambda: nc.scalar.copy(out=cvc[:, 4:8, 3:131], in_=b1[:, :].rearrange("p (c t) -> p c t", c=4)), [b1], [cvc], 512)
                if qhalo:
                    projT(b0, 2048, xTt)
                    A(lambda: nc.scalar.copy(out=cvc[:, 0:4, 3:131], in_=b0[:, :].rearrange("p (c t) -> p c t", c=4)), [b0], [cvc], 512)
                G(lambda: nc.gpsimd.tensor_copy(out=cvn[:, h_lo:8, 0:3], in_=cvc[:, h_lo:8, 128:131]), [cvc], [cvn], 24)
                conv_mm(b1, 4)
                if main:
                    conv_mm(b0, 0)
                kb.lock_engine("act")
                if main:
                    A(lambda: nc.scalar.activation(out=qkaj[:, 0:4, :], in_=b0[:, :].rearrange("p (c t) -> p c t", c=4), func=AF.Silu), [b0], [qkaj], 512)
                    proj(b0, 1536, xTt)
                A(lambda: nc.scalar.activation(out=qkaj[:, 4:8, :], in_=b1[:, :].rearrange("p (c t) -> p c t", c=4), func=AF.Silu), [b1], [qkaj], 512)
                if main:
                    A(lambda: nc.scalar.activation(out=grj[:, :], in_=b0[:, :], func=AF.Silu), [b0], [grj], 512)
                kb.unlock_engine("act")
                yield
                for k in range(8):
                    T(lambda k=k: nc.tensor.matmul(ps_g[:, 0:8], lhsT=xTt[:, k, :], rhs=Win[:, k, 4096:4104], start=(k == 0), stop=(k == 7)), [xTt, Win], [ps_g], 8)
                V(lambda: nc.vector.tensor_tensor(out=smj[:, 8:16], in0=ps_g[:, 0:8], in1=bcs[:, 36:44], op=ALU.add), [ps_g, bcs], [smj], 8)
                A(lambda: nc.scalar.activation(out=smj[:, 16:20], in_=smj[:, 12:16], func=AF.Exp, scale=-1.0), [smj], [smj], 4)
                A(lambda: nc.scalar.activation(out=smj[:, 20:24], in_=smj[:, 16:20], func=AF.Ln, bias=1.0, scale=1.0), [smj], [smj], 4)
                T(lambda: nc.tensor.matmul(ps_cs[:, 8:12], lhsT=cst[:, C_MASK:C_MASK + P], rhs=smj[:, 20:24], start=True, stop=True), [cst, smj], [ps_cs], 16)
                T(lambda: nc.tensor.matmul(ps_cs[:, 12:16], lhsT=cst[:, C_ONES:C_ONES + P], rhs=smj[:, 20:24], start=True, stop=True), [cst, smj], [ps_cs], 16)
                V(lambda: nc.vector.tensor_tensor(out=smj[:, 24:28], in0=smj[:, 8:12], in1=ps_cs[:, 8:12], op=ALU.add), [smj, ps_cs], [smj], 4)
                A(lambda: nc.scalar.activation(out=smj[:, 28:32], in_=smj[:, 24:28], func=AF.Exp, bias=math.log(dscale), scale=1.0), [smj], [smj], 4)
                if main:
                    A(lambda: nc.scalar.activation(out=smj[:, 32:36], in_=ps_cs[:, 8:12], func=AF.Exp, scale=-1.0), [ps_cs], [smj], 4)
                dcur = dec[gi % 3]
                A(lambda: nc.scalar.activation(out=dcur[:, :], in_=ps_cs[:, 12:16], func=AF.Exp, scale=-1.0), [ps_cs], [dcur], 4)
                yield
                proj(b1, 512, xTt)
                V(lambda: nc.vector.tensor_tensor(out=ksj[:, :].rearrange("p (h e) -> p h e", h=8), in0=b1[:, :].rearrange("p (h e) -> p h e", h=8), in1=cst[:, C_GK:C_GK + 8].unsqueeze(2).broadcast_to([P, 8, 64]), op=ALU.mult), [b1, cst], [ksj], 512)
                rotary(ksj, krj, n)
                if main:
                    proj(b0, 0, xTt)
                    V(lambda: nc.vector.tensor_tensor(out=qsj[:, :].rearrange("p (h e) -> p h e", h=8), in0=b0[:, :].rearrange("p (h e) -> p h e", h=8), in1=cst[:, C_GQ:C_GQ + 8].unsqueeze(2).broadcast_to([P, 8, 64]), op=ALU.mult), [b0, cst], [qsj], 512)
                    rotary(qsj, qr, n)
                yield
                proj(b1, 1024, xTt)
                A(lambda: nc.scalar.copy(out=vrj[:, :], in_=b1[:, :]), [b1], [vrj], 512)
                proj(b0, 3072, xTt)
                A(lambda: nc.scalar.copy(out=vm1j[:, :, 0:128], in_=b0[:, :].rearrange("p (h e) -> p h e", h=4)), [b0], [vm1j], 512)
                if main:
                    proj(b1, 3584, xTt)
                    A(lambda: nc.scalar.activation(out=goj[:, :], in_=b1[:, :], func=AF.Exp, scale=-1.0), [b1], [goj], 512)
                    V(lambda: nc.vector.tensor_scalar(out=goj[:, :], in0=goj[:, :], scalar1=1.0, scalar2=None, op0=ALU.add), [goj], [goj], 512)
                    V(lambda: nc.vector.reciprocal(out=goj[:, :], in_=goj[:, :]), [goj], [goj], 512)
                yield
                if main:
                    b0h = b0[:, :].bitcast(BF16)
                    b1h = b1[:, :].bitcast(BF16)
                    for h in range(8):
                        T(lambda h=h: nc.tensor.transpose(out=b0h[0:64, h * P:(h + 1) * P], in_=qr[:, h * 64:(h + 1) * 64], identity=identb[:, :]), [qr, identb], [b0], 64)
                    for h in range(8):
                        T(lambda h=h: nc.tensor.transpose(out=b1h[0:64, h * P:(h + 1) * P], in_=krj[:, h * 64:(h + 1) * 64], identity=identb[:, :]), [krj, identb], [b1], 64)
                kb.lock(bkT)
                for h in range(4):
                    T(lambda h=h: nc.tensor.transpose(out=bkT[:, h * P:(h + 1) * P], in_=qkaj[:, 4 + h, :], identity=identb[:, :]), [qkaj, identb], [bkT])
                for h in range(4):
                    A(lambda h=h: nc.scalar.activation(out=khj[:, h, :], in_=bkT[:, h * P:(h + 1) * P], func=AF.Copy, scale=smj[:, 28 + h:29 + h]), [bkT, smj], [khj], 128)
                kb.unlock(bkT)
                if main:
                    A(lambda: nc.scalar.copy(out=qkTj[0:64, 0:8, :], in_=b0h[0:64, :].rearrange("p (k t) -> p k t", k=8)), [b0], [qkTj])
                    V(lambda: nc.vector.tensor_copy(out=qkTj[0:64, 8:16, :], in_=b1h[0:64, :].rearrange("p (k t) -> p k t", k=8)), [b1], [qkTj])

            def back(gi):
                xsrc, n, main = seq[gi]
                j = gi % 2
                xt = xs[gi % 3]
                h1t = h1t2[gi % 2]
                smj, vrj, grj, qkaj, krj, qkTj, vm1j, goj, khj = sm[j], vr[j], gr[j], qka[j], kr[j], qkTr[j], vm1[j], go[j], khat[j]
                dcur, dprev = dec[gi % 3], dec[(gi - 1) % 3]
                eu = lambda h: smj[:, 28 + h:29 + h]
                if main:
                    for h in range(8):
                        bank = b2 if h < 4 else b3
                        hh = h % 4
                        T(lambda h=h, bank=bank, hh=hh: nc.tensor.matmul(bank[:, hh * P:(hh + 1) * P], lhsT=qkTj[0:64, 8 + h, :],
                                                                         rhs=qkTj[0:64, h, :], start=True, stop=True), [qkTj], [bank])
                    for h in range(4):
                        T(lambda h=h: nc.tensor.matmul(b5[:, h * P:(h + 1) * P], lhsT=qkaj[:, 4 + h, :], rhs=qkaj[:, h, :], start=True, stop=True), [qkaj], [b5])
                    mb4 = cst[:, C_MASK:C_MASK + P].unsqueeze(1).broadcast_to([P, 4, P])
                    V(lambda: nc.vector.tensor_tensor(out=STt[:, 0:4, :], in0=b2[:, :].rearrange("p (h t) -> p h t", h=4), in1=mb4, op=ALU.mult), [b2, cst], [STt])
                    V(lambda: nc.vector.tensor_tensor(out=STt[:, 4:8, :], in0=b3[:, :].rearrange("p (h t) -> p h t", h=4), in1=mb4, op=ALU.mult), [b3, cst], [STt])
                    for h in range(4):
                        V(lambda h=h: nc.vector.scalar_tensor_tensor(out=PTt[:, h, :], in0=b5[:, h * P:(h + 1) * P], scalar=eu(h), in1=cst[:, C_MASK:C_MASK + P],
                                                                     op0=ALU.mult, op1=ALU.mult), [b5, smj, cst], [PTt], 128)
                yield
                for h in range(8):
                    T(lambda h=h: nc.tensor.matmul(b6[0:64, h * 64:(h + 1) * 64], lhsT=krj[:, h * 64:(h + 1) * 64],
                                                   rhs=vrj[:, h * 64:(h + 1) * 64], start=True, stop=True), [krj, vrj], [b6], 64)
                dsb = lambda h: (b2 if h < 2 else b3)
                for h in range(4):
                    o0 = (h % 2) * 129
                    T(lambda h=h, o0=o0: nc.tensor.matmul(dsb(h)[:, o0:o0 + 129], lhsT=khj[:, h, :], rhs=vm1j[:, h, :], start=True, stop=True), [khj, vm1j], [dsb(h)])
                V(lambda: nc.vector.tensor_tensor(out=Rst[0:64, :, :], in0=b6[0:64, :].rearrange("p (h e) -> p h e", h=8), in1=Rst[0:64, :, :], op=ALU.add), [b6, Rst], [Rst], 512)
                V(lambda: nc.vector.tensor_tensor(out=Rst[0:64, :, :], in0=Rst[0:64, :, :], in1=cst[0:64, C_GC:C_GC + 8].unsqueeze(2).broadcast_to([64, 8, 64]), op=ALU.mult), [Rst, cst], [Rst], 512)
                for h in range(4):
                    o0 = (h % 2) * 129
                    V(lambda h=h, o0=o0: nc.vector.scalar_tensor_tensor(out=Tst[:, h, :], in0=Tst[:, h, :], scalar=dprev[:, h:h + 1], in1=dsb(h)[:, o0:o0 + 129],
                                                                        op0=ALU.mult, op1=ALU.add), [Tst, dprev, dsb(h)], [Tst], 129)
                yield
                ob = lambda h: (b6 if h < 2 else b2)
                if main:
                    for h in range(8):
                        T(lambda h=h: nc.tensor.matmul(b5[:, h * 64:(h + 1) * 64], lhsT=STt[:, h, :], rhs=vrj[:, h * 64:(h + 1) * 64], start=True, stop=False),
                          [STt, vrj], [b5], 64)
                        T(lambda h=h: nc.tensor.matmul(b5[:, h * 64:(h + 1) * 64], lhsT=qkTj[0:64, h, :], rhs=Rbf[0:64, h, :],
                                                       start=False, stop=True), [qkTj, Rbf], [b5], 64)
                    for h in range(4):
                        o0 = (h % 2) * 129
                        T(lambda h=h, o0=o0: nc.tensor.matmul(ob(h)[:, o0:o0 + 129], lhsT=PTt[:, h, :], rhs=vm1j[:, h, :], start=True, stop=False), [PTt, vm1j], [ob(h)])
                        T(lambda h=h, o0=o0: nc.tensor.matmul(ob(h)[:, o0:o0 + 129], lhsT=qkaj[:, h, :], rhs=Sbf[:, h, :], start=False, stop=True), [qkaj, Sbf], [ob(h)])
                G(lambda: nc.gpsimd.tensor_copy(out=Rbf[0:64, :, :], in_=Rst[0:64, :, :]), [Rst], [Rbf], 512)
                for h in range(4):
                    A(lambda h=h: nc.scalar.activation(out=Sbf[:, h, :], in_=Tst[:, h, :], func=AF.Copy, scale=dcur[:, h:h + 1]), [Tst, dcur], [Sbf], 129)
                if not main:
                    return
                yield
                A(lambda: nc.scalar.activation(out=sqr[:, :], in_=b5[:, :], func=AF.Square), [b5], [sqr])
                V(lambda: nc.vector.reduce_sum(out=sm2[:, 0:8], in_=sqr[:, :].rearrange("p (h e) -> p h e", h=8), axis=AX.X), [sqr], [sm2], 512)
                A(lambda: nc.scalar.activation(out=sm2[:, 8:16], in_=sm2[:, 0:8], func=AF.Ln, bias=EPS, scale=1.0 / 64), [sm2], [sm2], 8)
                A(lambda: nc.scalar.activation(out=sm2[:, 16:24], in_=sm2[:, 8:16], func=AF.Exp, scale=-0.5), [sm2], [sm2], 8)
                V(lambda: nc.vector.tensor_tensor(out=rn[:, :].rearrange("p (h e) -> p h e", h=8), in0=b5[:, :].rearrange("p (h e) -> p h e", h=8),
                                                  in1=sm2[:, 16:24].unsqueeze(2).broadcast_to([P, 8, 64]), op=ALU.mult), [b5, sm2], [rn])
                G(lambda: nc.gpsimd.tensor_tensor(out=y[:, 0:512], in0=rn[:, :], in1=grj[:, :], op=ALU.mult), [rn, grj], [y], 512)
                yield
                if main:
                    for h in range(4):
                        o0 = (h % 2) * 129
                        V(lambda h=h, o0=o0: nc.vector.tensor_tensor(out=sm2[:, 24 + h:25 + h], in0=ob(h)[:, o0 + 128:o0 + 129], in1=smj[:, 32 + h:33 + h], op=ALU.mult),
                          [ob(h), smj], [sm2])
                    V(lambda: nc.vector.tensor_tensor(out=sm2[:, 28:32], in0=sm2[:, 24:28], in1=sm2[:, 24:28], op=ALU.mult), [sm2], [sm2])
                    V(lambda: nc.vector.tensor_scalar(out=sm2[:, 28:32], in0=sm2[:, 28:32], scalar1=1.0, scalar2=None, op0=ALU.max), [sm2], [sm2])
                    A(lambda: nc.scalar.activation(out=sm2[:, 32:36], in_=sm2[:, 28:32], func=AF.Ln), [sm2], [sm2])
                    A(lambda: nc.scalar.activation(out=sm2[:, 36:40], in_=sm2[:, 32:36], func=AF.Exp, scale=-0.5), [sm2], [sm2])
                    V(lambda: nc.vector.tensor_tensor(out=sm2[:, 40:44], in0=sm2[:, 36:40], in1=smj[:, 32:36], op=ALU.mult), [sm2, smj], [sm2])
                    for h in range(4):
                        o0 = (h % 2) * 129
                        V(lambda h=h, o0=o0: nc.vector.scalar_tensor_tensor(out=hg[:, h * P:(h + 1) * P], in0=ob(h)[:, o0:o0 + 128], scalar=sm2[:, 40 + h:41 + h],
                                                                            in1=goj[:, h * P:(h + 1) * P], op0=ALU.mult, op1=ALU.mult), [ob(h), sm2, goj], [hg])
                        A(lambda h=h: nc.scalar.activation(out=junk[:, h * P:(h + 1) * P], in_=hg[:, h * P:(h + 1) * P], func=AF.Square, accum_out=sm2[:, 44 + h:45 + h]),
                          [hg], [junk, sm2])
                    A(lambda: nc.scalar.activation(out=sm2[:, 48:52], in_=sm2[:, 44:48], func=AF.Ln, bias=EPS, scale=1.0 / 128), [sm2], [sm2])
                    A(lambda: nc.scalar.activation(out=sm2[:, 52:56], in_=sm2[:, 48:52], func=AF.Exp, scale=-0.5), [sm2], [sm2])
                    V(lambda: nc.vector.tensor_tensor(out=y[:, 512:1024].rearrange("p (h e) -> p h e", h=4), in0=hg[:, :].rearrange("p (h e) -> p h e", h=4),
                                                      in1=sm2[:, 52:56].unsqueeze(2).broadcast_to([P, 4, P]), op=ALU.mult), [hg, sm2], [y])
                yield
                transpose8(y, yT)
                for half in range(2):
                    bank = b3 if half == 0 else b5
                    for k in range(8):
                        T(lambda k=k, bank=bank, half=half: nc.tensor.matmul(bank[:, :], lhsT=yT[:, k, :], rhs=Wout[:, k, half * 512:(half + 1) * 512],
                                                                             start=(k == 0), stop=(k == 7)), [yT, Wout], [bank], 512)
                V(lambda: nc.vector.tensor_tensor(out=h1t[:, 0:512], in0=b3[:, :], in1=xt[:, 0:512], op=ALU.add), [b3, xt], [h1t], 512)
                V(lambda: nc.vector.tensor_tensor(out=h1t[:, 512:1024], in0=b5[:, :], in1=xt[:, 512:1024], op=ALU.add), [b5, xt], [h1t], 512)
                SP(lambda: nc.sync.dma_start(out=h1s.t[n * P:(n + 1) * P, :], in_=h1t[:, :]), [h1t], [h1s])

            def back2(gi):
                xsrc, n, main = seq[gi]
                h1t = h1t2[gi % 2]
                A(lambda: nc.scalar.activation(out=junk[:, :], in_=h1t[:, :], func=AF.Square, accum_out=sm2[:, 56:57]), [h1t], [junk, sm2])
                A(lambda: nc.scalar.activation(out=sm2[:, 57:58], in_=sm2[:, 56:57], func=AF.Ln, bias=EPS, scale=1.0 / D), [sm2], [sm2])
                A(lambda: nc.scalar.activation(out=sm2[:, 58:59], in_=sm2[:, 57:58], func=AF.Exp, scale=-0.5), [sm2], [sm2])
                V(lambda: nc.vector.scalar_tensor_tensor(out=xn2[:, :], in0=h1t[:, :], scalar=sm2[:, 58:59], in1=bmoe[:, :], op0=ALU.mult, op1=ALU.mult),
                  [h1t, sm2, bmoe], [xn2])
                transpose8(xn2, xn2T)
                for k in range(8):
                    T(lambda k=k: nc.tensor.matmul(ps_lg[:, 16:52], lhsT=xn2T[:, k, :], rhs=Wr[:, k, :], start=(k == 0), stop=(k == 7)), [xn2T, Wr], [ps_lg])
                yield
                V(lambda: nc.vector.tensor_tensor(out=rl[:, 0:36], in0=ps_lg[:, 16:52], in1=bcs[:, 0:36], op=ALU.add), [ps_lg, bcs], [rl])
                V(lambda: nc.vector.reduce_max(out=rl[:, 36:37], in_=rl[:, 0:4], axis=AX.X), [rl], [rl])
                V(lambda: nc.vector.tensor_scalar(out=rl[:, 37:38], in0=rl[:, 36:37], scalar1=-1.0, scalar2=None, op0=ALU.mult), [rl], [rl])
                A(lambda: nc.scalar.activation(out=rl[:, 44:48], in_=rl[:, 0:4], func=AF.Exp, bias=rl[:, 37:38], scale=1.0, accum_out=rl[:, 38:39]), [rl], [rl])
                V(lambda: nc.vector.reciprocal(out=rl[:, 39:40], in_=rl[:, 38:39]), [rl], [rl])
                V(lambda: nc.vector.tensor_scalar(out=rl[:, 40:44], in0=rl[:, 0:4], scalar1=rl[:, 36:37], scalar2=None, op0=ALU.is_ge), [rl], [rl])
                V(lambda: nc.vector.tensor_tensor(out=rl2[:, 0:32].rearrange("p (g e) -> p g e", g=4), in0=rl[:, 4:36].rearrange("p (g e) -> p g e", g=4),
                                                  in1=rl[:, 40:44].unsqueeze(2).broadcast_to([P, 4, 8]), op=ALU.mult), [rl], [rl2])
                V(lambda: nc.vector.reduce_sum(out=rl2[:, 32:40], in_=rl2[:, 0:32].rearrange("p (g e) -> p e g", g=4), axis=AX.X), [rl2], [rl2])
                V(lambda: nc.vector.max(out=m8[:, :], in_=rl2[:, 32:40]), [rl2], [m8])
                V(lambda: nc.vector.max_index(out=i8[:, :], in_max=m8[:, :], in_values=rl2[:, 32:40]), [m8, rl2], [i8])
                V(lambda: nc.vector.tensor_tensor(out=rl2[:, 40:44], in0=rl[:, 40:44], in1=cst[:, C_IOTA4:C_IOTA4 + 4], op=ALU.mult), [rl, cst], [rl2])
                V(lambda: nc.vector.reduce_sum(out=rl[:, 48:49], in_=rl2[:, 40:44], axis=AX.X), [rl2], [rl])
                V(lambda: nc.vector.tensor_tensor(out=rl[:, 49:50], in0=m8[:, 0:1], in1=m8[:, 1:2], op=ALU.subtract), [m8], [rl])
                A(lambda: nc.scalar.activation(out=rl[:, 50:51], in_=rl[:, 49:50], func=AF.Exp, scale=-1.0), [rl], [rl], 1)
                V(lambda: nc.vector.tensor_scalar(out=rl[:, 50:51], in0=rl[:, 50:51], scalar1=1.0, scalar2=None, op0=ALU.add), [rl], [rl], 1)
                V(lambda: nc.vector.reciprocal(out=rl[:, 50:51], in_=rl[:, 50:51]), [rl], [rl], 1)
                V(lambda: nc.vector.tensor_copy(out=rl2[:, 44:46], in_=i8[:, 0:2]), [i8], [rl2])
                V(lambda: nc.vector.scalar_tensor_tensor(out=rl[:, 51:53], in0=rl[:, 48:49].broadcast_to([P, 2]), scalar=8.0, in1=rl2[:, 44:46], op0=ALU.mult, op1=ALU.add),
                  [rl, rl2], [rl])
                yield
                for k_ in range(2):
                    V(lambda k_=k_: nc.vector.tensor_scalar(out=oh[:, k_, :], in0=cst[:, C_IOTA32:C_IOTA32 + 32], scalar1=rl[:, 51 + k_:52 + k_], scalar2=None, op0=ALU.is_equal),
                      [cst, rl], [oh])
                V(lambda: nc.vector.tensor_tensor(out=ohb[:, :], in0=oh[:, 0, :], in1=oh[:, 1, :], op=ALU.add), [oh], [ohb])
                T(lambda: nc.tensor.matmul(ps_pos[:, 64:96], lhsT=ustrb[:, :], rhs=ohb[:, :], start=True, stop=True), [ustrb, ohb], [ps_pos])
                T(lambda: nc.tensor.matmul(ps_tot[:, 96:128], lhsT=onesb[:, :], rhs=ohb[:, :], start=True, stop=True), [onesb, ohb], [ps_tot])
                V(lambda: nc.vector.tensor_tensor(out=pos_s[:, :], in0=ps_pos[:, 64:96], in1=CNT[:, :], op=ALU.add), [ps_pos, CNT], [pos_s])
                V(lambda: nc.vector.tensor_tensor(out=CNT[:, :], in0=ps_tot[:, 96:128], in1=CNT[:, :], op=ALU.add), [ps_tot, CNT], [CNT])
                for k_ in range(2):
                    V(lambda k_=k_: nc.vector.tensor_tensor(out=oh[:, 2, :], in0=oh[:, k_, :], in1=pos_s[:, :], op=ALU.mult), [oh, pos_s], [oh])
                    V(lambda k_=k_: nc.vector.reduce_sum(out=rl[:, 53 + k_:54 + k_], in_=oh[:, 2, :], axis=AX.X), [oh], [rl])
                V(lambda: nc.vector.tensor_scalar(out=rl[:, 55:57], in0=rl[:, 53:55], scalar1=float(CAP), scalar2=None, op0=ALU.is_lt), [rl], [rl])
                V(lambda: nc.vector.scalar_tensor_tensor(out=rl[:, 57:59], in0=rl[:, 51:53], scalar=float(CAP), in1=rl[:, 53:55], op0=ALU.mult, op1=ALU.add), [rl], [rl])
                V(lambda: nc.vector.tensor_scalar(out=rl2[:, 46:48], in0=rl[:, 55:57], scalar1=-4.0e6, scalar2=4.0e6, op0=ALU.mult, op1=ALU.add), [rl], [rl2])
                V(lambda: nc.vector.tensor_tensor(out=rl[:, 57:59], in0=rl[:, 57:59], in1=rl2[:, 46:48], op=ALU.add), [rl, rl2], [rl])
                V(lambda: nc.vector.tensor_copy(out=dest_i[:, n, :], in_=rl[:, 57:59]), [rl], [dest_i])
                V(lambda: nc.vector.tensor_tensor(out=rl2[:, 48:49], in0=rl[:, 39:40], in1=rl[:, 50:51], op=ALU.mult), [rl], [rl2])
                V(lambda: nc.vector.tensor_tensor(out=rl2[:, 49:50], in0=rl[:, 39:40], in1=rl2[:, 48:49], op=ALU.subtract), [rl, rl2], [rl2])
                V(lambda: nc.vector.tensor_tensor(out=gates[:, n, :], in0=rl2[:, 48:50], in1=rl[:, 55:57], op=ALU.mult), [rl2, rl], [gates])
                for k_ in range(2 if scatter else 0):
                    GD(lambda k_=k_: nc.gpsimd.indirect_dma_start(out=xbuf.t[:, :], out_offset=bass.IndirectOffsetOnAxis(ap=dest_i[:, n, k_:k_ + 1], axis=0),
                                                                   in_=xn2[:, :], in_offset=None, bounds_check=bc_reg, oob_is_err=False),
                       [xn2, dest_i, xbuf_z], [xbuf])

            def interleave(gens):
                gens = [g for g in gens if g is not None]
                while gens:
                    for g in list(gens):
                        try:
                            next(g)
                        except StopIteration:
                            gens.remove(g)

            def apply_carry():
                cm = cst[:, C_CARRY:C_CARRY + 1]
                V(lambda: nc.vector.tensor_scalar(out=Tst[:, :, :], in0=Tst[:, :, :], scalar1=cm, scalar2=None, op0=ALU.mult), [Tst, cst], [Tst])
                V(lambda: nc.vector.tensor_scalar(out=Sbf[:, :, :], in0=Sbf[:, :, :], scalar1=cm, scalar2=None, op0=ALU.mult), [Sbf, cst], [Sbf])
                V(lambda: nc.vector.tensor_scalar(out=Rst[:, :, :], in0=Rst[:, :, :], scalar1=cm, scalar2=None, op0=ALU.mult), [Rst, cst], [Rst])
                V(lambda: nc.vector.tensor_scalar(out=Rbf[:, :, :], in0=Rbf[:, :, :], scalar1=cm, scalar2=None, op0=ALU.mult), [Rbf, cst], [Rbf])
                for i in range(2):
                    V(lambda i=i: nc.vector.tensor_scalar(out=cv[i][:, :, 0:3], in0=cv[i][:, :, 0:3], scalar1=cm, scalar2=None, op0=ALU.mult), [cv[i], cst], [cv[i]])

            seq = [(x_pre, n, False) for n in range(PT)] + [(x_main, n, True) for n in range(NT)]
            NS = len(seq)
            load_x(0)
            if NS > 1:
                load_x(1)
            if PT > 0:
                make_trig(pos_pre, PT, xs[2])
                kb.merge([kb.record(pre(0))])
                pr1 = kb.record(pre(1)) if NS > 1 else []
                kb.merge([kb.record(front(0)), pr1])
                if NS > 2:
                    load_x(2)
                for gi in range(PT):
                    nxt = kb.record(front(gi + 1)) if gi + 1 < PT else []
                    prn = kb.record(pre(gi + 2)) if gi + 2 < NS else []
                    kb.merge([kb.record(back(gi)), nxt, prn])
                    if gi + 3 < NS:
                        load_x(gi + 3)
                    zfill()
                while zpending:
                    zfill()
                apply_carry()
                make_trig(pos_main, NT, h1t2[1])
                kb.merge([kb.record(front(PT))])
            else:
                while zpending:
                    zfill()
                make_trig(pos_main, NT, xs[2])
                kb.merge([kb.record(pre(0))])
                pr1 = kb.record(pre(1)) if NS > 1 else []
                kb.merge([kb.record(front(0)), pr1])
                if NS > 2:
                    load_x(2)
            for gi in range(PT, NS):
                nxt = kb.record(front(gi + 1)) if gi + 1 < NS else []
                prv = kb.record(back2(gi - 1)) if gi - 1 >= PT else []
                prn = kb.record(pre(gi + 2)) if gi + 2 < NS else []
                kb.merge([kb.record(back(gi)), prv, nxt, prn])
                if gi + 3 < NS:
                    load_x(gi + 3)
            kb.merge([kb.record(back2(NS - 1))])
            kb.barrier()
            if stop <= 3:
                return nc

        MODEL.update(MODEL_BC)
        with ExitStack() as bst:
            w1s = kb.sb(bst, "w1s", [P, 8, DEXP], F32)
            w3s = kb.sb(bst, "w3s", [P, 8, DEXP], F32)
            w2s = kb.sb(bst, "w2s", [P, 4, D], F32)
            w1b = [kb.sb(bst, "w1b%d" % i, [P, 8, DEXP], BF16) for i in range(2)]
            w3b = [kb.sb(bst, "w3b%d" % i, [P, 8, DEXP], BF16) for i in range(2)]
            w2b = [kb.sb(bst, "w2b%d" % i, [P, 4, D], BF16) for i in range(2)]
            xg = [kb.sb(bst, "xg%d" % i, [P, NB, D], BF16) for i in range(2)]
            xgT = [kb.sb(bst, "xgT%d" % i, [P, 8, CAP], BF16) for i in range(2)]
            sil = [kb.sb(bst, "sil%d" % i, [P, CAP], F32) for i in range(2)]
            hT = [kb.sb(bst, "hT%d" % i, [P, 4, CAP], BF16) for i in range(2)]
            yo = [kb.sb(bst, "yo%d" % i, [P, D], BF16) for i in range(3)]

            def load_expert(e):
                SP(lambda: nc.sync.dma_start(out=w1s[:, :, :], in_=w1.t[e, :, :].rearrange("(k p) n -> p k n", p=P)), [w1], [w1s])
                SP(lambda: nc.sync.dma_start(out=w3s[:, :, :], in_=w3.t[e, :, :].rearrange("(k p) n -> p k n", p=P)), [w3], [w3s])
                SP(lambda: nc.sync.dma_start(out=w2s[:, :, :], in_=w2.t[e, :, :].rearrange("(k p) n -> p k n", p=P)), [w2], [w2s])
                SP(lambda: nc.sync.dma_start(out=xg[e % 2][:, :, :], in_=xbuf.t[e * CAP:(e + 1) * CAP, :].rearrange("(a p) f -> p a f", p=P)), [xbuf], [xg[e % 2]])

            def head(e):
                j = e % 2
                for k in range(0, 8, 2):
                    G(lambda k=k: nc.gpsimd.tensor_copy(out=w1b[j][:, k:k + 2, :], in_=w1s[:, k:k + 2, :]), [w1s], [w1b[j]], 1024)
                for k in range(0, 8, 2):
                    A(lambda k=k: nc.scalar.copy(out=w3b[j][:, k:k + 2, :], in_=w3s[:, k:k + 2, :]), [w3s], [w3b[j]], 1024)
                for k in range(4):
                    V(lambda k=k: nc.vector.tensor_copy(out=w2b[j][:, k, :], in_=w2s[:, k, :]), [w2s], [w2b[j]], 1024)
                if e + 1 < NE:
                    load_expert(e + 1)
                yield
                for a in range(NB):
                    kb.lock(bkT)
                    for k in range(8):
                        T(lambda a=a, k=k: nc.tensor.transpose(out=bkT[:, k * P:(k + 1) * P], in_=xg[j][:, a, k * P:(k + 1) * P], identity=identb[:, :]), [xg[j], identb], [bkT])
                    if a % 2 == 0:
                        A(lambda a=a: nc.scalar.copy(out=xgT[j][:, :, a * P:(a + 1) * P], in_=bkT[:, :].rearrange("p (k t) -> p k t", k=8)), [bkT], [xgT[j]])
                    else:
                        V(lambda a=a: nc.vector.tensor_copy(out=xgT[j][:, :, a * P:(a + 1) * P], in_=bkT[:, :].rearrange("p (k t) -> p k t", k=8)), [bkT], [xgT[j]])
                    kb.unlock(bkT)
                yield
                for m in range(4):
                    pa, pb_ = (b0, b1) if m % 2 == 0 else (b2, b3)
                    for k in range(8):
                        T(lambda k=k, m=m, pa=pa: nc.tensor.matmul(pa[:, 0:CAP], lhsT=w1b[j][:, k, m * P:(m + 1) * P], rhs=xgT[j][:, k, :], start=(k == 0), stop=(k == 7)), [w1b[j], xgT[j]], [pa], CAP)
                    for k in range(8):
                        T(lambda k=k, m=m, pb_=pb_: nc.tensor.matmul(pb_[:, 0:CAP], lhsT=w3b[j][:, k, m * P:(m + 1) * P], rhs=xgT[j][:, k, :], start=(k == 0), stop=(k == 7)), [w3b[j], xgT[j]], [pb_], CAP)
                    s_ = sil[m % 2]
                    A(lambda pa=pa, s_=s_: nc.scalar.activation(out=s_[:, :], in_=pa[:, 0:CAP], func=AF.Silu), [pa], [s_], CAP)
                    V(lambda m=m, pb_=pb_, s_=s_: nc.vector.tensor_tensor(out=hT[j][:, m, :], in0=pb_[:, 0:CAP], in1=s_[:, :], op=ALU.mult), [pb_, s_], [hT[j]], CAP)
                    yield

            def tail(e):
                j = e % 2
                for a in range(NB):
                    yt = yo[(e * NB + a) % 3]
                    for half in range(2):
                        bank = b5 if half == 0 else b6
                        for m in range(4):
                            T(lambda a=a, m=m, half=half, bank=bank: nc.tensor.matmul(bank[:, :], lhsT=hT[j][:, m, a * P:(a + 1) * P], rhs=w2b[j][:, m, half * 512:(half + 1) * 512],
                                                                                      start=(m == 0), stop=(m == 3)), [hT[j], w2b[j]], [bank], 512)
                        if half == 0:
                            A(lambda yt=yt, bank=bank: nc.scalar.copy(out=yt[:, 0:512], in_=bank[:, :]), [bank], [yt], 512)
                        else:
                            V(lambda yt=yt, bank=bank: nc.vector.tensor_copy(out=yt[:, 512:1024], in_=bank[:, :]), [bank], [yt], 512)
                    r0 = e * CAP + a * P
                    SP(lambda yt=yt, r0=r0: nc.sync.dma_start(out=ybuf.t[r0:r0 + P, :], in_=yt[:, :]), [yt], [ybuf])
                    yield

            load_expert(0)
            kb.merge([kb.record(head(0))])
            for e in range(1, NE):
                kb.merge([kb.record(tail(e - 1)), kb.record(head(e))])
            kb.merge([kb.record(tail(NE - 1))])
            kb.barrier()
            if stop <= 4:
                return nc

        with ExitStack() as cstk:
            Wg = kb.sb(cstk, "Wg", [P, 8, D], BF16)
            Wu = kb.sb(cstk, "Wu", [P, 2, D], BF16)
            gkt2 = kb.sb(cstk, "gkt2", [P, 24], F32)
            bple = kb.sb(cstk, "bple", [P, D], F32)
            bfin = kb.sb(cstk, "bfin", [P, D], F32)
            SP(lambda: nc.sync.dma_start(out=gkt2[:, :], in_=gk[:, :]), [gk], [gkt2])
            SP(lambda: nc.sync.dma_start(out=bple[:, :], in_=bc_ple[:, :]), [bc_ple], [bple])
            SP(lambda: nc.sync.dma_start(out=bfin[:, :], in_=bc_fin[:, :]), [bc_fin], [bfin])
            with ExitStack() as lst:
                load_weight(lst, Wg, lambda c0, cw: w_gate.t[:, c0:c0 + cw].rearrange("(k p) n -> p k n", p=P), 8, D,
                            (lambda k: gkt2[:, 16 + k:17 + k], gkt2), "wg")
                load_weight(lst, Wu, lambda c0, cw: w_up.t[:, c0:c0 + cw].rearrange("(k p) n -> p k n", p=P), 2, D, None, "wu")
                kb.barrier()
            h1c = [kb.sb(cstk, "h1c%d" % i, [P, D], F32) for i in range(4)]
            Y1 = [kb.sb(cstk, "Y1_%d" % i, [P, D], BF16) for i in range(4)]
            Y2 = [kb.sb(cstk, "Y2_%d" % i, [P, D], BF16) for i in range(4)]
            pt = [kb.sb(cstk, "pt%d" % i, [P, PLE], F32) for i in range(4)]
            ptb = [kb.sb(cstk, "ptb%d" % i, [P, PLE], BF16) for i in range(2)]
            pT = [kb.sb(cstk, "pT%d" % i, [P, 2, P], BF16) for i in range(2)]
            h2 = [kb.sb(cstk, "h2_%d" % i, [P, D], F32) for i in range(2)]
            hb = [kb.sb(cstk, "hb%d" % i, [P, D], BF16) for i in range(2)]
            hbT = [kb.sb(cstk, "hbT%d" % i, [P, 8, P], BF16) for i in range(2)]
            gt = [kb.sb(cstk, "gt%d" % i, [P, D], F32) for i in range(2)]
            et = [kb.sb(cstk, "et%d" % i, [P, D], F32) for i in range(2)]
            junk2 = kb.sb(cstk, "junk2", [P, D], BF16)
            ot = [kb.sb(cstk, "ot%d" % i, [P, D], F32) for i in range(2)]
            sc = [kb.sb(cstk, "sc%d" % i, [P, 16], F32) for i in range(2)]
            for i in range(4):
                G(lambda i=i: nc.gpsimd.memset(Y1[i][:, :], 0.0), [], [Y1[i]])
                G(lambda i=i: nc.gpsimd.memset(Y2[i][:, :], 0.0), [], [Y2[i]])

            def load_C(n):
                j = n % 4
                SP(lambda: nc.sync.dma_start(out=h1c[j][:, :], in_=h1s.t[n * P:(n + 1) * P, :]), [h1s], [h1c[j]])
                SP(lambda: nc.sync.dma_start(out=pt[j][:, :], in_=p_main.t[n * P:(n + 1) * P, :]), [p_main], [pt[j]])
                for (k_, Yk) in ((0, Y1[j]), (1, Y2[j])):
                    GD(lambda k_=k_, Yk=Yk: nc.gpsimd.indirect_dma_start(out=Yk[:, :], out_offset=None, in_=ybuf.t[:, :],
                                                                          in_offset=bass.IndirectOffsetOnAxis(ap=dest_i[:, n, k_:k_ + 1], axis=0),
                                                                          bounds_check=bc_reg, oob_is_err=False), [ybuf, dest_i], [Yk])

            def tile_C(n):
                j = n % 2
                j4 = n % 4
                gb, pb0, pb1 = (b0, b1, b2) if j == 0 else (b3, b5, b6)
                h2j, hbj, hbTj, gtj, etj, scj, ptbj, pTj, o_ = h2[j], hb[j], hbT[j], gt[j], et[j], sc[j], ptb[j], pT[j], ot[j]
                V(lambda: nc.vector.scalar_tensor_tensor(out=h2j[:, :], in0=Y1[j4][:, :], scalar=gates[:, n, 0:1], in1=h1c[j4][:, :], op0=ALU.mult, op1=ALU.add),
                  [Y1[j4], gates, h1c[j4]], [h2j])
                V(lambda: nc.vector.scalar_tensor_tensor(out=h2j[:, :], in0=Y2[j4][:, :], scalar=gates[:, n, 1:2], in1=h2j[:, :], op0=ALU.mult, op1=ALU.add),
                  [Y2[j4], gates, h2j], [h2j])
                A(lambda: nc.scalar.activation(out=junk2[:, :], in_=h2j[:, :], func=AF.Square, accum_out=scj[:, 0:1]), [h2j], [junk2, scj])
                A(lambda: nc.scalar.activation(out=scj[:, 1:2], in_=scj[:, 0:1], func=AF.Ln, bias=EPS, scale=1.0 / D), [scj], [scj], 1)
                A(lambda: nc.scalar.activation(out=scj[:, 2:3], in_=scj[:, 1:2], func=AF.Exp, scale=-0.5), [scj], [scj], 1)
                A(lambda: nc.scalar.activation(out=hbj[:, :], in_=h2j[:, :], func=AF.Copy, scale=scj[:, 2:3]), [h2j, scj], [hbj])
                yield
                G(lambda: nc.gpsimd.tensor_copy(out=ptbj[:, :], in_=pt[j4][:, :]), [pt[j4]], [ptbj], 256)
                kb.lock(bkT)
                for k in range(2):
                    T(lambda k=k: nc.tensor.transpose(out=bkT[:, k * P:(k + 1) * P], in_=ptbj[:, k * P:(k + 1) * P], identity=identb[:, :]), [ptbj, identb], [bkT])
                A(lambda: nc.scalar.copy(out=pTj[:, :, :], in_=bkT[:, 0:2 * P].rearrange("p (k t) -> p k t", k=2)), [bkT], [pTj], 256)
                kb.unlock(bkT)
                for half in range(2):
                    bank = pb0 if half == 0 else pb1
                    for k in range(2):
                        T(lambda k=k, bank=bank, half=half: nc.tensor.matmul(bank[:, :], lhsT=pTj[:, k, :], rhs=Wu[:, k, half * 512:(half + 1) * 512], start=(k == 0), stop=(k == 1)),
                          [pTj, Wu], [bank], 512)
                    A(lambda bank=bank, half=half: nc.scalar.activation(out=junk2[:, half * 512:(half + 1) * 512], in_=bank[:, :], func=AF.Square, accum_out=scj[:, 4 + half:5 + half]),
                      [bank], [junk2, scj], 512)
                V(lambda: nc.vector.tensor_tensor(out=scj[:, 6:7], in0=scj[:, 4:5], in1=scj[:, 5:6], op=ALU.add), [scj], [scj], 1)
                A(lambda: nc.scalar.activation(out=scj[:, 7:8], in_=scj[:, 6:7], func=AF.Ln, bias=EPS, scale=1.0 / D), [scj], [scj], 1)
                A(lambda: nc.scalar.activation(out=scj[:, 8:9], in_=scj[:, 7:8], func=AF.Exp, scale=-0.5), [scj], [scj], 1)
                for half in range(2):
                    bank = pb0 if half == 0 else pb1
                    sl = slice(half * 512, (half + 1) * 512)
                    V(lambda bank=bank, sl=sl: nc.vector.scalar_tensor_tensor(out=etj[:, sl], in0=bank[:, :], scalar=scj[:, 8:9], in1=bple[:, sl], op0=ALU.mult, op1=ALU.mult),
                      [bank, scj, bple], [etj], 512)
                yield
                kb.mark()
                kb.lock(bkT)
                for k in range(8):
                    T(lambda k=k: nc.tensor.transpose(out=bkT[:, k * P:(k + 1) * P], in_=hbj[:, k * P:(k + 1) * P], identity=identb[:, :]), [hbj, identb], [bkT])
                A(lambda: nc.scalar.copy(out=hbTj[:, :, :], in_=bkT[:, :].rearrange("p (k t) -> p k t", k=8)), [bkT], [hbTj])
                kb.unlock(bkT)
                gbs = (gb, pb0)
                for half in range(2):
                    for k in range(8):
                        T(lambda k=k, half=half: nc.tensor.matmul(gbs[half][:, :], lhsT=hbTj[:, k, :], rhs=Wg[:, k, half * 512:(half + 1) * 512], start=(k == 0), stop=(k == 7)),
                          [hbTj, Wg], [gbs[half]], 512)
                kb.lock_engine("act")
                for half in range(2):
                    A(lambda half=half: nc.scalar.activation(out=gtj[:, half * 512:(half + 1) * 512], in_=gbs[half][:, :], func=AF.Sigmoid), [gbs[half]], [gtj], 512)
                kb.unlock_engine("act")
                yield
                G(lambda: nc.gpsimd.tensor_tensor(out=etj[:, :], in0=etj[:, :], in1=gtj[:, :], op=ALU.mult), [etj, gtj], [etj])
                V(lambda: nc.vector.tensor_tensor(out=h2j[:, :], in0=h2j[:, :], in1=etj[:, :], op=ALU.add), [h2j, etj], [h2j])
                A(lambda: nc.scalar.activation(out=junk2[:, :], in_=h2j[:, :], func=AF.Square, accum_out=scj[:, 10:11]), [h2j], [junk2, scj])
                A(lambda: nc.scalar.activation(out=scj[:, 11:12], in_=scj[:, 10:11], func=AF.Ln, bias=EPS, scale=1.0 / D), [scj], [scj], 1)
                A(lambda: nc.scalar.activation(out=scj[:, 12:13], in_=scj[:, 11:12], func=AF.Exp, scale=-0.5), [scj], [scj], 1)
                V(lambda: nc.vector.scalar_tensor_tensor(out=o_[:, :], in0=h2j[:, :], scalar=scj[:, 12:13], in1=bfin[:, :], op0=ALU.mult, op1=ALU.mult),
                  [h2j, scj, bfin], [o_])
                SP(lambda: nc.sync.dma_start(out=out_d.t[n * P:(n + 1) * P, :], in_=o_[:, :]), [o_], [out_d])

            load_C(0)
            if NT > 1:
                load_C(1)
            prev2 = []
            for n in range(NT):
                if n + 2 < NT:
                    load_C(n + 2)
                st_ = kb.record(tile_C(n))
                mk = st_.index(("mark",))
                kb.merge([prev2, st_[:mk]])
                prev2 = st_[mk + 1:]
            kb.merge([prev2])
            kb.barrier()
        build_program.stats = (kb.n_inst, kb.n_wait)
    return nc


def make_consts(carry):
    c = np.zeros((P, CST_W), np.float32)
    idx = np.arange(P)
    c[:, C_ID:C_ID + P] = np.eye(P, dtype=np.float32)
    c[:, C_MASK:C_MASK + P] = (idx[None, :] >= idx[:, None]).astype(np.float32)
    c[:, C_USTR:C_USTR + P] = (idx[:, None] < idx[None, :]).astype(np.float32)
    c[:, C_ONES:C_ONES + P] = 1.0
    h = np.arange(8, dtype=np.float64)
    lg = np.log1p(-(2.0 ** (-5.0 - h)))
    t = idx.astype(np.float64)
    gq = np.exp(lg[None, :] * t[:, None])
    gkk = np.exp(-lg[None, :] * t[:, None]) * (64.0 ** -0.5)
    c[:, C_GQ:C_GQ + 8] = gq
    c[:, C_GK:C_GK + 8] = gkk
    gC = np.exp(lg * 128.0)
    c[:, C_GC:C_GC + 8] = gC[None, :]
    freqs = (10000.0 ** (-np.arange(32, dtype=np.float32) / 32)).astype(np.float32)
    c[:, C_FREQ:C_FREQ + 32] = freqs[None, :]
    c[:, C_IOTA32:C_IOTA32 + 32] = np.arange(32, dtype=np.float32)[None, :]
    c[:, C_IOTA4:C_IOTA4 + 4] = np.arange(4, dtype=np.float32)[None, :]
    c[:, C_CARRY] = carry
    return c


def arr_pk(v):
    return np.ascontiguousarray(v.reshape(-1, P).T)


def rep(v):
    return np.ascontiguousarray(np.broadcast_to(v.reshape(1, -1), (P, v.size))).astype(np.float32)


def shared_maps(inp):
    f = lambda a: np.ascontiguousarray(np.asarray(a, dtype=np.float32))
    m = {}
    m["w_in"] = f(inp["w_in"][0])
    m["w_out"] = f(inp["w_out"][0])
    m["w1"] = f(inp["w1"][0])
    m["w3"] = f(inp["w3"][0])
    m["w2"] = f(inp["w2"][0])
    m["w_up"] = f(inp["w_ple_up"][0])
    m["w_gate"] = f(inp["w_ple_gate"][0])
    m["wr"] = np.ascontiguousarray(np.concatenate([f(inp["w_group"][0]), f(inp["w_router"][0])], axis=1))
    gout = np.concatenate([f(inp["ret_gn"][0]), f(inp["ml_gn"][0])])
    m["gk"] = np.ascontiguousarray(np.concatenate([arr_pk(f(inp["attn_norm"][0])), arr_pk(gout), arr_pk(f(inp["ple_gate_norm"][0]))], axis=1))
    m["bc_moe"] = rep(f(inp["moe_norm"][0]))
    m["bc_ple"] = rep(f(inp["ple_norm"][0]))
    m["bc_fin"] = rep(f(inp["final_norm"]))
    m["bc_small"] = rep(np.concatenate([f(inp["b_group"][0]), f(inp["b_router"][0]), f(inp["b_igate"][0]), f(inp["b_fgate"][0])]))
    cw = f(inp["conv_w"][0])
    cb = f(inp["conv_b"][0])
    cwa = np.zeros((P, 8, 4), np.float32)
    for c in range(8):
        cwa[:, c, :] = cw[:, c * P:(c + 1) * P].T
    m["convw"] = np.ascontiguousarray(np.concatenate([cwa.reshape(P, 32), arr_pk(cb)], axis=1))
    m["convb_row"] = np.ascontiguousarray(cb.reshape(1, 1024))
    return m


_CACHE = {}


def kernel(**inputs):
    x = np.asarray(inputs["x"], dtype=np.float32)
    p = np.asarray(inputs["p"], dtype=np.float32)[0]
    pos = np.asarray(inputs["positions"]).astype(np.int32)
    B, S, _ = x.shape
    half = S // 2
    NT = half // P
    PT = NT
    key = (NT, PT)
    if key not in _CACHE:
        _CACHE[key] = build_program(NT, PT)
    nc = _CACHE[key]
    sh = shared_maps(inputs)
    zeros_src = np.zeros((1024, 512), np.float32)
    in_maps = []
    for core in range(8):
        b, s = core // 2, core % 2
        m = dict(sh)
        lo = s * half
        m["x_main"] = np.ascontiguousarray(x[b, lo:lo + half])
        m["pos_main"] = np.ascontiguousarray(pos[b, lo:lo + half].reshape(NT, P).T)
        m["p_main"] = np.ascontiguousarray(p[b, lo:lo + half])
        if s == 0:
            m["x_pre"] = np.zeros((half, D), np.float32)
            m["pos_pre"] = np.zeros((P, PT), np.int32)
        else:
            m["x_pre"] = np.ascontiguousarray(x[b, 0:half])
            m["pos_pre"] = np.ascontiguousarray(pos[b, 0:half].reshape(PT, P).T)
        m["cst"] = make_consts(float(s))
        m["zsrc"] = zeros_src
        in_maps.append(m)
    res = run_bass_kernel_spmd(nc, in_maps, core_ids=list(range(8)))
    out = np.empty((B, S, D), np.float32)
    for core in range(8):
        b, s = core // 2, core % 2
        out[b, s * half:(s + 1) * half] = res.results[core]["out"]
    return out
```

```python
import math
from contextlib import ExitStack
import numpy as np
import concourse.bass as bass
import concourse.mybir as mybir
from concourse.bass_utils import run_bass_kernel_spmd

F32 = mybir.dt.float32
BF16 = mybir.dt.bfloat16
I32 = mybir.dt.int32
U32 = mybir.dt.uint32
ALU = mybir.AluOpType
AF = mybir.ActivationFunctionType
AX = mybir.AxisListType

P = 128
D = 1024
NPROJ = 4104
NE = 32
DEXP = 512
PLE = 256
EPS = 1e-6
TWO_PI = 2.0 * math.pi

C_ID, C_MASK, C_USTR, C_ONES = 0, 128, 256, 384
C_GQ, C_GK = 512, 520
C_GC = 528
C_FREQ = 536
C_IOTA32 = 568
C_IOTA4 = 600
C_CARRY = 604
CST_W = 608


MODEL_A = {'pa': 30.0, 'pb': 0.3, 'lat': 0.0, 'ga': 100.0, 'gb': 2.0, 'va': 50.0, 'vb': 1.0}
MODEL_BC = {'pa': 64.0, 'pb': 0.45, 'lat': 60.0, 'ga': 300.0, 'gb': 3.0, 'va': 150.0, 'vb': 1.2}
MODEL = dict(MODEL_A)


class Res:
    __slots__ = ("name", "w", "r", "sem", "cnt", "t", "tw", "tr", "fsz")

    def __init__(self, name, t=None):
        self.name, self.t = name, t
        self.w, self.r = {}, {}
        self.sem, self.cnt = None, 0
        self.tw = self.tr = 0.0
        try:
            sh = list(t.shape)
            f = 1
            for d_ in sh[1:]:
                f *= d_
            self.fsz = f
        except Exception:
            self.fsz = 512

    def __getitem__(self, key):
        return self.t[key]


class KB:
    SAME_ENGINE_SYNC = True

    def __init__(self, nc, stack):
        self.nc, self.stack = nc, stack
        self.eng = {"pe": nc.tensor, "act": nc.scalar, "dve": nc.vector, "pool": nc.gpsimd, "sp": nc.sync}
        self.sem, self.cnt, self.waited = {}, {}, {}
        for e in self.eng:
            self.sem[e] = stack.enter_context(nc.semaphore("s_" + e))
            self.cnt[e] = 0
            self.waited[e] = {}
        self.all_res = []
        self.n_inst = self.n_wait = 0
        self.rec = None
        self.tE = {e: 0.0 for e in self.eng}

    def sb(self, st, name, shape, dt):
        r = Res(name, st.enter_context(self.nc.sbuf_tensor(name, list(shape), dt)))
        self.all_res.append(r)
        return r

    def ps(self, st, name, shape, dt):
        r = Res(name, st.enter_context(self.nc.psum_tensor(name, list(shape), dt)))
        self.all_res.append(r)
        return r

    def view(self, name, t):
        r = Res(name, t)
        self.all_res.append(r)
        return r

    def _waits(self, e, reads, writes):
        need = {}
        for r in reads:
            for k, sv in r.w.items():
                if need.get(k, (None, 0))[1] < sv[1]:
                    need[k] = sv
        for w in writes:
            for d in (w.w, w.r):
                for k, sv in d.items():
                    if need.get(k, (None, 0))[1] < sv[1]:
                        need[k] = sv
        wd = self.waited[e]
        for k, (s, v) in need.items():
            if k == e and (e == "pe" or not self.SAME_ENGINE_SYNC):
                continue
            if wd.get(k, 0) >= v:
                continue
            self.eng[e].wait_ge(s, v)
            self.n_wait += 1
            wd[k] = v

    def _dur(self, kind, e, writes, n):
        if kind == "dma":
            return 100.0
        if e == "pe":
            return MODEL['pa'] + MODEL['pb'] * (n if n is not None else 128)
        f = min(writes[0].fsz, 1024) if n is None else n
        if e == "pool":
            return MODEL['ga'] + MODEL['gb'] * f
        return MODEL['va'] + MODEL['vb'] * f

    def _model(self, kind, e, reads, writes, n, commit):
        t = self.tE[e]
        for r in reads:
            if r.tw > t:
                t = r.tw
        for w in writes:
            if w.tw > t:
                t = w.tw
            if w.tr > t:
                t = w.tr
        if commit:
            d = self._dur(kind, e, writes, n)
            self.tE[e] = t + d
            end = t + (2500.0 if kind == "dma" else d + MODEL['lat'])
            for r in reads:
                if end > r.tr:
                    r.tr = end
            for w in writes:
                w.tw = end
        return t

    def lock(self, res):
        if self.rec is not None:
            self.rec.append(("lock", res))

    def unlock(self, res):
        if self.rec is not None:
            self.rec.append(("unlock", res))

    def mark(self):
        if self.rec is not None:
            self.rec.append(("mark",))

    def lock_engine(self, e):
        if self.rec is not None:
            self.rec.append(("elock", e))

    def unlock_engine(self, e):
        if self.rec is not None:
            self.rec.append(("eunlock", e))

    def record(self, gen):
        assert self.rec is None
        self.rec = []
        for _ in gen:
            pass
        out, self.rec = self.rec, None
        return out

    def merge(self, streams):
        streams = [st for st in streams if st]
        idx = [0] * len(streams)
        locks = {}
        elocks = {}
        while True:
            best = None
            for i, st in enumerate(streams):
                if idx[i] >= len(st):
                    continue
                it = st[idx[i]]
                if it[0] == "lock":
                    if locks.get(it[1].name, i) != i:
                        continue
                    t = -2.0
                elif it[0] == "unlock" or it[0] == "eunlock":
                    t = -3.0
                elif it[0] == "elock":
                    if elocks.get(it[1], i) != i:
                        continue
                    t = -2.0
                else:
                    kind, e, fn, reads, writes, n = it
                    blocked = elocks.get(e, i) != i
                    for r in reads + writes:
                        if locks.get(r.name, i) != i:
                            blocked = True
                            break
                    if blocked:
                        continue
                    t = self._model(kind, e, reads, writes, n, False)
                if best is None or t < best[0]:
                    best = (t, i)
            if best is None:
                assert all(idx[i] >= len(st) for i, st in enumerate(streams)), "merge deadlock"
                return
            i = best[1]
            it = streams[i][idx[i]]
            idx[i] += 1
            if it[0] == "lock":
                locks[it[1].name] = i
            elif it[0] == "unlock":
                locks.pop(it[1].name, None)
            elif it[0] == "elock":
                elocks[it[1]] = i
            elif it[0] == "eunlock":
                elocks.pop(it[1], None)
            elif it[0] == "op":
                self.op(it[1], it[2], it[3], it[4], it[5])
            else:
                self.dma(it[1], it[2], it[3], it[4])

    def op(self, e, fn, reads=(), writes=(), n=None):
        if self.rec is not None:
            self.rec.append(("op", e, fn, tuple(reads), tuple(writes), n))
            return None
        self._model("op", e, reads, writes, n, True)
        self._waits(e, reads, writes)
        ins = fn()
        self.cnt[e] += 1
        tok = (self.sem[e], self.cnt[e])
        ins.then_inc(self.sem[e], 1)
        self.n_inst += 1
        for r in reads:
            r.r[e] = tok
        for w in writes:
            w.w[e] = tok
        return ins

    def dma(self, e, fn, reads=(), writes=()):
        if self.rec is not None:
            self.rec.append(("dma", e, fn, tuple(reads), tuple(writes), None))
            return None
        self._model("dma", e, reads, writes, None, True)
        self._waits(e, reads, writes)
        tgt = writes[0]
        if tgt.sem is None:
            tgt.sem = self.stack.enter_context(self.nc.semaphore("d_" + tgt.name))
        ins = fn()
        tgt.cnt += 16
        ins.then_inc(tgt.sem, 16)
        key = "dma:" + tgt.name
        tok = (tgt.sem, tgt.cnt)
        self.n_inst += 1
        for r in reads:
            r.r[key] = tok
        for w in writes:
            w.w[key] = tok
        return ins

    def barrier(self):
        for e in self.eng:
            wd = self.waited[e]
            for e2 in self.eng:
                if self.cnt[e2] == 0 or (e2 == e and (e == "pe" or not self.SAME_ENGINE_SYNC)):
                    continue
                if wd.get(e2, 0) < self.cnt[e2]:
                    self.eng[e].wait_ge(self.sem[e2], self.cnt[e2])
                    wd[e2] = self.cnt[e2]
            for r in self.all_res:
                if r.sem is not None and r.cnt > 0:
                    k = "dma:" + r.name
                    if wd.get(k, 0) < r.cnt:
                        self.eng[e].wait_ge(r.sem, r.cnt)
                        wd[k] = r.cnt


def build_program(NT, PT, CAP=512, stop=9, scatter=True, sub=99):
    nc = bass.Bass("TRN2", target_bir_lowering=False)
    NB = CAP // P
    NROWS = NE * CAP
    dscale = 128.0 ** -0.5

    def din(name, shape, dt=F32):
        return Res(name, nc.dram_tensor(name, list(shape), dt, kind="ExternalInput"))

    x_main = din("x_main", [NT * P, D])
    x_pre = din("x_pre", [max(PT, 1) * P, D])
    pos_main = din("pos_main", [P, NT], I32)
    pos_pre = din("pos_pre", [P, max(PT, 1)], I32)
    p_main = din("p_main", [NT * P, PLE])
    w_in = din("w_in", [D, NPROJ])
    w_out = din("w_out", [D, D])
    w1 = din("w1", [NE, D, DEXP])
    w3 = din("w3", [NE, D, DEXP])
    w2 = din("w2", [NE, DEXP, D])
    w_up = din("w_up", [PLE, D])
    w_gate = din("w_gate", [D, D])
    wr = din("wr", [D, 36])
    gk = din("gk", [P, 24])
    bc_moe = din("bc_moe", [P, D])
    bc_ple = din("bc_ple", [P, D])
    bc_fin = din("bc_fin", [P, D])
    bc_small = din("bc_small", [P, 44])
    convw = din("convw", [P, 40])
    cst_d = din("cst", [P, CST_W])
    zsrc = din("zsrc", [1024, 512])
    convb_row = din("convb_row", [1, 1024])
    out_d = Res("out", nc.dram_tensor("out", [NT * P, D], F32, kind="ExternalOutput"))
    h1s = Res("h1s", nc.dram_tensor("h1s", [NT * P, D], F32, kind="Internal"))
    xbuf = Res("xbuf", nc.dram_tensor("xbuf", [NROWS, D], BF16, kind="Internal"))
    ybuf = Res("ybuf", nc.dram_tensor("ybuf", [NROWS, D], BF16, kind="Internal"))

    with ExitStack() as gst:
        kb = KB(nc, gst)
        kb.all_res += [out_d, h1s, xbuf, ybuf]
        V = lambda fn, r=(), w=(), n=None: kb.op("dve", fn, r, w, n)
        A = lambda fn, r=(), w=(), n=None: kb.op("act", fn, r, w, n)
        G = lambda fn, r=(), w=(), n=None: kb.op("pool", fn, r, w, n)
        T = lambda fn, r=(), w=(), n=None: kb.op("pe", fn, r, w, n)
        SP = lambda fn, r=(), w=(): kb.dma("sp", fn, r, w)
        GD = lambda fn, r=(), w=(): kb.dma("pool", fn, r, w)

        bc_reg = nc.gpsimd.to_reg(NROWS - 1)
        cst = kb.sb(gst, "cstt", [P, CST_W], F32)
        identb = kb.sb(gst, "identb", [P, P], BF16)
        maskb = kb.sb(gst, "maskb", [P, P], BF16)
        ustrb = kb.sb(gst, "ustrb", [P, P], BF16)
        onesb = kb.sb(gst, "onesb", [P, P], BF16)
        dest_i = kb.sb(gst, "dest_i", [P, NT, 2], I32)
        gates = kb.sb(gst, "gates", [P, NT, 2], F32)
        bcs = kb.sb(gst, "bcs", [P, 44], F32)
        bk = [kb.ps(gst, "bk%d" % i, [P, 512], F32) for i in range(4)]
        bkT = kb.ps(gst, "bkT", [P, 1024], BF16)
        bk += [kb.ps(gst, "bk%d" % i, [P, 512], F32) for i in (5, 6)]
        bk7t = gst.enter_context(nc.psum_tensor("bk7", [P, 512], F32))
        ps_g = kb.view("bk7", bk7t)
        ps_cs = ps_lg = ps_pos = ps_tot = ps_g
        b0, b1, b2, b3, b5, b6 = bk

        SP(lambda: nc.sync.dma_start(out=cst[:, :], in_=cst_d[:, :]), [cst_d], [cst])
        xbuf_z = kb.view("xbuf_z", xbuf.t)
        zpending = list(range(NROWS // 1024))

        def zfill():
            if zpending:
                zi = zpending.pop(0)
                SP(lambda: nc.sync.dma_start(out=xbuf.t[zi * 1024:(zi + 1) * 1024, :], in_=zsrc.t[:, :].bitcast(BF16)), [zsrc], [xbuf_z])
        SP(lambda: nc.sync.dma_start(out=bcs[:, :], in_=bc_small[:, :]), [bc_small], [bcs])
        G(lambda: nc.gpsimd.tensor_copy(out=identb[:, :], in_=cst[:, C_ID:C_ID + P]), [cst], [identb])
        G(lambda: nc.gpsimd.tensor_copy(out=ustrb[:, :], in_=cst[:, C_USTR:C_USTR + P]), [cst], [ustrb])
        G(lambda: nc.gpsimd.tensor_copy(out=onesb[:, :], in_=cst[:, C_ONES:C_ONES + P]), [cst], [onesb])
        G(lambda: nc.gpsimd.memset(dest_i[:, :, :], 0), [], [dest_i])
        G(lambda: nc.gpsimd.memset(gates[:, :, :], 0.0), [], [gates])
        ident_f = cst
        mask_ap = lambda: cst[:, C_MASK:C_MASK + P]

        def rsqrt_chain(dst_ap, src_ap, n, res_list_r, res_list_w, tmp):
            A(lambda: nc.scalar.activation(out=tmp_ap(tmp, src_ap), in_=src_ap, func=AF.Ln, bias=EPS, scale=1.0 / n),
              res_list_r, [tmp])
            A(lambda: nc.scalar.activation(out=dst_ap, in_=tmp_ap(tmp, src_ap), func=AF.Exp, scale=-0.5),
              [tmp], res_list_w)

        def tmp_ap(tmp, like):
            w = like.shape[-1] if len(like.shape) == 2 else None
            return tmp[:, 0:w]

        def load_weight(st, dst, src_ap_fn, nk, ncols, gscale, qname, blk=512):
            stg = [kb.sb(st, qname + "_stg%d" % i, [P, nk, blk], F32) for i in range(2)]
            i = 0
            for c0 in range(0, ncols, blk):
                cw = min(blk, ncols - c0)
                s_ = stg[i % 2]
                SP(lambda s_=s_, c0=c0, cw=cw: nc.sync.dma_start(out=s_[:, :, 0:cw], in_=src_ap_fn(c0, cw)), [], [s_])
                for k in range(nk):
                    eng = ("dve", "act")[(i * nk + k) % 2]
                    o_ = dst[:, k, c0:c0 + cw]
                    i_ = s_[:, k, 0:cw]
                    if gscale is None:
                        if eng == "act":
                            A(lambda o_=o_, i_=i_: nc.scalar.copy(out=o_, in_=i_), [s_], [dst])
                        elif eng == "dve":
                            V(lambda o_=o_, i_=i_: nc.vector.tensor_copy(out=o_, in_=i_), [s_], [dst])
                        else:
                            G(lambda o_=o_, i_=i_: nc.gpsimd.tensor_copy(out=o_, in_=i_), [s_], [dst])
                    else:
                        gs, gres = gscale
                        sc = gs(k)
                        if eng == "act":
                            A(lambda o_=o_, i_=i_, sc=sc: nc.scalar.activation(out=o_, in_=i_, func=AF.Copy, scale=sc), [s_, gres], [dst])
                        elif eng == "dve":
                            V(lambda o_=o_, i_=i_, sc=sc: nc.vector.tensor_scalar(out=o_, in0=i_, scalar1=sc, scalar2=None, op0=ALU.mult), [s_, gres], [dst])
                        else:
                            G(lambda o_=o_, i_=i_, sc=sc: nc.gpsimd.tensor_scalar(out=o_, in0=i_, scalar1=sc, scalar2=None, op0=ALU.mult), [s_, gres], [dst])
                i += 1

        MODEL.update(MODEL_A)
        with ExitStack() as ast:
            Win = kb.sb(ast, "Win", [P, 8, NPROJ], BF16)
            Wout = kb.sb(ast, "Wout", [P, 8, D], BF16)
            Wr = kb.sb(ast, "Wr", [P, 8, 36], BF16)
            gkt = kb.sb(ast, "gkt", [P, 24], F32)
            cvw = kb.sb(ast, "cvw", [P, 40], F32)
            bmoe = kb.sb(ast, "bmoe", [P, D], F32)
            SP(lambda: nc.sync.dma_start(out=gkt[:, :], in_=gk[:, :]), [gk], [gkt])
            SP(lambda: nc.sync.dma_start(out=cvw[:, :], in_=convw[:, :]), [convw], [cvw])
            SP(lambda: nc.sync.dma_start(out=bmoe[:, :], in_=bc_moe[:, :]), [bc_moe], [bmoe])
            with ExitStack() as lst:
                load_weight(lst, Win, lambda c0, cw: w_in.t[:, c0:c0 + cw].rearrange("(k p) n -> p k n", p=P), 8, NPROJ,
                            (lambda k: gkt[:, k:k + 1], gkt), "win")
                load_weight(lst, Wout, lambda c0, cw: w_out.t[:, c0:c0 + cw].rearrange("(k p) n -> p k n", p=P), 8, D,
                            (lambda k: gkt[:, 8 + k:9 + k], gkt), "wout")
                wrs = kb.sb(lst, "wrs", [P, 8, 36], F32)
                SP(lambda: nc.sync.dma_start(out=wrs[:, :, :], in_=wr.t[:, :].rearrange("(k p) n -> p k n", p=P)), [wr], [wrs])
                V(lambda: nc.vector.tensor_copy(out=Wr[:, :, :], in_=wrs[:, :, :]), [wrs], [Wr])
                kb.barrier()
                if stop <= 1:
                    return nc

            xs = [kb.sb(ast, "xs%d" % i, [P, D], F32) for i in range(3)]
            junk = kb.sb(ast, "junk", [P, D], BF16)
            sm = [kb.sb(ast, "sm_%d" % i, [P, 64], F32) for i in range(2)]
            sm2 = kb.sb(ast, "sm2", [P, 64], F32)
            xb = kb.sb(ast, "xb", [P, D], BF16)
            xT = [kb.sb(ast, "xT%d" % i, [P, 8, P], BF16) for i in range(2)]
            qs = [kb.sb(ast, "qs%d" % i, [P, 512], F32) for i in range(2)]
            ks = [kb.sb(ast, "ks%d" % i, [P, 512], F32) for i in range(2)]
            rt = [kb.sb(ast, "rt%d" % i, [P, 8, 32], F32) for i in range(4)]
            qr = kb.sb(ast, "qr", [P, 512], BF16)
            kr = [kb.sb(ast, "kr%d" % i, [P, 512], BF16) for i in range(2)]
            qkTr = [kb.sb(ast, "qkTr%d" % i, [P, 16, P], BF16) for i in range(2)]
            vr = [kb.sb(ast, "vr%d" % i, [P, 512], BF16) for i in range(2)]
            gr = [kb.sb(ast, "gr%d" % i, [P, 512], F32) for i in range(2)]
            STt = kb.sb(ast, "STt", [P, 8, P], BF16)
            sqr = kb.sb(ast, "sqr", [P, 512], F32)
            rn = kb.sb(ast, "rn", [P, 512], F32)
            y = kb.sb(ast, "y", [P, D], BF16)
            yT = kb.sb(ast, "yT", [P, 8, P], BF16)
            cv = [kb.sb(ast, "cv%d" % i, [P, 8, 131], BF16) for i in range(2)]
            Dg = kb.sb(ast, "Dg", [P, 32, P], BF16)
            brow = kb.sb(ast, "brow", [1, 1024], BF16)
            onesr = kb.sb(ast, "onesr", [1, P], BF16)
            qka = [kb.sb(ast, "qka%d" % i, [P, 8, P], BF16) for i in range(2)]
            vm1 = [kb.sb(ast, "vm1_%d" % i, [P, 4, 129], BF16) for i in range(2)]
            go = [kb.sb(ast, "go%d" % i, [P, 512], F32) for i in range(2)]
            PTt = kb.sb(ast, "PTt", [P, 4, P], BF16)
            khat = [kb.sb(ast, "khat%d" % i, [P, 4, P], BF16) for i in range(2)]
            hg = kb.sb(ast, "hg", [P, 512], F32)
            Tst = kb.sb(ast, "Tst", [P, 4, 129], F32)
            Sbf = kb.sb(ast, "Sbf", [P, 4, 129], BF16)
            dec = [kb.sb(ast, "dec%d" % i, [P, 4], F32) for i in range(3)]
            Rst = kb.sb(ast, "Rst", [P, 8, 64], F32)
            Rbf = kb.sb(ast, "Rbf", [P, 8, 64], BF16)
            h1t2 = [kb.sb(ast, "h1t%d" % i, [P, D], F32) for i in range(2)]
            h1t = h1t2[0]
            xn2 = kb.sb(ast, "xn2", [P, D], BF16)
            xn2T = kb.sb(ast, "xn2T", [P, 8, P], BF16)
            cosT = kb.sb(ast, "cosT", [P, max(NT, PT), 32], F32)
            sinT = kb.sb(ast, "sinT", [P, max(NT, PT), 32], F32)
            posi = kb.sb(ast, "posi", [P, max(NT, PT)], I32)
            posf = kb.sb(ast, "posf", [P, max(NT, PT)], F32)
            rl = kb.sb(ast, "rl", [P, 64], F32)
            rl2 = kb.sb(ast, "rl2", [P, 64], F32)
            m8 = kb.sb(ast, "m8", [P, 8], F32)
            i8 = kb.sb(ast, "i8", [P, 8], U32)
            oh = kb.sb(ast, "oh", [P, 3, 32], F32)
            ohb = kb.sb(ast, "ohb", [P, 32], BF16)
            CNT = kb.sb(ast, "CNT", [P, 32], F32)
            pos_s = kb.sb(ast, "pos_s", [P, 32], F32)

            G(lambda: nc.gpsimd.memset(Tst[:, :, :], 0.0), [], [Tst])
            G(lambda: nc.gpsimd.memset(Sbf[:, :, :], 0.0), [], [Sbf])
            G(lambda: nc.gpsimd.memset(Rst[:, :, :], 0.0), [], [Rst])
            G(lambda: nc.gpsimd.memset(Rbf[:, :, :], 0.0), [], [Rbf])
            G(lambda: nc.gpsimd.memset(CNT[:, :], 0.0), [], [CNT])
            for i in range(2):
                G(lambda i=i: nc.gpsimd.memset(vm1[i][:, :, :], 1.0), [], [vm1[i]])
            for i in range(3):
                G(lambda i=i: nc.gpsimd.memset(dec[i][:, :], 1.0), [], [dec[i]])
            for i in range(2):
                G(lambda i=i: nc.gpsimd.memset(cv[i][:, :, :], 0.0), [], [cv[i]])

            for c in range(8):
                for jj in range(4):
                    V(lambda c=c, jj=jj: nc.vector.tensor_scalar(out=Dg[:, c * 4 + jj, :], in0=cst[:, C_ID:C_ID + P], scalar1=cvw[:, c * 4 + jj:c * 4 + jj + 1],
                                                                 scalar2=None, op0=ALU.mult), [cst, cvw], [Dg], 128)
            SP(lambda: nc.sync.dma_start(out=h1t2[1][0:1, :], in_=convb_row.t[:, :]), [convb_row], [h1t2[1]])
            V(lambda: nc.vector.tensor_copy(out=brow[:, :], in_=h1t2[1][0:1, :]), [h1t2[1]], [brow])
            V(lambda: nc.vector.memset(onesr[:, :], 1.0), [], [onesr])

            def make_trig(pos_res, n, ibuf=None):
                SP(lambda: nc.sync.dma_start(out=posi[:, 0:n], in_=pos_res.t[:, 0:n]), [pos_res], [posi])
                V(lambda: nc.vector.tensor_copy(out=posf[:, 0:n], in_=posi[:, 0:n]), [posi], [posf])
                for n0 in range(0, n, 16):
                    m_ = min(16, n - n0)
                    rA, rB, rI = h1t2[0], h1t2[1], sqr
                    aA = rA[:, 0:m_ * 32].rearrange("p (n j) -> p n j", j=32)
                    aB = rB[:, 0:m_ * 32].rearrange("p (n j) -> p n j", j=32)
                    aI = rI[:, 0:m_ * 32].bitcast(I32).rearrange("p (n j) -> p n j", j=32)
                    V(lambda: nc.vector.tensor_tensor(out=aA, in0=posf[:, n0:n0 + m_].unsqueeze(2).broadcast_to([P, m_, 32]),
                                                      in1=cst[:, C_FREQ:C_FREQ + 32].unsqueeze(1).broadcast_to([P, m_, 32]), op=ALU.mult),
                      [posf, cst], [rA])
                    for (shift, dstT) in ((0.0, sinT), (math.pi / 2, cosT)):
                        V(lambda shift=shift: nc.vector.tensor_scalar(out=aB, in0=aA, scalar1=shift, scalar2=1.0 / TWO_PI,
                                                                      op0=ALU.add, op1=ALU.mult), [rA], [rB])
                        V(lambda: nc.vector.tensor_copy(out=aI, in_=aB), [rB], [rI])
                        V(lambda: nc.vector.tensor_copy(out=aB, in_=aI), [rI], [rB])
                        V(lambda: nc.vector.scalar_tensor_tensor(out=aB, in0=aB, scalar=-TWO_PI, in1=aA,
                                                                 op0=ALU.mult, op1=ALU.add), [rB, rA], [rB])
                        V(lambda shift=shift: nc.vector.tensor_scalar(out=aB, in0=aB, scalar1=shift, scalar2=math.pi,
                                                                      op0=ALU.add, op1=ALU.min), [rB], [rB])
                        V(lambda: nc.vector.tensor_scalar(out=aB, in0=aB, scalar1=-math.pi, scalar2=None,
                                                          op0=ALU.max), [rB], [rB])
                        A(lambda dstT=dstT: nc.scalar.activation(out=dstT[:, n0:n0 + m_, :], in_=aB, func=AF.Sin), [rB], [dstT])

            def rotary(src, dst, n):
                sv = src[:, :].rearrange("p (h t j) -> p h t j", h=8, t=2)
                dv = dst[:, :].rearrange("p (h t j) -> p h t j", h=8, t=2)
                cb = cosT[:, n, :].unsqueeze(1).broadcast_to([P, 8, 32])
                sb_ = sinT[:, n, :].unsqueeze(1).broadcast_to([P, 8, 32])
                q1, q2 = sv[:, :, 0, :], sv[:, :, 1, :]
                G(lambda: nc.gpsimd.tensor_tensor(out=rt[0][:, :, :], in0=q1, in1=cb, op=ALU.mult), [src, cosT], [rt[0]])
                G(lambda: nc.gpsimd.tensor_tensor(out=rt[1][:, :, :], in0=q2, in1=sb_, op=ALU.mult), [src, sinT], [rt[1]])
                G(lambda: nc.gpsimd.tensor_tensor(out=rt[2][:, :, :], in0=q1, in1=sb_, op=ALU.mult), [src, sinT], [rt[2]])
                G(lambda: nc.gpsimd.tensor_tensor(out=rt[3][:, :, :], in0=q2, in1=cb, op=ALU.mult), [src, cosT], [rt[3]])
                G(lambda: nc.gpsimd.tensor_tensor(out=dv[:, :, 0, :], in0=rt[0][:, :, :], in1=rt[1][:, :, :], op=ALU.subtract), [rt[0], rt[1]], [dst])
                G(lambda: nc.gpsimd.tensor_tensor(out=dv[:, :, 1, :], in0=rt[2][:, :, :], in1=rt[3][:, :, :], op=ALU.add), [rt[2], rt[3]], [dst])

            def inproj(bank, c0, ncols, base=0):
                for k in range(8):
                    T(lambda k=k: nc.tensor.matmul(bank[:, base:base + ncols], lhsT=xT[:, k, :], rhs=Win[:, k, c0:c0 + ncols],
                                                   start=(k == 0), stop=(k == 7)), [xT, Win], [bank])

            def transpose8(src, dstT, nk=8):
                kb.lock(bkT)
                for k in range(nk):
                    T(lambda k=k: nc.tensor.transpose(out=bkT[:, k * P:(k + 1) * P], in_=src[:, k * P:(k + 1) * P], identity=identb[:, :]),
                      [src, identb], [bkT])
                A(lambda: nc.scalar.copy(out=dstT[:, 0:nk, :], in_=bkT[:, 0:nk * P].rearrange("p (k t) -> p k t", k=nk)), [bkT], [dstT])
                kb.unlock(bkT)

            def load_x(gi_):
                xsrc_, n_, _ = seq[gi_]
                xt_ = xs[gi_ % 3]
                SP(lambda: nc.sync.dma_start(out=xt_[:, :], in_=xsrc_.t[n_ * P:(n_ + 1) * P, :]), [xsrc_], [xt_])

            def proj(bank, c0, xTt):
                for k in range(8):
                    T(lambda k=k: nc.tensor.matmul(bank[:, :], lhsT=xTt[:, k, :], rhs=Win[:, k, c0:c0 + 512],
                                                   start=(k == 0), stop=(k == 7)), [xTt, Win], [bank], 512)

            def projT(bank, c0, xTt):
                for cc in range(4):
                    for k in range(8):
                        T(lambda k=k, cc=cc: nc.tensor.matmul(bank[:, cc * P:(cc + 1) * P], lhsT=Win[:, k, c0 + cc * P:c0 + (cc + 1) * P], rhs=xTt[:, k, :],
                                                              start=(k == 0), stop=(k == 7)), [xTt, Win], [bank])

            def pre(gi):
                xt = xs[gi % 3]
                smj = sm[gi % 2]
                A(lambda: nc.scalar.activation(out=junk[:, :], in_=xt[:, :], func=AF.Square, accum_out=smj[:, 0:1]), [xt], [junk, smj])
                A(lambda: nc.scalar.activation(out=smj[:, 1:2], in_=smj[:, 0:1], func=AF.Ln, bias=EPS, scale=1.0 / D), [smj], [smj], 1)
                A(lambda: nc.scalar.activation(out=smj[:, 2:3], in_=smj[:, 1:2], func=AF.Exp, scale=-0.5), [smj], [smj], 1)
                A(lambda: nc.scalar.activation(out=xb[:, :], in_=xt[:, :], func=AF.Copy, scale=smj[:, 2:3]), [xt, smj], [xb])
                transpose8(xb, xT[gi % 2])
                yield

            def front(gi):
                xsrc, n, main = seq[gi]
                j = gi % 2
                xt = xs[gi % 3]
                smj, qsj, ksj, vrj, grj, qkaj, krj, qkTj, vm1j, goj, khj = sm[j], qs[j], ks[j], vr[j], gr[j], qka[j], kr[j], qkTr[j], vm1[j], go[j], khat[j]
                qhalo = main or (gi + 1 < len(seq) and seq[gi + 1][2])
                xTt = xT[j]
                cvc, cvn = cv[j], cv[1 - j]
                c_lo = 0 if main else 4
                h_lo = 0 if qhalo else 4

                def conv_mm(bank, c0):
                    for cc in range(4):
                        c = c0 + cc
                        for jj in range(4):
                            T(lambda c=c, cc=cc, jj=jj: nc.tensor.matmul(bank[:, cc * P:(cc + 1) * P], lhsT=Dg[:, c * 4 + jj, :], rhs=cvc[:, c, jj:jj + 128],
                                                                         start=(jj == 0), stop=False), [Dg, cvc], [bank])
                        T(lambda c=c, cc=cc: nc.tensor.matmul(bank[:, cc * P:(cc + 1) * P], lhsT=brow[0:1, c * P:(c + 1) * P], rhs=onesr[0:1, :],
                                                              start=False, stop=True), [brow, onesr], [bank])

                projT(b1, 2560, xTt)
                A(lambda: nc.scalar.copy(out=cvc[:, 4:8, 3:131], in_=b1[:, :].rearrange("p (c t) -> p c t", c=4)), [b1], [cvc], 512)
                if qhalo:
                    projT(b0, 2048, xTt)
                    A(lambda: nc.scalar.copy(out=cvc[:, 0:4, 3:131], in_=b0[:, :].rearrange("p (c t) -> p c t", c=4)), [b0], [cvc], 512)
                G(lambda: nc.gpsimd.tensor_copy(out=cvn[:, h_lo:8, 0:3], in_=cvc[:, h_lo:8, 128:131]), [cvc], [cvn], 24)
                conv_mm(b1, 4)
                if main:
                    conv_mm(b0, 0)
                kb.lock_engine("act")
                if main:
                    A(lambda: nc.scalar.activation(out=qkaj[:, 0:4, :], in_=b0[:, :].rearrange("p (c t) -> p c t", c=4), func=AF.Silu), [b0], [qkaj], 512)
                    proj(b0, 1536, xTt)
                A(lambda: nc.scalar.activation(out=qkaj[:, 4:8, :], in_=b1[:, :].rearrange("p (c t) -> p c t", c=4), func=AF.Silu), [b1], [qkaj], 512)
                if main:
                    A(lambda: nc.scalar.activation(out=grj[:, :], in_=b0[:, :], func=AF.Silu), [b0], [grj], 512)
                kb.unlock_engine("act")
                yield
                for k in range(8):
                    T(lambda k=k: nc.tensor.matmul(ps_g[:, 0:8], lhsT=xTt[:, k, :], rhs=Win[:, k, 4096:4104], start=(k == 0), stop=(k == 7)), [xTt, Win], [ps_g], 8)
                V(lambda: nc.vector.tensor_tensor(out=smj[:, 8:16], in0=ps_g[:, 0:8], in1=bcs[:, 36:44], op=ALU.add), [ps_g, bcs], [smj], 8)
                A(lambda: nc.scalar.activation(out=smj[:, 16:20], in_=smj[:, 12:16], func=AF.Exp, scale=-1.0), [smj], [smj], 4)
                A(lambda: nc.scalar.activation(out=smj[:, 20:24], in_=smj[:, 16:20], func=AF.Ln, bias=1.0, scale=1.0), [smj], [smj], 4)
                T(lambda: nc.tensor.matmul(ps_cs[:, 8:12], lhsT=cst[:, C_MASK:C_MASK + P], rhs=smj[:, 20:24], start=True, stop=True), [cst, smj], [ps_cs], 16)
                T(lambda: nc.tensor.matmul(ps_cs[:, 12:16], lhsT=cst[:, C_ONES:C_ONES + P], rhs=smj[:, 20:24], start=True, stop=True), [cst, smj], [ps_cs], 16)
                V(lambda: nc.vector.tensor_tensor(out=smj[:, 24:28], in0=smj[:, 8:12], in1=ps_cs[:, 8:12], op=ALU.add), [smj, ps_cs], [smj], 4)
                A(lambda: nc.scalar.activation(out=smj[:, 28:32], in_=smj[:, 24:28], func=AF.Exp, bias=math.log(dscale), scale=1.0), [smj], [smj], 4)
                if main:
                    A(lambda: nc.scalar.activation(out=smj[:, 32:36], in_=ps_cs[:, 8:12], func=AF.Exp, scale=-1.0), [ps_cs], [smj], 4)
                dcur = dec[gi % 3]
                A(lambda: nc.scalar.activation(out=dcur[:, :], in_=ps_cs[:, 12:16], func=AF.Exp, scale=-1.0), [ps_cs], [dcur], 4)
                yield
                proj(b1, 512, xTt)
                V(lambda: nc.vector.tensor_tensor(out=ksj[:, :].rearrange("p (h e) -> p h e", h=8), in0=b1[:, :].rearrange("p (h e) -> p h e", h=8), in1=cst[:, C_GK:C_GK + 8].unsqueeze(2).broadcast_to([P, 8, 64]), op=ALU.mult), [b1, cst], [ksj], 512)
                rotary(ksj, krj, n)
                if main:
                    proj(b0, 0, xTt)
                    V(lambda: nc.vector.tensor_tensor(out=qsj[:, :].rearrange("p (h e) -> p h e", h=8), in0=b0[:, :].rearrange("p (h e) -> p h e", h=8), in1=cst[:, C_GQ:C_GQ + 8].unsqueeze(2).broadcast_to([P, 8, 64]), op=ALU.mult), [b0, cst], [qsj], 512)
                    rotary(qsj, qr, n)
                yield
                proj(b1, 1024, xTt)
                A(lambda: nc.scalar.copy(out=vrj[:, :], in_=b1[:, :]), [b1], [vrj], 512)
                proj(b0, 3072, xTt)
                A(lambda: nc.scalar.copy(out=vm1j[:, :, 0:128], in_=b0[:, :].rearrange("p (h e) -> p h e", h=4)), [b0], [vm1j], 512)
                if main:
                    proj(b1, 3584, xTt)
                    A(lambda: nc.scalar.activation(out=goj[:, :], in_=b1[:, :], func=AF.Exp, scale=-1.0), [b1], [goj], 512)
                    V(lambda: nc.vector.tensor_scalar(out=goj[:, :], in0=goj[:, :], scalar1=1.0, scalar2=None, op0=ALU.add), [goj], [goj], 512)
                    V(lambda: nc.vector.reciprocal(out=goj[:, :], in_=goj[:, :]), [goj], [goj], 512)
                yield
                if main:
                    b0h = b0[:, :].bitcast(BF16)
                    b1h = b1[:, :].bitcast(BF16)
                    for h in range(8):
                        T(lambda h=h: nc.tensor.transpose(out=b0h[0:64, h * P:(h + 1) * P], in_=qr[:, h * 64:(h + 1) * 64], identity=identb[:, :]), [qr, identb], [b0], 64)
                    for h in range(8):
                        T(lambda h=h: nc.tensor.transpose(out=b1h[0:64, h * P:(h + 1) * P], in_=krj[:, h * 64:(h + 1) * 64], identity=identb[:, :]), [krj, identb], [b1], 64)
                kb.lock(bkT)
                for h in range(4):
                    T(lambda h=h: nc.tensor.transpose(out=bkT[:, h * P:(h + 1) * P], in_=qkaj[:, 4 + h, :], identity=identb[:, :]), [qkaj, identb], [bkT])
                for h in range(4):
                    A(lambda h=h: nc.scalar.activation(out=khj[:, h, :], in_=bkT[:, h * P:(h + 1) * P], func=AF.Copy, scale=smj[:, 28 + h:29 + h]), [bkT, smj], [khj], 128)
                kb.unlock(bkT)
                if main:
                    A(lambda: nc.scalar.copy(out=qkTj[0:64, 0:8, :], in_=b0h[0:64, :].rearrange("p (k t) -> p k t", k=8)), [b0], [qkTj])
                    V(lambda: nc.vector.tensor_copy(out=qkTj[0:64, 8:16, :], in_=b1h[0:64, :].rearrange("p (k t) -> p k t", k=8)), [b1], [qkTj])

            def back(gi):
                xsrc, n, main = seq[gi]
                j = gi % 2
                xt = xs[gi % 3]
                h1t = h1t2[gi % 2]
                smj, vrj, grj, qkaj, krj, qkTj, vm1j, goj, khj = sm[j], vr[j], gr[j], qka[j], kr[j], qkTr[j], vm1[j], go[j], khat[j]
                dcur, dprev = dec[gi % 3], dec[(gi - 1) % 3]
                eu = lambda h: smj[:, 28 + h:29 + h]
                if main:
                    for h in range(8):
                        bank = b2 if h < 4 else b3
                        hh = h % 4
                        T(lambda h=h, bank=bank, hh=hh: nc.tensor.matmul(bank[:, hh * P:(hh + 1) * P], lhsT=qkTj[0:64, 8 + h, :],
                                                                         rhs=qkTj[0:64, h, :], start=True, stop=True), [qkTj], [bank])
                    for h in range(4):
                        T(lambda h=h: nc.tensor.matmul(b5[:, h * P:(h + 1) * P], lhsT=qkaj[:, 4 + h, :], rhs=qkaj[:, h, :], start=True, stop=True), [qkaj], [b5])
                    mb4 = cst[:, C_MASK:C_MASK + P].unsqueeze(1).broadcast_to([P, 4, P])
                    V(lambda: nc.vector.tensor_tensor(out=STt[:, 0:4, :], in0=b2[:, :].rearrange("p (h t) -> p h t", h=4), in1=mb4, op=ALU.mult), [b2, cst], [STt])
                    V(lambda: nc.vector.tensor_tensor(out=STt[:, 4:8, :], in0=b3[:, :].rearrange("p (h t) -> p h t", h=4), in1=mb4, op=ALU.mult), [b3, cst], [STt])
                    for h in range(4):
                        V(lambda h=h: nc.vector.scalar_tensor_tensor(out=PTt[:, h, :], in0=b5[:, h * P:(h + 1) * P], scalar=eu(h), in1=cst[:, C_MASK:C_MASK + P],
                                                                     op0=ALU.mult, op1=ALU.mult), [b5, smj, cst], [PTt], 128)
                yield
                for h in range(8):
                    T(lambda h=h: nc.tensor.matmul(b6[0:64, h * 64:(h + 1) * 64], lhsT=krj[:, h * 64:(h + 1) * 64],
                                                   rhs=vrj[:, h * 64:(h + 1) * 64], start=True, stop=True), [krj, vrj], [b6], 64)
                dsb = lambda h: (b2 if h < 2 else b3)
                for h in range(4):
                    o0 = (h % 2) * 129
                    T(lambda h=h, o0=o0: nc.tensor.matmul(dsb(h)[:, o0:o0 + 129], lhsT=khj[:, h, :], rhs=vm1j[:, h, :], start=True, stop=True), [khj, vm1j], [dsb(h)])
                V(lambda: nc.vector.tensor_tensor(out=Rst[0:64, :, :], in0=b6[0:64, :].rearrange("p (h e) -> p h e", h=8), in1=Rst[0:64, :, :], op=ALU.add), [b6, Rst], [Rst], 512)
                V(lambda: nc.vector.tensor_tensor(out=Rst[0:64, :, :], in0=Rst[0:64, :, :], in1=cst[0:64, C_GC:C_GC + 8].unsqueeze(2).broadcast_to([64, 8, 64]), op=ALU.mult), [Rst, cst], [Rst], 512)
                for h in range(4):
                    o0 = (h % 2) * 129
                    V(lambda h=h, o0=o0: nc.vector.scalar_tensor_tensor(out=Tst[:, h, :], in0=Tst[:, h, :], scalar=dprev[:, h:h + 1], in1=dsb(h)[:, o0:o0 + 129],
                                                                        op0=ALU.mult, op1=ALU.add), [Tst, dprev, dsb(h)], [Tst], 129)
                yield
                ob = lambda h: (b6 if h < 2 else b2)
                if main:
                    for h in range(8):
                        T(lambda h=h: nc.tensor.matmul(b5[:, h * 64:(h + 1) * 64], lhsT=STt[:, h, :], rhs=vrj[:, h * 64:(h + 1) * 64], start=True, stop=False),
                          [STt, vrj], [b5], 64)
                        T(lambda h=h: nc.tensor.matmul(b5[:, h * 64:(h + 1) * 64], lhsT=qkTj[0:64, h, :], rhs=Rbf[0:64, h, :],
                                                       start=False, stop=True), [qkTj, Rbf], [b5], 64)
                    for h in range(4):
                        o0 = (h % 2) * 129
                        T(lambda h=h, o0=o0: nc.tensor.matmul(ob(h)[:, o0:o0 + 129], lhsT=PTt[:, h, :], rhs=vm1j[:, h, :], start=True, stop=False), [PTt, vm1j], [ob(h)])
                        T(lambda h=h, o0=o0: nc.tensor.matmul(ob(h)[:, o0:o0 + 129], lhsT=qkaj[:, h, :], rhs=Sbf[:, h, :], start=False, stop=True), [qkaj, Sbf], [ob(h)])
                G(lambda: nc.gpsimd.tensor_copy(out=Rbf[0:64, :, :], in_=Rst[0:64, :, :]), [Rst], [Rbf], 512)
                for h in range(4):
                    A(lambda h=h: nc.scalar.activation(out=Sbf[:, h, :], in_=Tst[:, h, :], func=AF.Copy, scale=dcur[:, h:h + 1]), [Tst, dcur], [Sbf], 129)
                if not main:
                    return
                yield
                A(lambda: nc.scalar.activation(out=sqr[:, :], in_=b5[:, :], func=AF.Square), [b5], [sqr])
                V(lambda: nc.vector.reduce_sum(out=sm2[:, 0:8], in_=sqr[:, :].rearrange("p (h e) -> p h e", h=8), axis=AX.X), [sqr], [sm2], 512)
                A(lambda: nc.scalar.activation(out=sm2[:, 8:16], in_=sm2[:, 0:8], func=AF.Ln, bias=EPS, scale=1.0 / 64), [sm2], [sm2], 8)
                A(lambda: nc.scalar.activation(out=sm2[:, 16:24], in_=sm2[:, 8:16], func=AF.Exp, scale=-0.5), [sm2], [sm2], 8)
                V(lambda: nc.vector.tensor_tensor(out=rn[:, :].rearrange("p (h e) -> p h e", h=8), in0=b5[:, :].rearrange("p (h e) -> p h e", h=8),
                                                  in1=sm2[:, 16:24].unsqueeze(2).broadcast_to([P, 8, 64]), op=ALU.mult), [b5, sm2], [rn])
                G(lambda: nc.gpsimd.tensor_tensor(out=y[:, 0:512], in0=rn[:, :], in1=grj[:, :], op=ALU.mult), [rn, grj], [y], 512)
                yield
                if main:
                    for h in range(4):
                        o0 = (h % 2) * 129
                        V(lambda h=h, o0=o0: nc.vector.tensor_tensor(out=sm2[:, 24 + h:25 + h], in0=ob(h)[:, o0 + 128:o0 + 129], in1=smj[:, 32 + h:33 + h], op=ALU.mult),
                          [ob(h), smj], [sm2])
                    V(lambda: nc.vector.tensor_tensor(out=sm2[:, 28:32], in0=sm2[:, 24:28], in1=sm2[:, 24:28], op=ALU.mult), [sm2], [sm2])
                    V(lambda: nc.vector.tensor_scalar(out=sm2[:, 28:32], in0=sm2[:, 28:32], scalar1=1.0, scalar2=None, op0=ALU.max), [sm2], [sm2])
                    A(lambda: nc.scalar.activation(out=sm2[:, 32:36], in_=sm2[:, 28:32], func=AF.Ln), [sm2], [sm2])
                    A(lambda: nc.scalar.activation(out=sm2[:, 36:40], in_=sm2[:, 32:36], func=AF.Exp, scale=-0.5), [sm2], [sm2])
                    V(lambda: nc.vector.tensor_tensor(out=sm2[:, 40:44], in0=sm2[:, 36:40], in1=smj[:, 32:36], op=ALU.mult), [sm2, smj], [sm2])
                    for h in range(4):
                        o0 = (h % 2) * 129
                        V(lambda h=h, o0=o0: nc.vector.scalar_tensor_tensor(out=hg[:, h * P:(h + 1) * P], in0=ob(h)[:, o0:o0 + 128], scalar=sm2[:, 40 + h:41 + h],
                                                                            in1=goj[:, h * P:(h + 1) * P], op0=ALU.mult, op1=ALU.mult), [ob(h), sm2, goj], [hg])
                        A(lambda h=h: nc.scalar.activation(out=junk[:, h * P:(h + 1) * P], in_=hg[:, h * P:(h + 1) * P], func=AF.Square, accum_out=sm2[:, 44 + h:45 + h]),
                          [hg], [junk, sm2])
                    A(lambda: nc.scalar.activation(out=sm2[:, 48:52], in_=sm2[:, 44:48], func=AF.Ln, bias=EPS, scale=1.0 / 128), [sm2], [sm2])
                    A(lambda: nc.scalar.activation(out=sm2[:, 52:56], in_=sm2[:, 48:52], func=AF.Exp, scale=-0.5), [sm2], [sm2])
                    V(lambda: nc.vector.tensor_tensor(out=y[:, 512:1024].rearrange("p (h e) -> p h e", h=4), in0=hg[:, :].rearrange("p (h e) -> p h e", h=4),
                                                      in1=sm2[:, 52:56].unsqueeze(2).broadcast_to([P, 4, P]), op=ALU.mult), [hg, sm2], [y])
                yield
                transpose8(y, yT)
                for half in range(2):
                    bank = b3 if half == 0 else b5
                    for k in range(8):
                        T(lambda k=k, bank=bank, half=half: nc.tensor.matmul(bank[:, :], lhsT=yT[:, k, :], rhs=Wout[:, k, half * 512:(half + 1) * 512],
                                                                             start=(k == 0), stop=(k == 7)), [yT, Wout], [bank], 512)
                V(lambda: nc.vector.tensor_tensor(out=h1t[:, 0:512], in0=b3[:, :], in1=xt[:, 0:512], op=ALU.add), [b3, xt], [h1t], 512)
                V(lambda: nc.vector.tensor_tensor(out=h1t[:, 512:1024], in0=b5[:, :], in1=xt[:, 512:1024], op=ALU.add), [b5, xt], [h1t], 512)
                SP(lambda: nc.sync.dma_start(out=h1s.t[n * P:(n + 1) * P, :], in_=h1t[:, :]), [h1t], [h1s])

            def back2(gi):
                xsrc, n, main = seq[gi]
                h1t = h1t2[gi % 2]
                A(lambda: nc.scalar.activation(out=junk[:, :], in_=h1t[:, :], func=AF.Square, accum_out=sm2[:, 56:57]), [h1t], [junk, sm2])
                A(lambda: nc.scalar.activation(out=sm2[:, 57:58], in_=sm2[:, 56:57], func=AF.Ln, bias=EPS, scale=1.0 / D), [sm2], [sm2])
                A(lambda: nc.scalar.activation(out=sm2[:, 58:59], in_=sm2[:, 57:58], func=AF.Exp, scale=-0.5), [sm2], [sm2])
                V(lambda: nc.vector.scalar_tensor_tensor(out=xn2[:, :], in0=h1t[:, :], scalar=sm2[:, 58:59], in1=bmoe[:, :], op0=ALU.mult, op1=ALU.mult),
                  [h1t, sm2, bmoe], [xn2])
                transpose8(xn2, xn2T)
                for k in range(8):
                    T(lambda k=k: nc.tensor.matmul(ps_lg[:, 16:52], lhsT=xn2T[:, k, :], rhs=Wr[:, k, :], start=(k == 0), stop=(k == 7)), [xn2T, Wr], [ps_lg])
                yield
                V(lambda: nc.vector.tensor_tensor(out=rl[:, 0:36], in0=ps_lg[:, 16:52], in1=bcs[:, 0:36], op=ALU.add), [ps_lg, bcs], [rl])
                V(lambda: nc.vector.reduce_max(out=rl[:, 36:37], in_=rl[:, 0:4], axis=AX.X), [rl], [rl])
                V(lambda: nc.vector.tensor_scalar(out=rl[:, 37:38], in0=rl[:, 36:37], scalar1=-1.0, scalar2=None, op0=ALU.mult), [rl], [rl])
                A(lambda: nc.scalar.activation(out=rl[:, 44:48], in_=rl[:, 0:4], func=AF.Exp, bias=rl[:, 37:38], scale=1.0, accum_out=rl[:, 38:39]), [rl], [rl])
                V(lambda: nc.vector.reciprocal(out=rl[:, 39:40], in_=rl[:, 38:39]), [rl], [rl])
                V(lambda: nc.vector.tensor_scalar(out=rl[:, 40:44], in0=rl[:, 0:4], scalar1=rl[:, 36:37], scalar2=None, op0=ALU.is_ge), [rl], [rl])
                V(lambda: nc.vector.tensor_tensor(out=rl2[:, 0:32].rearrange("p (g e) -> p g e", g=4), in0=rl[:, 4:36].rearrange("p (g e) -> p g e", g=4),
                                                  in1=rl[:, 40:44].unsqueeze(2).broadcast_to([P, 4, 8]), op=ALU.mult), [rl], [rl2])
                V(lambda: nc.vector.reduce_sum(out=rl2[:, 32:40], in_=rl2[:, 0:32].rearrange("p (g e) -> p e g", g=4), axis=AX.X), [rl2], [rl2])
                V(lambda: nc.vector.max(out=m8[:, :], in_=rl2[:, 32:40]), [rl2], [m8])
                V(lambda: nc.vector.max_index(out=i8[:, :], in_max=m8[:, :], in_values=rl2[:, 32:40]), [m8, rl2], [i8])
                V(lambda: nc.vector.tensor_tensor(out=rl2[:, 40:44], in0=rl[:, 40:44], in1=cst[:, C_IOTA4:C_IOTA4 + 4], op=ALU.mult), [rl, cst], [rl2])
                V(lambda: nc.vector.reduce_sum(out=rl[:, 48:49], in_=rl2[:, 40:44], axis=AX.X), [rl2], [rl])
                V(lambda: nc.vector.tensor_tensor(out=rl[:, 49:50], in0=m8[:, 0:1], in1=m8[:, 1:2], op=ALU.subtract), [m8], [rl])
                A(lambda: nc.scalar.activation(out=rl[:, 50:51], in_=rl[:, 49:50], func=AF.Exp, scale=-1.0), [rl], [rl], 1)
                V(lambda: nc.vector.tensor_scalar(out=rl[:, 50:51], in0=rl[:, 50:51], scalar1=1.0, scalar2=None, op0=ALU.add), [rl], [rl], 1)
                V(lambda: nc.vector.reciprocal(out=rl[:, 50:51], in_=rl[:, 50:51]), [rl], [rl], 1)
                V(lambda: nc.vector.tensor_copy(out=rl2[:, 44:46], in_=i8[:, 0:2]), [i8], [rl2])
                V(lambda: nc.vector.scalar_tensor_tensor(out=rl[:, 51:53], in0=rl[:, 48:49].broadcast_to([P, 2]), scalar=8.0, in1=rl2[:, 44:46], op0=ALU.mult, op1=ALU.add),
                  [rl, rl2], [rl])
                yield
                for k_ in range(2):
                    V(lambda k_=k_: nc.vector.tensor_scalar(out=oh[:, k_, :], in0=cst[:, C_IOTA32:C_IOTA32 + 32], scalar1=rl[:, 51 + k_:52 + k_], scalar2=None, op0=ALU.is_equal),
                      [cst, rl], [oh])
                V(lambda: nc.vector.tensor_tensor(out=ohb[:, :], in0=oh[:, 0, :], in1=oh[:, 1, :], op=ALU.add), [oh], [ohb])
                T(lambda: nc.tensor.matmul(ps_pos[:, 64:96], lhsT=ustrb[:, :], rhs=ohb[:, :], start=True, stop=True), [ustrb, ohb], [ps_pos])
                T(lambda: nc.tensor.matmul(ps_tot[:, 96:128], lhsT=onesb[:, :], rhs=ohb[:, :], start=True, stop=True), [onesb, ohb], [ps_tot])
                V(lambda: nc.vector.tensor_tensor(out=pos_s[:, :], in0=ps_pos[:, 64:96], in1=CNT[:, :], op=ALU.add), [ps_pos, CNT], [pos_s])
                V(lambda: nc.vector.tensor_tensor(out=CNT[:, :], in0=ps_tot[:, 96:128], in1=CNT[:, :], op=ALU.add), [ps_tot, CNT], [CNT])
                for k_ in range(2):
                    V(lambda k_=k_: nc.vector.tensor_tensor(out=oh[:, 2, :], in0=oh[:, k_, :], in1=pos_s[:, :], op=ALU.mult), [oh, pos_s], [oh])
                    V(lambda k_=k_: nc.vector.reduce_sum(out=rl[:, 53 + k_:54 + k_], in_=oh[:, 2, :], axis=AX.X), [oh], [rl])
                V(lambda: nc.vector.tensor_scalar(out=rl[:, 55:57], in0=rl[:, 53:55], scalar1=float(CAP), scalar2=None, op0=ALU.is_lt), [rl], [rl])
                V(lambda: nc.vector.scalar_tensor_tensor(out=rl[:, 57:59], in0=rl[:, 51:53], scalar=float(CAP), in1=rl[:, 53:55], op0=ALU.mult, op1=ALU.add), [rl], [rl])
                V(lambda: nc.vector.tensor_scalar(out=rl2[:, 46:48], in0=rl[:, 55:57], scalar1=-4.0e6, scalar2=4.0e6, op0=ALU.mult, op1=ALU.add), [rl], [rl2])
                V(lambda: nc.vector.tensor_tensor(out=rl[:, 57:59], in0=rl[:, 57:59], in1=rl2[:, 46:48], op=ALU.add), [rl, rl2], [rl])
                V(lambda: nc.vector.tensor_copy(out=dest_i[:, n, :], in_=rl[:, 57:59]), [rl], [dest_i])
                V(lambda: nc.vector.tensor_tensor(out=rl2[:, 48:49], in0=rl[:, 39:40], in1=rl[:, 50:51], op=ALU.mult), [rl], [rl2])
                V(lambda: nc.vector.tensor_tensor(out=rl2[:, 49:50], in0=rl[:, 39:40], in1=rl2[:, 48:49], op=ALU.subtract), [rl, rl2], [rl2])
                V(lambda: nc.vector.tensor_tensor(out=gates[:, n, :], in0=rl2[:, 48:50], in1=rl[:, 55:57], op=ALU.mult), [rl2, rl], [gates])
                for k_ in range(2 if scatter else 0):
                    GD(lambda k_=k_: nc.gpsimd.indirect_dma_start(out=xbuf.t[:, :], out_offset=bass.IndirectOffsetOnAxis(ap=dest_i[:, n, k_:k_ + 1], axis=0),
                                                                   in_=xn2[:, :], in_offset=None, bounds_check=bc_reg, oob_is_err=False),
                       [xn2, dest_i, xbuf_z], [xbuf])

            def interleave(gens):
                gens = [g for g in gens if g is not None]
                while gens:
                    for g in list(gens):
                        try:
                            next(g)
                        except StopIteration:
                            gens.remove(g)

            def apply_carry():
                cm = cst[:, C_CARRY:C_CARRY + 1]
                V(lambda: nc.vector.tensor_scalar(out=Tst[:, :, :], in0=Tst[:, :, :], scalar1=cm, scalar2=None, op0=ALU.mult), [Tst, cst], [Tst])
                V(lambda: nc.vector.tensor_scalar(out=Sbf[:, :, :], in0=Sbf[:, :, :], scalar1=cm, scalar2=None, op0=ALU.mult), [Sbf, cst], [Sbf])
                V(lambda: nc.vector.tensor_scalar(out=Rst[:, :, :], in0=Rst[:, :, :], scalar1=cm, scalar2=None, op0=ALU.mult), [Rst, cst], [Rst])
                V(lambda: nc.vector.tensor_scalar(out=Rbf[:, :, :], in0=Rbf[:, :, :], scalar1=cm, scalar2=None, op0=ALU.mult), [Rbf, cst], [Rbf])
                for i in range(2):
                    V(lambda i=i: nc.vector.tensor_scalar(out=cv[i][:, :, 0:3], in0=cv[i][:, :, 0:3], scalar1=cm, scalar2=None, op0=ALU.mult), [cv[i], cst], [cv[i]])

            seq = [(x_pre, n, False) for n in range(PT)] + [(x_main, n, True) for n in range(NT)]
            NS = len(seq)
            load_x(0)
            if NS > 1:
                load_x(1)
            if PT > 0:
                make_trig(pos_pre, PT, xs[2])
                kb.merge([kb.record(pre(0))])
                pr1 = kb.record(pre(1)) if NS > 1 else []
                kb.merge([kb.record(front(0)), pr1])
                if NS > 2:
                    load_x(2)
                for gi in range(PT):
                    nxt = kb.record(front(gi + 1)) if gi + 1 < PT else []
                    prn = kb.record(pre(gi + 2)) if gi + 2 < NS else []
                    kb.merge([kb.record(back(gi)), nxt, prn])
                    if gi + 3 < NS:
                        load_x(gi + 3)
                    zfill()
                while zpending:
                    zfill()
                apply_carry()
                make_trig(pos_main, NT, h1t2[1])
                kb.merge([kb.record(front(PT))])
            else:
                while zpending:
                    zfill()
                make_trig(pos_main, NT, xs[2])
                kb.merge([kb.record(pre(0))])
                pr1 = kb.record(pre(1)) if NS > 1 else []
                kb.merge([kb.record(front(0)), pr1])
                if NS > 2:
                    load_x(2)
            for gi in range(PT, NS):
                nxt = kb.record(front(gi + 1)) if gi + 1 < NS else []
                prv = kb.record(back2(gi - 1)) if gi - 1 >= PT else []
                prn = kb.record(pre(gi + 2)) if gi + 2 < NS else []
                kb.merge([kb.record(back(gi)), prv, nxt, prn])
                if gi + 3 < NS:
                    load_x(gi + 3)
            kb.merge([kb.record(back2(NS - 1))])
            kb.barrier()
            if stop <= 3:
                return nc

        MODEL.update(MODEL_BC)
        with ExitStack() as bst:
            w1s = kb.sb(bst, "w1s", [P, 8, DEXP], F32)
            w3s = kb.sb(bst, "w3s", [P, 8, DEXP], F32)
            w2s = kb.sb(bst, "w2s", [P, 4, D], F32)
            w1b = [kb.sb(bst, "w1b%d" % i, [P, 8, DEXP], BF16) for i in range(2)]
            w3b = [kb.sb(bst, "w3b%d" % i, [P, 8, DEXP], BF16) for i in range(2)]
            w2b = [kb.sb(bst, "w2b%d" % i, [P, 4, D], BF16) for i in range(2)]
            xg = [kb.sb(bst, "xg%d" % i, [P, NB, D], BF16) for i in range(2)]
            xgT = [kb.sb(bst, "xgT%d" % i, [P, 8, CAP], BF16) for i in range(2)]
            sil = [kb.sb(bst, "sil%d" % i, [P, CAP], F32) for i in range(2)]
            hT = [kb.sb(bst, "hT%d" % i, [P, 4, CAP], BF16) for i in range(2)]
            yo = [kb.sb(bst, "yo%d" % i, [P, D], BF16) for i in range(3)]

            def load_expert(e):
                SP(lambda: nc.sync.dma_start(out=w1s[:, :, :], in_=w1.t[e, :, :].rearrange("(k p) n -> p k n", p=P)), [w1], [w1s])
                SP(lambda: nc.sync.dma_start(out=w3s[:, :, :], in_=w3.t[e, :, :].rearrange("(k p) n -> p k n", p=P)), [w3], [w3s])
                SP(lambda: nc.sync.dma_start(out=w2s[:, :, :], in_=w2.t[e, :, :].rearrange("(k p) n -> p k n", p=P)), [w2], [w2s])
                SP(lambda: nc.sync.dma_start(out=xg[e % 2][:, :, :], in_=xbuf.t[e * CAP:(e + 1) * CAP, :].rearrange("(a p) f -> p a f", p=P)), [xbuf], [xg[e % 2]])

            def head(e):
                j = e % 2
                for k in range(0, 8, 2):
                    G(lambda k=k: nc.gpsimd.tensor_copy(out=w1b[j][:, k:k + 2, :], in_=w1s[:, k:k + 2, :]), [w1s], [w1b[j]], 1024)
                for k in range(0, 8, 2):
                    A(lambda k=k: nc.scalar.copy(out=w3b[j][:, k:k + 2, :], in_=w3s[:, k:k + 2, :]), [w3s], [w3b[j]], 1024)
                for k in range(4):
                    V(lambda k=k: nc.vector.tensor_copy(out=w2b[j][:, k, :], in_=w2s[:, k, :]), [w2s], [w2b[j]], 1024)
                if e + 1 < NE:
                    load_expert(e + 1)
                yield
                for a in range(NB):
                    kb.lock(bkT)
                    for k in range(8):
                        T(lambda a=a, k=k: nc.tensor.transpose(out=bkT[:, k * P:(k + 1) * P], in_=xg[j][:, a, k * P:(k + 1) * P], identity=identb[:, :]), [xg[j], identb], [bkT])
                    if a % 2 == 0:
                        A(lambda a=a: nc.scalar.copy(out=xgT[j][:, :, a * P:(a + 1) * P], in_=bkT[:, :].rearrange("p (k t) -> p k t", k=8)), [bkT], [xgT[j]])
                    else:
                        V(lambda a=a: nc.vector.tensor_copy(out=xgT[j][:, :, a * P:(a + 1) * P], in_=bkT[:, :].rearrange("p (k t) -> p k t", k=8)), [bkT], [xgT[j]])
                    kb.unlock(bkT)
                yield
                for m in range(4):
                    pa, pb_ = (b0, b1) if m % 2 == 0 else (b2, b3)
                    for k in range(8):
                        T(lambda k=k, m=m, pa=pa: nc.tensor.matmul(pa[:, 0:CAP], lhsT=w1b[j][:, k, m * P:(m + 1) * P], rhs=xgT[j][:, k, :], start=(k == 0), stop=(k == 7)), [w1b[j], xgT[j]], [pa], CAP)
                    for k in range(8):
                        T(lambda k=k, m=m, pb_=pb_: nc.tensor.matmul(pb_[:, 0:CAP], lhsT=w3b[j][:, k, m * P:(m + 1) * P], rhs=xgT[j][:, k, :], start=(k == 0), stop=(k == 7)), [w3b[j], xgT[j]], [pb_], CAP)
                    s_ = sil[m % 2]
                    A(lambda pa=pa, s_=s_: nc.scalar.activation(out=s_[:, :], in_=pa[:, 0:CAP], func=AF.Silu), [pa], [s_], CAP)
                    V(lambda m=m, pb_=pb_, s_=s_: nc.vector.tensor_tensor(out=hT[j][:, m, :], in0=pb_[:, 0:CAP], in1=s_[:, :], op=ALU.mult), [pb_, s_], [hT[j]], CAP)
                    yield

            def tail(e):
                j = e % 2
                for a in range(NB):
                    yt = yo[(e * NB + a) % 3]
                    for half in range(2):
                        bank = b5 if half == 0 else b6
                        for m in range(4):
                            T(lambda a=a, m=m, half=half, bank=bank: nc.tensor.matmul(bank[:, :], lhsT=hT[j][:, m, a * P:(a + 1) * P], rhs=w2b[j][:, m, half * 512:(half + 1) * 512],
                                                                                      start=(m == 0), stop=(m == 3)), [hT[j], w2b[j]], [bank], 512)
                        if half == 0:
                            A(lambda yt=yt, bank=bank: nc.scalar.copy(out=yt[:, 0:512], in_=bank[:, :]), [bank], [yt], 512)
                        else:
                            V(lambda yt=yt, bank=bank: nc.vector.tensor_copy(out=yt[:, 512:1024], in_=bank[:, :]), [bank], [yt], 512)
                    r0 = e * CAP + a * P
                    SP(lambda yt=yt, r0=r0: nc.sync.dma_start(out=ybuf.t[r0:r0 + P, :], in_=yt[:, :]), [yt], [ybuf])
                    yield

            load_expert(0)
            kb.merge([kb.record(head(0))])
            for e in range(1, NE):
                kb.merge([kb.record(tail(e - 1)), kb.record(head(e))])
            kb.merge([kb.record(tail(NE - 1))])
            kb.barrier()
            if stop <= 4:
                return nc

        with ExitStack() as cstk:
            Wg = kb.sb(cstk, "Wg", [P, 8, D], BF16)
            Wu = kb.sb(cstk, "Wu", [P, 2, D], BF16)
            gkt2 = kb.sb(cstk, "gkt2", [P, 24], F32)
            bple = kb.sb(cstk, "bple", [P, D], F32)
            bfin = kb.sb(cstk, "bfin", [P, D], F32)
            SP(lambda: nc.sync.dma_start(out=gkt2[:, :], in_=gk[:, :]), [gk], [gkt2])
            SP(lambda: nc.sync.dma_start(out=bple[:, :], in_=bc_ple[:, :]), [bc_ple], [bple])
            SP(lambda: nc.sync.dma_start(out=bfin[:, :], in_=bc_fin[:, :]), [bc_fin], [bfin])
            with ExitStack() as lst:
                load_weight(lst, Wg, lambda c0, cw: w_gate.t[:, c0:c0 + cw].rearrange("(k p) n -> p k n", p=P), 8, D,
                            (lambda k: gkt2[:, 16 + k:17 + k], gkt2), "wg")
                load_weight(lst, Wu, lambda c0, cw: w_up.t[:, c0:c0 + cw].rearrange("(k p) n -> p k n", p=P), 2, D, None, "wu")
                kb.barrier()
            h1c = [kb.sb(cstk, "h1c%d" % i, [P, D], F32) for i in range(4)]
            Y1 = [kb.sb(cstk, "Y1_%d" % i, [P, D], BF16) for i in range(4)]
            Y2 = [kb.sb(cstk, "Y2_%d" % i, [P, D], BF16) for i in range(4)]
            pt = [kb.sb(cstk, "pt%d" % i, [P, PLE], F32) for i in range(4)]
            ptb = [kb.sb(cstk, "ptb%d" % i, [P, PLE], BF16) for i in range(2)]
            pT = [kb.sb(cstk, "pT%d" % i, [P, 2, P], BF16) for i in range(2)]
            h2 = [kb.sb(cstk, "h2_%d" % i, [P, D], F32) for i in range(2)]
            hb = [kb.sb(cstk, "hb%d" % i, [P, D], BF16) for i in range(2)]
            hbT = [kb.sb(cstk, "hbT%d" % i, [P, 8, P], BF16) for i in range(2)]
            gt = [kb.sb(cstk, "gt%d" % i, [P, D], F32) for i in range(2)]
            et = [kb.sb(cstk, "et%d" % i, [P, D], F32) for i in range(2)]
            junk2 = kb.sb(cstk, "junk2", [P, D], BF16)
            ot = [kb.sb(cstk, "ot%d" % i, [P, D], F32) for i in range(2)]
            sc = [kb.sb(cstk, "sc%d" % i, [P, 16], F32) for i in range(2)]
            for i in range(4):
                G(lambda i=i: nc.gpsimd.memset(Y1[i][:, :], 0.0), [], [Y1[i]])
                G(lambda i=i: nc.gpsimd.memset(Y2[i][:, :], 0.0), [], [Y2[i]])

            def load_C(n):
                j = n % 4
                SP(lambda: nc.sync.dma_start(out=h1c[j][:, :], in_=h1s.t[n * P:(n + 1) * P, :]), [h1s], [h1c[j]])
                SP(lambda: nc.sync.dma_start(out=pt[j][:, :], in_=p_main.t[n * P:(n + 1) * P, :]), [p_main], [pt[j]])
                for (k_, Yk) in ((0, Y1[j]), (1, Y2[j])):
                    GD(lambda k_=k_, Yk=Yk: nc.gpsimd.indirect_dma_start(out=Yk[:, :], out_offset=None, in_=ybuf.t[:, :],
                                                                          in_offset=bass.IndirectOffsetOnAxis(ap=dest_i[:, n, k_:k_ + 1], axis=0),
                                                                          bounds_check=bc_reg, oob_is_err=False), [ybuf, dest_i], [Yk])

            def tile_C(n):
                j = n % 2
                j4 = n % 4
                gb, pb0, pb1 = (b0, b1, b2) if j == 0 else (b3, b5, b6)
                h2j, hbj, hbTj, gtj, etj, scj, ptbj, pTj, o_ = h2[j], hb[j], hbT[j], gt[j], et[j], sc[j], ptb[j], pT[j], ot[j]
                V(lambda: nc.vector.scalar_tensor_tensor(out=h2j[:, :], in0=Y1[j4][:, :], scalar=gates[:, n, 0:1], in1=h1c[j4][:, :], op0=ALU.mult, op1=ALU.add),
                  [Y1[j4], gates, h1c[j4]], [h2j])
                V(lambda: nc.vector.scalar_tensor_tensor(out=h2j[:, :], in0=Y2[j4][:, :], scalar=gates[:, n, 1:2], in1=h2j[:, :], op0=ALU.mult, op1=ALU.add),
                  [Y2[j4], gates, h2j], [h2j])
                A(lambda: nc.scalar.activation(out=junk2[:, :], in_=h2j[:, :], func=AF.Square, accum_out=scj[:, 0:1]), [h2j], [junk2, scj])
                A(lambda: nc.scalar.activation(out=scj[:, 1:2], in_=scj[:, 0:1], func=AF.Ln, bias=EPS, scale=1.0 / D), [scj], [scj], 1)
                A(lambda: nc.scalar.activation(out=scj[:, 2:3], in_=scj[:, 1:2], func=AF.Exp, scale=-0.5), [scj], [scj], 1)
                A(lambda: nc.scalar.activation(out=hbj[:, :], in_=h2j[:, :], func=AF.Copy, scale=scj[:, 2:3]), [h2j, scj], [hbj])
                yield
                G(lambda: nc.gpsimd.tensor_copy(out=ptbj[:, :], in_=pt[j4][:, :]), [pt[j4]], [ptbj], 256)
                kb.lock(bkT)
                for k in range(2):
                    T(lambda k=k: nc.tensor.transpose(out=bkT[:, k * P:(k + 1) * P], in_=ptbj[:, k * P:(k + 1) * P], identity=identb[:, :]), [ptbj, identb], [bkT])
                A(lambda: nc.scalar.copy(out=pTj[:, :, :], in_=bkT[:, 0:2 * P].rearrange("p (k t) -> p k t", k=2)), [bkT], [pTj], 256)
                kb.unlock(bkT)
                for half in range(2):
                    bank = pb0 if half == 0 else pb1
                    for k in range(2):
                        T(lambda k=k, bank=bank, half=half: nc.tensor.matmul(bank[:, :], lhsT=pTj[:, k, :], rhs=Wu[:, k, half * 512:(half + 1) * 512], start=(k == 0), stop=(k == 1)),
                          [pTj, Wu], [bank], 512)
                    A(lambda bank=bank, half=half: nc.scalar.activation(out=junk2[:, half * 512:(half + 1) * 512], in_=bank[:, :], func=AF.Square, accum_out=scj[:, 4 + half:5 + half]),
                      [bank], [junk2, scj], 512)
                V(lambda: nc.vector.tensor_tensor(out=scj[:, 6:7], in0=scj[:, 4:5], in1=scj[:, 5:6], op=ALU.add), [scj], [scj], 1)
                A(lambda: nc.scalar.activation(out=scj[:, 7:8], in_=scj[:, 6:7], func=AF.Ln, bias=EPS, scale=1.0 / D), [scj], [scj], 1)
                A(lambda: nc.scalar.activation(out=scj[:, 8:9], in_=scj[:, 7:8], func=AF.Exp, scale=-0.5), [scj], [scj], 1)
                for half in range(2):
                    bank = pb0 if half == 0 else pb1
                    sl = slice(half * 512, (half + 1) * 512)
                    V(lambda bank=bank, sl=sl: nc.vector.scalar_tensor_tensor(out=etj[:, sl], in0=bank[:, :], scalar=scj[:, 8:9], in1=bple[:, sl], op0=ALU.mult, op1=ALU.mult),
                      [bank, scj, bple], [etj], 512)
                yield
                kb.mark()
                kb.lock(bkT)
                for k in range(8):
                    T(lambda k=k: nc.tensor.transpose(out=bkT[:, k * P:(k + 1) * P], in_=hbj[:, k * P:(k + 1) * P], identity=identb[:, :]), [hbj, identb], [bkT])
                V(lambda: nc.vector.tensor_copy(out=hbTj[:, :, :], in_=bkT[:, :].rearrange("p (k t) -> p k t", k=8)), [bkT], [hbTj])
                kb.unlock(bkT)
                gbs = (gb, pb0)
                for half in range(2):
                    for k in range(8):
                        T(lambda k=k, half=half: nc.tensor.matmul(gbs[half][:, :], lhsT=hbTj[:, k, :], rhs=Wg[:, k, half * 512:(half + 1) * 512], start=(k == 0), stop=(k == 7)),
                          [hbTj, Wg], [gbs[half]], 512)
                kb.lock_engine("act")
                for half in range(2):
                    A(lambda half=half: nc.scalar.activation(out=gtj[:, half * 512:(half + 1) * 512], in_=gbs[half][:, :], func=AF.Sigmoid), [gbs[half]], [gtj], 512)
                kb.unlock_engine("act")
                yield
                G(lambda: nc.gpsimd.tensor_tensor(out=etj[:, :], in0=etj[:, :], in1=gtj[:, :], op=ALU.mult), [etj, gtj], [etj])
                V(lambda: nc.vector.tensor_tensor(out=h2j[:, :], in0=h2j[:, :], in1=etj[:, :], op=ALU.add), [h2j, etj], [h2j])
                A(lambda: nc.scalar.activation(out=junk2[:, :], in_=h2j[:, :], func=AF.Square, accum_out=scj[:, 10:11]), [h2j], [junk2, scj])
                A(lambda: nc.scalar.activation(out=scj[:, 11:12], in_=scj[:, 10:11], func=AF.Ln, bias=EPS, scale=1.0 / D), [scj], [scj], 1)
                A(lambda: nc.scalar.activation(out=scj[:, 12:13], in_=scj[:, 11:12], func=AF.Exp, scale=-0.5), [scj], [scj], 1)
                V(lambda: nc.vector.scalar_tensor_tensor(out=o_[:, :], in0=h2j[:, :], scalar=scj[:, 12:13], in1=bfin[:, :], op0=ALU.mult, op1=ALU.mult),
                  [h2j, scj, bfin], [o_])
                SP(lambda: nc.sync.dma_start(out=out_d.t[n * P:(n + 1) * P, :], in_=o_[:, :]), [o_], [out_d])

            load_C(0)
            if NT > 1:
                load_C(1)
            prev2 = []
            for n in range(NT):
                if n + 2 < NT:
                    load_C(n + 2)
                st_ = kb.record(tile_C(n))
                mk = st_.index(("mark",))
                kb.merge([prev2, st_[:mk]])
                prev2 = st_[mk + 1:]
            kb.merge([prev2])
            kb.barrier()
        build_program.stats = (kb.n_inst, kb.n_wait)
    return nc


def make_consts(carry):
    c = np.zeros((P, CST_W), np.float32)
    idx = np.arange(P)
    c[:, C_ID:C_ID + P] = np.eye(P, dtype=np.float32)
    c[:, C_MASK:C_MASK + P] = (idx[None, :] >= idx[:, None]).astype(np.float32)
    c[:, C_USTR:C_USTR + P] = (idx[:, None] < idx[None, :]).astype(np.float32)
    c[:, C_ONES:C_ONES + P] = 1.0
    h = np.arange(8, dtype=np.float64)
    lg = np.log1p(-(2.0 ** (-5.0 - h)))
    t = idx.astype(np.float64)
    gq = np.exp(lg[None, :] * t[:, None])
    gkk = np.exp(-lg[None, :] * t[:, None]) * (64.0 ** -0.5)
    c[:, C_GQ:C_GQ + 8] = gq
    c[:, C_GK:C_GK + 8] = gkk
    gC = np.exp(lg * 128.0)
    c[:, C_GC:C_GC + 8] = gC[None, :]
    freqs = (10000.0 ** (-np.arange(32, dtype=np.float32) / 32)).astype(np.float32)
    c[:, C_FREQ:C_FREQ + 32] = freqs[None, :]
    c[:, C_IOTA32:C_IOTA32 + 32] = np.arange(32, dtype=np.float32)[None, :]
    c[:, C_IOTA4:C_IOTA4 + 4] = np.arange(4, dtype=np.float32)[None, :]
    c[:, C_CARRY] = carry
    return c


def arr_pk(v):
    return np.ascontiguousarray(v.reshape(-1, P).T)


def rep(v):
    return np.ascontiguousarray(np.broadcast_to(v.reshape(1, -1), (P, v.size))).astype(np.float32)


def shared_maps(inp):
    f = lambda a: np.ascontiguousarray(np.asarray(a, dtype=np.float32))
    m = {}
    m["w_in"] = f(inp["w_in"][0])
    m["w_out"] = f(inp["w_out"][0])
    m["w1"] = f(inp["w1"][0])
    m["w3"] = f(inp["w3"][0])
    m["w2"] = f(inp["w2"][0])
    m["w_up"] = f(inp["w_ple_up"][0])
    m["w_gate"] = f(inp["w_ple_gate"][0])
    m["wr"] = np.ascontiguousarray(np.concatenate([f(inp["w_group"][0]), f(inp["w_router"][0])], axis=1))
    gout = np.concatenate([f(inp["ret_gn"][0]), f(inp["ml_gn"][0])])
    m["gk"] = np.ascontiguousarray(np.concatenate([arr_pk(f(inp["attn_norm"][0])), arr_pk(gout), arr_pk(f(inp["ple_gate_norm"][0]))], axis=1))
    m["bc_moe"] = rep(f(inp["moe_norm"][0]))
    m["bc_ple"] = rep(f(inp["ple_norm"][0]))
    m["bc_fin"] = rep(f(inp["final_norm"]))
    m["bc_small"] = rep(np.concatenate([f(inp["b_group"][0]), f(inp["b_router"][0]), f(inp["b_igate"][0]), f(inp["b_fgate"][0])]))
    cw = f(inp["conv_w"][0])
    cb = f(inp["conv_b"][0])
    cwa = np.zeros((P, 8, 4), np.float32)
    for c in range(8):
        cwa[:, c, :] = cw[:, c * P:(c + 1) * P].T
    m["convw"] = np.ascontiguousarray(np.concatenate([cwa.reshape(P, 32), arr_pk(cb)], axis=1))
    m["convb_row"] = np.ascontiguousarray(cb.reshape(1, 1024))
    return m


_CACHE = {}


def kernel(**inputs):
    x = np.asarray(inputs["x"], dtype=np.float32)
    p = np.asarray(inputs["p"], dtype=np.float32)[0]
    pos = np.asarray(inputs["positions"]).astype(np.int32)
    B, S, _ = x.shape
    half = S // 2
    NT = half // P
    PT = NT
    key = (NT, PT)
    if key not in _CACHE:
        _CACHE[key] = build_program(NT, PT)
    nc = _CACHE[key]
    sh = shared_maps(inputs)
    zeros_src = np.zeros((1024, 512), np.float32)
    in_maps = []
    for core in range(8):
        b, s = core // 2, core % 2
        m = dict(sh)
        lo = s * half
        m["x_main"] = np.ascontiguousarray(x[b, lo:lo + half])
        m["pos_main"] = np.ascontiguousarray(pos[b, lo:lo + half].reshape(NT, P).T)
        m["p_main"] = np.ascontiguousarray(p[b, lo:lo + half])
        if s == 0:
            m["x_pre"] = np.zeros((half, D), np.float32)
            m["pos_pre"] = np.zeros((P, PT), np.int32)
        else:
            m["x_pre"] = np.ascontiguousarray(x[b, 0:half])
            m["pos_pre"] = np.ascontiguousarray(pos[b, 0:half].reshape(PT, P).T)
        m["cst"] = make_consts(float(s))
        m["zsrc"] = zeros_src
        in_maps.append(m)
    res = run_bass_kernel_spmd(nc, in_maps, core_ids=list(range(8)))
    out = np.empty((B, S, D), np.float32)
    for core in range(8):
        b, s = core // 2, core % 2
        out[b, s * half:(s + 1) * half] = res.results[core]["out"]
    return out
```

```python
import math
from contextlib import ExitStack
import numpy as np
import concourse.bass as bass
import concourse.mybir as mybir
from concourse.bass_utils import run_bass_kernel_spmd

F32 = mybir.dt.float32
BF16 = mybir.dt.bfloat16
I32 = mybir.dt.int32
U32 = mybir.dt.uint32
ALU = mybir.AluOpType
AF = mybir.ActivationFunctionType
AX = mybir.AxisListType

P = 128
D = 1024
NPROJ = 4104
NE = 32
DEXP = 512
PLE = 256
EPS = 1e-6
TWO_PI = 2.0 * math.pi

C_ID, C_MASK, C_USTR, C_ONES = 0, 128, 256, 384
C_GQ, C_GK = 512, 520
C_GC = 528
C_FREQ = 536
C_IOTA32 = 568
C_IOTA4 = 600
C_CARRY = 604
CST_W = 608


MODEL_A = {'pa': 30.0, 'pb': 0.3, 'lat': 0.0, 'ga': 100.0, 'gb': 2.0, 'va': 50.0, 'vb': 1.0}
MODEL_BC = {'pa': 64.0, 'pb': 0.45, 'lat': 60.0, 'ga': 300.0, 'gb': 3.0, 'va': 150.0, 'vb': 1.2}
MODEL = dict(MODEL_A)


class Res:
    __slots__ = ("name", "w", "r", "sem", "cnt", "t", "tw", "tr", "fsz")

    def __init__(self, name, t=None):
        self.name, self.t = name, t
        self.w, self.r = {}, {}
        self.sem, self.cnt = None, 0
        self.tw = self.tr = 0.0
        try:
            sh = list(t.shape)
            f = 1
            for d_ in sh[1:]:
                f *= d_
            self.fsz = f
        except Exception:
            self.fsz = 512

    def __getitem__(self, key):
        return self.t[key]


class KB:
    SAME_ENGINE_SYNC = True

    def __init__(self, nc, stack):
        self.nc, self.stack = nc, stack
        self.eng = {"pe": nc.tensor, "act": nc.scalar, "dve": nc.vector, "pool": nc.gpsimd, "sp": nc.sync}
        self.sem, self.cnt, self.waited = {}, {}, {}
        for e in self.eng:
            self.sem[e] = stack.enter_context(nc.semaphore("s_" + e))
            self.cnt[e] = 0
            self.waited[e] = {}
        self.all_res = []
        self.n_inst = self.n_wait = 0
        self.rec = None
        self.tE = {e: 0.0 for e in self.eng}

    def sb(self, st, name, shape, dt):
        r = Res(name, st.enter_context(self.nc.sbuf_tensor(name, list(shape), dt)))
        self.all_res.append(r)
        return r

    def ps(self, st, name, shape, dt):
        r = Res(name, st.enter_context(self.nc.psum_tensor(name, list(shape), dt)))
        self.all_res.append(r)
        return r

    def view(self, name, t):
        r = Res(name, t)
        self.all_res.append(r)
        return r

    def _waits(self, e, reads, writes):
        need = {}
        for r in reads:
            for k, sv in r.w.items():
                if need.get(k, (None, 0))[1] < sv[1]:
                    need[k] = sv
        for w in writes:
            for d in (w.w, w.r):
                for k, sv in d.items():
                    if need.get(k, (None, 0))[1] < sv[1]:
                        need[k] = sv
        wd = self.waited[e]
        for k, (s, v) in need.items():
            if k == e and (e == "pe" or not self.SAME_ENGINE_SYNC):
                continue
            if wd.get(k, 0) >= v:
                continue
            self.eng[e].wait_ge(s, v)
            self.n_wait += 1
            wd[k] = v

    def _dur(self, kind, e, writes, n):
        if kind == "dma":
            return 100.0
        if e == "pe":
            return MODEL['pa'] + MODEL['pb'] * (n if n is not None else 128)
        f = min(writes[0].fsz, 1024) if n is None else n
        if e == "pool":
            return MODEL['ga'] + MODEL['gb'] * f
        return MODEL['va'] + MODEL['vb'] * f

    def _model(self, kind, e, reads, writes, n, commit):
        t = self.tE[e]
        for r in reads:
            if r.tw > t:
                t = r.tw
        for w in writes:
            if w.tw > t:
                t = w.tw
            if w.tr > t:
                t = w.tr
        if commit:
            d = self._dur(kind, e, writes, n)
            self.tE[e] = t + d
            end = t + (2500.0 if kind == "dma" else d + MODEL['lat'])
            for r in reads:
                if end > r.tr:
                    r.tr = end
            for w in writes:
                w.tw = end
        return t

    def lock(self, res):
        if self.rec is not None:
            self.rec.append(("lock", res))

    def unlock(self, res):
        if self.rec is not None:
            self.rec.append(("unlock", res))

    def mark(self):
        if self.rec is not None:
            self.rec.append(("mark",))

    def lock_engine(self, e):
        if self.rec is not None:
            self.rec.append(("elock", e))

    def unlock_engine(self, e):
        if self.rec is not None:
            self.rec.append(("eunlock", e))

    def record(self, gen):
        assert self.rec is None
        self.rec = []
        for _ in gen:
            pass
        out, self.rec = self.rec, None
        return out

    def merge(self, streams):
        streams = [st for st in streams if st]
        idx = [0] * len(streams)
        locks = {}
        elocks = {}
        while True:
            best = None
            for i, st in enumerate(streams):
                if idx[i] >= len(st):
                    continue
                it = st[idx[i]]
                if it[0] == "lock":
                    if locks.get(it[1].name, i) != i:
                        continue
                    t = -2.0
                elif it[0] == "unlock" or it[0] == "eunlock":
                    t = -3.0
                elif it[0] == "elock":
                    if elocks.get(it[1], i) != i:
                        continue
                    t = -2.0
                else:
                    kind, e, fn, reads, writes, n = it
                    blocked = elocks.get(e, i) != i
                    for r in reads + writes:
                        if locks.get(r.name, i) != i:
                            blocked = True
                            break
                    if blocked:
                        continue
                    t = self._model(kind, e, reads, writes, n, False)
                if best is None or t < best[0]:
                    best = (t, i)
            if best is None:
                assert all(idx[i] >= len(st) for i, st in enumerate(streams)), "merge deadlock"
                return
            i = best[1]
            it = streams[i][idx[i]]
            idx[i] += 1
            if it[0] == "lock":
                locks[it[1].name] = i
            elif it[0] == "unlock":
                locks.pop(it[1].name, None)
            elif it[0] == "elock":
                elocks[it[1]] = i
            elif it[0] == "eunlock":
                elocks.pop(it[1], None)
            elif it[0] == "op":
                self.op(it[1], it[2], it[3], it[4], it[5])
            else:
                self.dma(it[1], it[2], it[3], it[4])

    def op(self, e, fn, reads=(), writes=(), n=None):
        if self.rec is not None:
            self.rec.append(("op", e, fn, tuple(reads), tuple(writes), n))
            return None
        self._model("op", e, reads, writes, n, True)
        self._waits(e, reads, writes)
        ins = fn()
        self.cnt[e] += 1
        tok = (self.sem[e], self.cnt[e])
        ins.then_inc(self.sem[e], 1)
        self.n_inst += 1
        for r in reads:
            r.r[e] = tok
        for w in writes:
            w.w[e] = tok
        return ins

    def dma(self, e, fn, reads=(), writes=()):
        if self.rec is not None:
            self.rec.append(("dma", e, fn, tuple(reads), tuple(writes), None))
            return None
        self._model("dma", e, reads, writes, None, True)
        self._waits(e, reads, writes)
        tgt = writes[0]
        if tgt.sem is None:
            tgt.sem = self.stack.enter_context(self.nc.semaphore("d_" + tgt.name))
        ins = fn()
        tgt.cnt += 16
        ins.then_inc(tgt.sem, 16)
        key = "dma:" + tgt.name
        tok = (tgt.sem, tgt.cnt)
        self.n_inst += 1
        for r in reads:
            r.r[key] = tok
        for w in writes:
            w.w[key] = tok
        return ins

    def barrier(self):
        for e in self.eng:
            wd = self.waited[e]
            for e2 in self.eng:
                if self.cnt[e2] == 0 or (e2 == e and (e == "pe" or not self.SAME_ENGINE_SYNC)):
                    continue
                if wd.get(e2, 0) < self.cnt[e2]:
                    self.eng[e].wait_ge(self.sem[e2], self.cnt[e2])
                    wd[e2] = self.cnt[e2]
            for r in self.all_res:
                if r.sem is not None and r.cnt > 0:
                    k = "dma:" + r.name
                    if wd.get(k, 0) < r.cnt:
                        self.eng[e].wait_ge(r.sem, r.cnt)
                        wd[k] = r.cnt


def build_program(NT, PT, CAP=512, stop=9, scatter=True, sub=99):
    nc = bass.Bass("TRN2", target_bir_lowering=False)
    NB = CAP // P
    NROWS = NE * CAP
    dscale = 128.0 ** -0.5

    def din(name, shape, dt=F32):
        return Res(name, nc.dram_tensor(name, list(shape), dt, kind="ExternalInput"))

    x_main = din("x_main", [NT * P, D])
    x_pre = din("x_pre", [max(PT, 1) * P, D])
    pos_main = din("pos_main", [P, NT], I32)
    pos_pre = din("pos_pre", [P, max(PT, 1)], I32)
    p_main = din("p_main", [NT * P, PLE])
    w_in = din("w_in", [D, NPROJ])
    w_out = din("w_out", [D, D])
    w1 = din("w1", [NE, D, DEXP])
    w3 = din("w3", [NE, D, DEXP])
    w2 = din("w2", [NE, DEXP, D])
    w_up = din("w_up", [PLE, D])
    w_gate = din("w_gate", [D, D])
    wr = din("wr", [D, 36])
    gk = din("gk", [P, 24])
    bc_moe = din("bc_moe", [P, D])
    bc_ple = din("bc_ple", [P, D])
    bc_fin = din("bc_fin", [P, D])
    bc_small = din("bc_small", [P, 44])
    convw = din("convw", [P, 40])
    cst_d = din("cst", [P, CST_W])
    zsrc = din("zsrc", [1024, 512])
    convb_row = din("convb_row", [1, 1024])
    out_d = Res("out", nc.dram_tensor("out", [NT * P, D], F32, kind="ExternalOutput"))
    h1s = Res("h1s", nc.dram_tensor("h1s", [NT * P, D], F32, kind="Internal"))
    xbuf = Res("xbuf", nc.dram_tensor("xbuf", [NROWS, D], BF16, kind="Internal"))
    ybuf = Res("ybuf", nc.dram_tensor("ybuf", [NROWS, D], BF16, kind="Internal"))

    with ExitStack() as gst:
        kb = KB(nc, gst)
        kb.all_res += [out_d, h1s, xbuf, ybuf]
        V = lambda fn, r=(), w=(), n=None: kb.op("dve", fn, r, w, n)
        A = lambda fn, r=(), w=(), n=None: kb.op("act", fn, r, w, n)
        G = lambda fn, r=(), w=(), n=None: kb.op("pool", fn, r, w, n)
        T = lambda fn, r=(), w=(), n=None: kb.op("pe", fn, r, w, n)
        SP = lambda fn, r=(), w=(): kb.dma("sp", fn, r, w)
        GD = lambda fn, r=(), w=(): kb.dma("pool", fn, r, w)

        bc_reg = nc.gpsimd.to_reg(NROWS - 1)
        cst = kb.sb(gst, "cstt", [P, CST_W], F32)
        identb = kb.sb(gst, "identb", [P, P], BF16)
        maskb = kb.sb(gst, "maskb", [P, P], BF16)
        ustrb = kb.sb(gst, "ustrb", [P, P], BF16)
        onesb = kb.sb(gst, "onesb", [P, P], BF16)
        dest_i = kb.sb(gst, "dest_i", [P, NT, 2], I32)
        gates = kb.sb(gst, "gates", [P, NT, 2], F32)
        bcs = kb.sb(gst, "bcs", [P, 44], F32)
        bk = [kb.ps(gst, "bk%d" % i, [P, 512], F32) for i in range(4)]
        bkT = kb.ps(gst, "bkT", [P, 1024], BF16)
        bk += [kb.ps(gst, "bk%d" % i, [P, 512], F32) for i in (5, 6)]
        bk7t = gst.enter_context(nc.psum_tensor("bk7", [P, 512], F32))
        ps_g = kb.view("bk7", bk7t)
        ps_cs = ps_lg = ps_pos = ps_tot = ps_g
        b0, b1, b2, b3, b5, b6 = bk

        SP(lambda: nc.sync.dma_start(out=cst[:, :], in_=cst_d[:, :]), [cst_d], [cst])
        xbuf_z = kb.view("xbuf_z", xbuf.t)
        zpending = list(range(NROWS // 1024))

        def zfill():
            if zpending:
                zi = zpending.pop(0)
                SP(lambda: nc.sync.dma_start(out=xbuf.t[zi * 1024:(zi + 1) * 1024, :], in_=zsrc.t[:, :].bitcast(BF16)), [zsrc], [xbuf_z])
        SP(lambda: nc.sync.dma_start(out=bcs[:, :], in_=bc_small[:, :]), [bc_small], [bcs])
        G(lambda: nc.gpsimd.tensor_copy(out=identb[:, :], in_=cst[:, C_ID:C_ID + P]), [cst], [identb])
        G(lambda: nc.gpsimd.tensor_copy(out=ustrb[:, :], in_=cst[:, C_USTR:C_USTR + P]), [cst], [ustrb])
        G(lambda: nc.gpsimd.tensor_copy(out=onesb[:, :], in_=cst[:, C_ONES:C_ONES + P]), [cst], [onesb])
        G(lambda: nc.gpsimd.memset(dest_i[:, :, :], 0), [], [dest_i])
        G(lambda: nc.gpsimd.memset(gates[:, :, :], 0.0), [], [gates])
        ident_f = cst
        mask_ap = lambda: cst[:, C_MASK:C_MASK + P]

        def rsqrt_chain(dst_ap, src_ap, n, res_list_r, res_list_w, tmp):
            A(lambda: nc.scalar.activation(out=tmp_ap(tmp, src_ap), in_=src_ap, func=AF.Ln, bias=EPS, scale=1.0 / n),
              res_list_r, [tmp])
            A(lambda: nc.scalar.activation(out=dst_ap, in_=tmp_ap(tmp, src_ap), func=AF.Exp, scale=-0.5),
              [tmp], res_list_w)

        def tmp_ap(tmp, like):
            w = like.shape[-1] if len(like.shape) == 2 else None
            return tmp[:, 0:w]

        def load_weight(st, dst, src_ap_fn, nk, ncols, gscale, qname, blk=512):
            stg = [kb.sb(st, qname + "_stg%d" % i, [P, nk, blk], F32) for i in range(2)]
            i = 0
            for c0 in range(0, ncols, blk):
                cw = min(blk, ncols - c0)
                s_ = stg[i % 2]
                SP(lambda s_=s_, c0=c0, cw=cw: nc.sync.dma_start(out=s_[:, :, 0:cw], in_=src_ap_fn(c0, cw)), [], [s_])
                for k in range(nk):
                    eng = ("dve", "act")[(i * nk + k) % 2]
                    o_ = dst[:, k, c0:c0 + cw]
                    i_ = s_[:, k, 0:cw]
                    if gscale is None:
                        if eng == "act":
                            A(lambda o_=o_, i_=i_: nc.scalar.copy(out=o_, in_=i_), [s_], [dst])
                        elif eng == "dve":
                            V(lambda o_=o_, i_=i_: nc.vector.tensor_copy(out=o_, in_=i_), [s_], [dst])
                        else:
                            G(lambda o_=o_, i_=i_: nc.gpsimd.tensor_copy(out=o_, in_=i_), [s_], [dst])
                    else:
                        gs, gres = gscale
                        sc = gs(k)
                        if eng == "act":
                            A(lambda o_=o_, i_=i_, sc=sc: nc.scalar.activation(out=o_, in_=i_, func=AF.Copy, scale=sc), [s_, gres], [dst])
                        elif eng == "dve":
                            V(lambda o_=o_, i_=i_, sc=sc: nc.vector.tensor_scalar(out=o_, in0=i_, scalar1=sc, scalar2=None, op0=ALU.mult), [s_, gres], [dst])
                        else:
                            G(lambda o_=o_, i_=i_, sc=sc: nc.gpsimd.tensor_scalar(out=o_, in0=i_, scalar1=sc, scalar2=None, op0=ALU.mult), [s_, gres], [dst])
                i += 1

        MODEL.update(MODEL_A)
        with ExitStack() as ast:
            Win = kb.sb(ast, "Win", [P, 8, NPROJ], BF16)
            Wout = kb.sb(ast, "Wout", [P, 8, D], BF16)
            Wr = kb.sb(ast, "Wr", [P, 8, 36], BF16)
            gkt = kb.sb(ast, "gkt", [P, 24], F32)
            cvw = kb.sb(ast, "cvw", [P, 40], F32)
            bmoe = kb.sb(ast, "bmoe", [P, D], F32)
            SP(lambda: nc.sync.dma_start(out=gkt[:, :], in_=gk[:, :]), [gk], [gkt])
            SP(lambda: nc.sync.dma_start(out=cvw[:, :], in_=convw[:, :]), [convw], [cvw])
            SP(lambda: nc.sync.dma_start(out=bmoe[:, :], in_=bc_moe[:, :]), [bc_moe], [bmoe])
            with ExitStack() as lst:
                load_weight(lst, Win, lambda c0, cw: w_in.t[:, c0:c0 + cw].rearrange("(k p) n -> p k n", p=P), 8, NPROJ,
                            (lambda k: gkt[:, k:k + 1], gkt), "win")
                load_weight(lst, Wout, lambda c0, cw: w_out.t[:, c0:c0 + cw].rearrange("(k p) n -> p k n", p=P), 8, D,
                            (lambda k: gkt[:, 8 + k:9 + k], gkt), "wout")
                wrs = kb.sb(lst, "wrs", [P, 8, 36], F32)
                SP(lambda: nc.sync.dma_start(out=wrs[:, :, :], in_=wr.t[:, :].rearrange("(k p) n -> p k n", p=P)), [wr], [wrs])
                V(lambda: nc.vector.tensor_copy(out=Wr[:, :, :], in_=wrs[:, :, :]), [wrs], [Wr])
                kb.barrier()
                if stop <= 1:
                    return nc

            xs = [kb.sb(ast, "xs%d" % i, [P, D], F32) for i in range(3)]
            junk = kb.sb(ast, "junk", [P, D], BF16)
            sm = [kb.sb(ast, "sm_%d" % i, [P, 64], F32) for i in range(2)]
            sm2 = kb.sb(ast, "sm2", [P, 64], F32)
            xb = kb.sb(ast, "xb", [P, D], BF16)
            xT = [kb.sb(ast, "xT%d" % i, [P, 8, P], BF16) for i in range(2)]
            qs = [kb.sb(ast, "qs%d" % i, [P, 512], F32) for i in range(2)]
            ks = [kb.sb(ast, "ks%d" % i, [P, 512], F32) for i in range(2)]
            rt = [kb.sb(ast, "rt%d" % i, [P, 8, 32], F32) for i in range(4)]
            qr = kb.sb(ast, "qr", [P, 512], BF16)
            kr = [kb.sb(ast, "kr%d" % i, [P, 512], BF16) for i in range(2)]
            qkTr = [kb.sb(ast, "qkTr%d" % i, [P, 16, P], BF16) for i in range(2)]
            vr = [kb.sb(ast, "vr%d" % i, [P, 512], BF16) for i in range(2)]
            gr = [kb.sb(ast, "gr%d" % i, [P, 512], F32) for i in range(2)]
            STt = kb.sb(ast, "STt", [P, 8, P], BF16)
            sqr = kb.sb(ast, "sqr", [P, 512], F32)
            rn = kb.sb(ast, "rn", [P, 512], F32)
            y = kb.sb(ast, "y", [P, D], BF16)
            yT = kb.sb(ast, "yT", [P, 8, P], BF16)
            cv = [kb.sb(ast, "cv%d" % i, [P, 8, 131], BF16) for i in range(2)]
            Dg = kb.sb(ast, "Dg", [P, 32, P], BF16)
            brow = kb.sb(ast, "brow", [1, 1024], BF16)
            onesr = kb.sb(ast, "onesr", [1, P], BF16)
            qka = [kb.sb(ast, "qka%d" % i, [P, 8, P], BF16) for i in range(2)]
            vm1 = [kb.sb(ast, "vm1_%d" % i, [P, 4, 129], BF16) for i in range(2)]
            go = [kb.sb(ast, "go%d" % i, [P, 512], F32) for i in range(2)]
            PTt = kb.sb(ast, "PTt", [P, 4, P], BF16)
            khat = [kb.sb(ast, "khat%d" % i, [P, 4, P], BF16) for i in range(2)]
            hg = kb.sb(ast, "hg", [P, 512], F32)
            Tst = kb.sb(ast, "Tst", [P, 4, 129], F32)
            Sbf = kb.sb(ast, "Sbf", [P, 4, 129], BF16)
            dec = [kb.sb(ast, "dec%d" % i, [P, 4], F32) for i in range(3)]
            Rst = kb.sb(ast, "Rst", [P, 8, 64], F32)
            Rbf = kb.sb(ast, "Rbf", [P, 8, 64], BF16)
            h1t2 = [kb.sb(ast, "h1t%d" % i, [P, D], F32) for i in range(2)]
            h1t = h1t2[0]
            xn2 = kb.sb(ast, "xn2", [P, D], BF16)
            xn2T = kb.sb(ast, "xn2T", [P, 8, P], BF16)
            cosT = kb.sb(ast, "cosT", [P, max(NT, PT), 32], F32)
            sinT = kb.sb(ast, "sinT", [P, max(NT, PT), 32], F32)
            posi = kb.sb(ast, "posi", [P, max(NT, PT)], I32)
            posf = kb.sb(ast, "posf", [P, max(NT, PT)], F32)
            rl = kb.sb(ast, "rl", [P, 64], F32)
            rl2 = kb.sb(ast, "rl2", [P, 64], F32)
            m8 = kb.sb(ast, "m8", [P, 8], F32)
            i8 = kb.sb(ast, "i8", [P, 8], U32)
            oh = kb.sb(ast, "oh", [P, 3, 32], F32)
            ohb = kb.sb(ast, "ohb", [P, 32], BF16)
            CNT = kb.sb(ast, "CNT", [P, 32], F32)
            pos_s = kb.sb(ast, "pos_s", [P, 32], F32)

            G(lambda: nc.gpsimd.memset(Tst[:, :, :], 0.0), [], [Tst])
            G(lambda: nc.gpsimd.memset(Sbf[:, :, :], 0.0), [], [Sbf])
            G(lambda: nc.gpsimd.memset(Rst[:, :, :], 0.0), [], [Rst])
            G(lambda: nc.gpsimd.memset(Rbf[:, :, :], 0.0), [], [Rbf])
            G(lambda: nc.gpsimd.memset(CNT[:, :], 0.0), [], [CNT])
            for i in range(2):
                G(lambda i=i: nc.gpsimd.memset(vm1[i][:, :, :], 1.0), [], [vm1[i]])
            for i in range(3):
                G(lambda i=i: nc.gpsimd.memset(dec[i][:, :], 1.0), [], [dec[i]])
            for i in range(2):
                G(lambda i=i: nc.gpsimd.memset(cv[i][:, :, :], 0.0), [], [cv[i]])

            for c in range(8):
                for jj in range(4):
                    V(lambda c=c, jj=jj: nc.vector.tensor_scalar(out=Dg[:, c * 4 + jj, :], in0=cst[:, C_ID:C_ID + P], scalar1=cvw[:, c * 4 + jj:c * 4 + jj + 1],
                                                                 scalar2=None, op0=ALU.mult), [cst, cvw], [Dg], 128)
            SP(lambda: nc.sync.dma_start(out=h1t2[1][0:1, :], in_=convb_row.t[:, :]), [convb_row], [h1t2[1]])
            V(lambda: nc.vector.tensor_copy(out=brow[:, :], in_=h1t2[1][0:1, :]), [h1t2[1]], [brow])
            V(lambda: nc.vector.memset(onesr[:, :], 1.0), [], [onesr])

            def make_trig(pos_res, n, ibuf=None):
                SP(lambda: nc.sync.dma_start(out=posi[:, 0:n], in_=pos_res.t[:, 0:n]), [pos_res], [posi])
                V(lambda: nc.vector.tensor_copy(out=posf[:, 0:n], in_=posi[:, 0:n]), [posi], [posf])
                for n0 in range(0, n, 16):
                    m_ = min(16, n - n0)
                    rA, rB, rI = h1t2[0], h1t2[1], sqr
                    aA = rA[:, 0:m_ * 32].rearrange("p (n j) -> p n j", j=32)
                    aB = rB[:, 0:m_ * 32].rearrange("p (n j) -> p n j", j=32)
                    aI = rI[:, 0:m_ * 32].bitcast(I32).rearrange("p (n j) -> p n j", j=32)
                    V(lambda: nc.vector.tensor_tensor(out=aA, in0=posf[:, n0:n0 + m_].unsqueeze(2).broadcast_to([P, m_, 32]),
                                                      in1=cst[:, C_FREQ:C_FREQ + 32].unsqueeze(1).broadcast_to([P, m_, 32]), op=ALU.mult),
                      [posf, cst], [rA])
                    for (shift, dstT) in ((0.0, sinT), (math.pi / 2, cosT)):
                        V(lambda shift=shift: nc.vector.tensor_scalar(out=aB, in0=aA, scalar1=shift, scalar2=1.0 / TWO_PI,
                                                                      op0=ALU.add, op1=ALU.mult), [rA], [rB])
                        V(lambda: nc.vector.tensor_copy(out=aI, in_=aB), [rB], [rI])
                        V(lambda: nc.vector.tensor_copy(out=aB, in_=aI), [rI], [rB])
                        V(lambda: nc.vector.scalar_tensor_tensor(out=aB, in0=aB, scalar=-TWO_PI, in1=aA,
                                                                 op0=ALU.mult, op1=ALU.add), [rB, rA], [rB])
                        V(lambda shift=shift: nc.vector.tensor_scalar(out=aB, in0=aB, scalar1=shift, scalar2=math.pi,
                                                                      op0=ALU.add, op1=ALU.min), [rB], [rB])
                        V(lambda: nc.vector.tensor_scalar(out=aB, in0=aB, scalar1=-math.pi, scalar2=None,
                                                          op0=ALU.max), [rB], [rB])
                        A(lambda dstT=dstT: nc.scalar.activation(out=dstT[:, n0:n0 + m_, :], in_=aB, func=AF.Sin), [rB], [dstT])

            def rotary(src, dst, n):
                sv = src[:, :].rearrange("p (h t j) -> p h t j", h=8, t=2)
                dv = dst[:, :].rearrange("p (h t j) -> p h t j", h=8, t=2)
                cb = cosT[:, n, :].unsqueeze(1).broadcast_to([P, 8, 32])
                sb_ = sinT[:, n, :].unsqueeze(1).broadcast_to([P, 8, 32])
                q1, q2 = sv[:, :, 0, :], sv[:, :, 1, :]
                G(lambda: nc.gpsimd.tensor_tensor(out=rt[0][:, :, :], in0=q1, in1=cb, op=ALU.mult), [src, cosT], [rt[0]])
                G(lambda: nc.gpsimd.tensor_tensor(out=rt[1][:, :, :], in0=q2, in1=sb_, op=ALU.mult), [src, sinT], [rt[1]])
                G(lambda: nc.gpsimd.tensor_tensor(out=rt[2][:, :, :], in0=q1, in1=sb_, op=ALU.mult), [src, sinT], [rt[2]])
                G(lambda: nc.gpsimd.tensor_tensor(out=rt[3][:, :, :], in0=q2, in1=cb, op=ALU.mult), [src, cosT], [rt[3]])
                G(lambda: nc.gpsimd.tensor_tensor(out=dv[:, :, 0, :], in0=rt[0][:, :, :], in1=rt[1][:, :, :], op=ALU.subtract), [rt[0], rt[1]], [dst])
                G(lambda: nc.gpsimd.tensor_tensor(out=dv[:, :, 1, :], in0=rt[2][:, :, :], in1=rt[3][:, :, :], op=ALU.add), [rt[2], rt[3]], [dst])

            def inproj(bank, c0, ncols, base=0):
                for k in range(8):
                    T(lambda k=k: nc.tensor.matmul(bank[:, base:base + ncols], lhsT=xT[:, k, :], rhs=Win[:, k, c0:c0 + ncols],
                                                   start=(k == 0), stop=(k == 7)), [xT, Win], [bank])

            def transpose8(src, dstT, nk=8):
                kb.lock(bkT)
                for k in range(nk):
                    T(lambda k=k: nc.tensor.transpose(out=bkT[:, k * P:(k + 1) * P], in_=src[:, k * P:(k + 1) * P], identity=identb[:, :]),
                      [src, identb], [bkT])
                A(lambda: nc.scalar.copy(out=dstT[:, 0:nk, :], in_=bkT[:, 0:nk * P].rearrange("p (k t) -> p k t", k=nk)), [bkT], [dstT])
                kb.unlock(bkT)

            def load_x(gi_):
                xsrc_, n_, _ = seq[gi_]
                xt_ = xs[gi_ % 3]
                SP(lambda: nc.sync.dma_start(out=xt_[:, :], in_=xsrc_.t[n_ * P:(n_ + 1) * P, :]), [xsrc_], [xt_])

            def proj(bank, c0, xTt):
                for k in range(8):
                    T(lambda k=k: nc.tensor.matmul(bank[:, :], lhsT=xTt[:, k, :], rhs=Win[:, k, c0:c0 + 512],
                                                   start=(k == 0), stop=(k == 7)), [xTt, Win], [bank], 512)

            def projT(bank, c0, xTt):
                for cc in range(4):
                    for k in range(8):
                        T(lambda k=k, cc=cc: nc.tensor.matmul(bank[:, cc * P:(cc + 1) * P], lhsT=Win[:, k, c0 + cc * P:c0 + (cc + 1) * P], rhs=xTt[:, k, :],
                                                              start=(k == 0), stop=(k == 7)), [xTt, Win], [bank])

            def pre(gi):
                xt = xs[gi % 3]
                smj = sm[gi % 2]
                A(lambda: nc.scalar.activation(out=junk[:, :], in_=xt[:, :], func=AF.Square, accum_out=smj[:, 0:1]), [xt], [junk, smj])
                A(lambda: nc.scalar.activation(out=smj[:, 1:2], in_=smj[:, 0:1], func=AF.Ln, bias=EPS, scale=1.0 / D), [smj], [smj], 1)
                A(lambda: nc.scalar.activation(out=smj[:, 2:3], in_=smj[:, 1:2], func=AF.Exp, scale=-0.5), [smj], [smj], 1)
                A(lambda: nc.scalar.activation(out=xb[:, :], in_=xt[:, :], func=AF.Copy, scale=smj[:, 2:3]), [xt, smj], [xb])
                transpose8(xb, xT[gi % 2])
                yield

            def front(gi):
                xsrc, n, main = seq[gi]
                j = gi % 2
                xt = xs[gi % 3]
                smj, qsj, ksj, vrj, grj, qkaj, krj, qkTj, vm1j, goj, khj = sm[j], qs[j], ks[j], vr[j], gr[j], qka[j], kr[j], qkTr[j], vm1[j], go[j], khat[j]
                qhalo = main or (gi + 1 < len(seq) and seq[gi + 1][2])
                xTt = xT[j]
                cvc, cvn = cv[j], cv[1 - j]
                c_lo = 0 if main else 4
                h_lo = 0 if qhalo else 4

                def conv_mm(bank, c0):
                    for cc in range(4):
                        c = c0 + cc
                        for jj in range(4):
                            T(lambda c=c, cc=cc, jj=jj: nc.tensor.matmul(bank[:, cc * P:(cc + 1) * P], lhsT=Dg[:, c * 4 + jj, :], rhs=cvc[:, c, jj:jj + 128],
                                                                         start=(jj == 0), stop=False), [Dg, cvc], [bank])
                        T(lambda c=c, cc=cc: nc.tensor.matmul(bank[:, cc * P:(cc + 1) * P], lhsT=brow[0:1, c * P:(c + 1) * P], rhs=onesr[0:1, :],
                                                              start=False, stop=True), [brow, onesr], [bank])

                projT(b1, 2560, xTt)
                A(lambda: nc.scalar.copy(out=cvc[:, 4:8, 3:131], in_=b1[:, :].rearrange("p (c t) -> p c t", c=4)), [b1], [cvc], 512)
                if qhalo:
                    projT(b0, 2048, xTt)
                    A(lambda: nc.scalar.copy(out=cvc[:, 0:4, 3:131], in_=b0[:, :].rearrange("p (c t) -> p c t", c=4)), [b0], [cvc], 512)
                G(lambda: nc.gpsimd.tensor_copy(out=cvn[:, h_lo:8, 0:3], in_=cvc[:, h_lo:8, 128:131]), [cvc], [cvn], 24)
                conv_mm(b1, 4)
                if main:
                    conv_mm(b0, 0)
                kb.lock_engine("act")
                if main:
                    A(lambda: nc.scalar.activation(out=qkaj[:, 0:4, :], in_=b0[:, :].rearrange("p (c t) -> p c t", c=4), func=AF.Silu), [b0], [qkaj], 512)
                    proj(b0, 1536, xTt)
                A(lambda: nc.scalar.activation(out=qkaj[:, 4:8, :], in_=b1[:, :].rearrange("p (c t) -> p c t", c=4), func=AF.Silu), [b1], [qkaj], 512)
                if main:
                    A(lambda: nc.scalar.activation(out=grj[:, :], in_=b0[:, :], func=AF.Silu), [b0], [grj], 512)
                kb.unlock_engine("act")
                yield
                for k in range(8):
                    T(lambda k=k: nc.tensor.matmul(ps_g[:, 0:8], lhsT=xTt[:, k, :], rhs=Win[:, k, 4096:4104], start=(k == 0), stop=(k == 7)), [xTt, Win], [ps_g], 8)
                V(lambda: nc.vector.tensor_tensor(out=smj[:, 8:16], in0=ps_g[:, 0:8], in1=bcs[:, 36:44], op=ALU.add), [ps_g, bcs], [smj], 8)
                A(lambda: nc.scalar.activation(out=smj[:, 16:20], in_=smj[:, 12:16], func=AF.Exp, scale=-1.0), [smj], [smj], 4)
                A(lambda: nc.scalar.activation(out=smj[:, 20:24], in_=smj[:, 16:20], func=AF.Ln, bias=1.0, scale=1.0), [smj], [smj], 4)
                T(lambda: nc.tensor.matmul(ps_cs[:, 8:12], lhsT=cst[:, C_MASK:C_MASK + P], rhs=smj[:, 20:24], start=True, stop=True), [cst, smj], [ps_cs], 16)
                T(lambda: nc.tensor.matmul(ps_cs[:, 12:16], lhsT=cst[:, C_ONES:C_ONES + P], rhs=smj[:, 20:24], start=True, stop=True), [cst, smj], [ps_cs], 16)
                V(lambda: nc.vector.tensor_tensor(out=smj[:, 24:28], in0=smj[:, 8:12], in1=ps_cs[:, 8:12], op=ALU.add), [smj, ps_cs], [smj], 4)
                A(lambda: nc.scalar.activation(out=smj[:, 28:32], in_=smj[:, 24:28], func=AF.Exp, bias=math.log(dscale), scale=1.0), [smj], [smj], 4)
                if main:
                    A(lambda: nc.scalar.activation(out=smj[:, 32:36], in_=ps_cs[:, 8:12], func=AF.Exp, scale=-1.0), [ps_cs], [smj], 4)
                dcur = dec[gi % 3]
                A(lambda: nc.scalar.activation(out=dcur[:, :], in_=ps_cs[:, 12:16], func=AF.Exp, scale=-1.0), [ps_cs], [dcur], 4)
                yield
                proj(b1, 512, xTt)
                V(lambda: nc.vector.tensor_tensor(out=ksj[:, :].rearrange("p (h e) -> p h e", h=8), in0=b1[:, :].rearrange("p (h e) -> p h e", h=8), in1=cst[:, C_GK:C_GK + 8].unsqueeze(2).broadcast_to([P, 8, 64]), op=ALU.mult), [b1, cst], [ksj], 512)
                rotary(ksj, krj, n)
                if main:
                    proj(b0, 0, xTt)
                    V(lambda: nc.vector.tensor_tensor(out=qsj[:, :].rearrange("p (h e) -> p h e", h=8), in0=b0[:, :].rearrange("p (h e) -> p h e", h=8), in1=cst[:, C_GQ:C_GQ + 8].unsqueeze(2).broadcast_to([P, 8, 64]), op=ALU.mult), [b0, cst], [qsj], 512)
                    rotary(qsj, qr, n)
                yield
                proj(b1, 1024, xTt)
                A(lambda: nc.scalar.copy(out=vrj[:, :], in_=b1[:, :]), [b1], [vrj], 512)
                proj(b0, 3072, xTt)
                A(lambda: nc.scalar.copy(out=vm1j[:, :, 0:128], in_=b0[:, :].rearrange("p (h e) -> p h e", h=4)), [b0], [vm1j], 512)
                if main:
                    proj(b1, 3584, xTt)
                    A(lambda: nc.scalar.activation(out=goj[:, :], in_=b1[:, :], func=AF.Exp, scale=-1.0), [b1], [goj], 512)
                    V(lambda: nc.vector.tensor_scalar(out=goj[:, :], in0=goj[:, :], scalar1=1.0, scalar2=None, op0=ALU.add), [goj], [goj], 512)
                    V(lambda: nc.vector.reciprocal(out=goj[:, :], in_=goj[:, :]), [goj], [goj], 512)
                yield
                if main:
                    b0h = b0[:, :].bitcast(BF16)
                    b1h = b1[:, :].bitcast(BF16)
                    for h in range(8):
                        T(lambda h=h: nc.tensor.transpose(out=b0h[0:64, h * P:(h + 1) * P], in_=qr[:, h * 64:(h + 1) * 64], identity=identb[:, :]), [qr, identb], [b0], 64)
                    for h in range(8):
                        T(lambda h=h: nc.tensor.transpose(out=b1h[0:64, h * P:(h + 1) * P], in_=krj[:, h * 64:(h + 1) * 64], identity=identb[:, :]), [krj, identb], [b1], 64)
                kb.lock(bkT)
                for h in range(4):
                    T(lambda h=h: nc.tensor.transpose(out=bkT[:, h * P:(h + 1) * P], in_=qkaj[:, 4 + h, :], identity=identb[:, :]), [qkaj, identb], [bkT])
                for h in range(4):
                    A(lambda h=h: nc.scalar.activation(out=khj[:, h, :], in_=bkT[:, h * P:(h + 1) * P], func=AF.Copy, scale=smj[:, 28 + h:29 + h]), [bkT, smj], [khj], 128)
                kb.unlock(bkT)
                if main:
                    A(lambda: nc.scalar.copy(out=qkTj[0:64, 0:8, :], in_=b0h[0:64, :].rearrange("p (k t) -> p k t", k=8)), [b0], [qkTj])
                    V(lambda: nc.vector.tensor_copy(out=qkTj[0:64, 8:16, :], in_=b1h[0:64, :].rearrange("p (k t) -> p k t", k=8)), [b1], [qkTj])

            def back(gi):
                xsrc, n, main = seq[gi]
                j = gi % 2
                xt = xs[gi % 3]
                h1t = h1t2[gi % 2]
                smj, vrj, grj, qkaj, krj, qkTj, vm1j, goj, khj = sm[j], vr[j], gr[j], qka[j], kr[j], qkTr[j], vm1[j], go[j], khat[j]
                dcur, dprev = dec[gi % 3], dec[(gi - 1) % 3]
                eu = lambda h: smj[:, 28 + h:29 + h]
                if main:
                    for h in range(8):
                        bank = b2 if h < 4 else b3
                        hh = h % 4
                        T(lambda h=h, bank=bank, hh=hh: nc.tensor.matmul(bank[:, hh * P:(hh + 1) * P], lhsT=qkTj[0:64, 8 + h, :],
                                                                         rhs=qkTj[0:64, h, :], start=True, stop=True), [qkTj], [bank])
                    for h in range(4):
                        T(lambda h=h: nc.tensor.matmul(b5[:, h * P:(h + 1) * P], lhsT=qkaj[:, 4 + h, :], rhs=qkaj[:, h, :], start=True, stop=True), [qkaj], [b5])
                    mb4 = cst[:, C_MASK:C_MASK + P].unsqueeze(1).broadcast_to([P, 4, P])
                    V(lambda: nc.vector.tensor_tensor(out=STt[:, 0:4, :], in0=b2[:, :].rearrange("p (h t) -> p h t", h=4), in1=mb4, op=ALU.mult), [b2, cst], [STt])
                    V(lambda: nc.vector.tensor_tensor(out=STt[:, 4:8, :], in0=b3[:, :].rearrange("p (h t) -> p h t", h=4), in1=mb4, op=ALU.mult), [b3, cst], [STt])
                    for h in range(4):
                        V(lambda h=h: nc.vector.scalar_tensor_tensor(out=PTt[:, h, :], in0=b5[:, h * P:(h + 1) * P], scalar=eu(h), in1=cst[:, C_MASK:C_MASK + P],
                                                                     op0=ALU.mult, op1=ALU.mult), [b5, smj, cst], [PTt], 128)
                yield
                for h in range(8):
                    T(lambda h=h: nc.tensor.matmul(b6[0:64, h * 64:(h + 1) * 64], lhsT=krj[:, h * 64:(h + 1) * 64],
                                                   rhs=vrj[:, h * 64:(h + 1) * 64], start=True, stop=True), [krj, vrj], [b6], 64)
                dsb = lambda h: (b2 if h < 2 else b3)
                for h in range(4):
                    o0 = (h % 2) * 129
                    T(lambda h=h, o0=o0: nc.tensor.matmul(dsb(h)[:, o0:o0 + 129], lhsT=khj[:, h, :], rhs=vm1j[:, h, :], start=True, stop=True), [khj, vm1j], [dsb(h)])
                V(lambda: nc.vector.tensor_tensor(out=Rst[0:64, :, :], in0=b6[0:64, :].rearrange("p (h e) -> p h e", h=8), in1=Rst[0:64, :, :], op=ALU.add), [b6, Rst], [Rst], 512)
                V(lambda: nc.vector.tensor_tensor(out=Rst[0:64, :, :], in0=Rst[0:64, :, :], in1=cst[0:64, C_GC:C_GC + 8].unsqueeze(2).broadcast_to([64, 8, 64]), op=ALU.mult), [Rst, cst], [Rst], 512)
                for h in range(4):
                    o0 = (h % 2) * 129
                    V(lambda h=h, o0=o0: nc.vector.scalar_tensor_tensor(out=Tst[:, h, :], in0=Tst[:, h, :], scalar=dprev[:, h:h + 1], in1=dsb(h)[:, o0:o0 + 129],
                                                                        op0=ALU.mult, op1=ALU.add), [Tst, dprev, dsb(h)], [Tst], 129)
                yield
                ob = lambda h: (b6 if h < 2 else b2)
                if main:
                    for h in range(8):
                        T(lambda h=h: nc.tensor.matmul(b5[:, h * 64:(h + 1) * 64], lhsT=STt[:, h, :], rhs=vrj[:, h * 64:(h + 1) * 64], start=True, stop=False),
                          [STt, vrj], [b5], 64)
                        T(lambda h=h: nc.tensor.matmul(b5[:, h * 64:(h + 1) * 64], lhsT=qkTj[0:64, h, :], rhs=Rbf[0:64, h, :],
                                                       start=False, stop=True), [qkTj, Rbf], [b5], 64)
                    for h in range(4):
                        o0 = (h % 2) * 129
                        T(lambda h=h, o0=o0: nc.tensor.matmul(ob(h)[:, o0:o0 + 129], lhsT=PTt[:, h, :], rhs=vm1j[:, h, :], start=True, stop=False), [PTt, vm1j], [ob(h)])
                        T(lambda h=h, o0=o0: nc.tensor.matmul(ob(h)[:, o0:o0 + 129], lhsT=qkaj[:, h, :], rhs=Sbf[:, h, :], start=False, stop=True), [qkaj, Sbf], [ob(h)])
                G(lambda: nc.gpsimd.tensor_copy(out=Rbf[0:64, :, :], in_=Rst[0:64, :, :]), [Rst], [Rbf], 512)
                for h in range(4):
                    A(lambda h=h: nc.scalar.activation(out=Sbf[:, h, :], in_=Tst[:, h, :], func=AF.Copy, scale=dcur[:, h:h + 1]), [Tst, dcur], [Sbf], 129)
                if not main:
                    return
                yield
                A(lambda: nc.scalar.activation(out=sqr[:, :], in_=b5[:, :], func=AF.Square), [b5], [sqr])
                V(lambda: nc.vector.reduce_sum(out=sm2[:, 0:8], in_=sqr[:, :].rearrange("p (h e) -> p h e", h=8), axis=AX.X), [sqr], [sm2], 512)
                A(lambda: nc.scalar.activation(out=sm2[:, 8:16], in_=sm2[:, 0:8], func=AF.Ln, bias=EPS, scale=1.0 / 64), [sm2], [sm2], 8)
                A(lambda: nc.scalar.activation(out=sm2[:, 16:24], in_=sm2[:, 8:16], func=AF.Exp, scale=-0.5), [sm2], [sm2], 8)
                V(lambda: nc.vector.tensor_tensor(out=rn[:, :].rearrange("p (h e) -> p h e", h=8), in0=b5[:, :].rearrange("p (h e) -> p h e", h=8),
                                                  in1=sm2[:, 16:24].unsqueeze(2).broadcast_to([P, 8, 64]), op=ALU.mult), [b5, sm2], [rn])
                G(lambda: nc.gpsimd.tensor_tensor(out=y[:, 0:512], in0=rn[:, :], in1=grj[:, :], op=ALU.mult), [rn, grj], [y], 512)
                yield
                if main:
                    for h in range(4):
                        o0 = (h % 2) * 129
                        V(lambda h=h, o0=o0: nc.vector.tensor_tensor(out=sm2[:, 24 + h:25 + h], in0=ob(h)[:, o0 + 128:o0 + 129], in1=smj[:, 32 + h:33 + h], op=ALU.mult),
                          [ob(h), smj], [sm2])
                    V(lambda: nc.vector.tensor_tensor(out=sm2[:, 28:32], in0=sm2[:, 24:28], in1=sm2[:, 24:28], op=ALU.mult), [sm2], [sm2])
                    V(lambda: nc.vector.tensor_scalar(out=sm2[:, 28:32], in0=sm2[:, 28:32], scalar1=1.0, scalar2=None, op0=ALU.max), [sm2], [sm2])
                    A(lambda: nc.scalar.activation(out=sm2[:, 32:36], in_=sm2[:, 28:32], func=AF.Ln), [sm2], [sm2])
                    A(lambda: nc.scalar.activation(out=sm2[:, 36:40], in_=sm2[:, 32:36], func=AF.Exp, scale=-0.5), [sm2], [sm2])
                    V(lambda: nc.vector.tensor_tensor(out=sm2[:, 40:44], in0=sm2[:, 36:40], in1=smj[:, 32:36], op=ALU.mult), [sm2, smj], [sm2])
                    for h in range(4):
                        o0 = (h % 2) * 129
                        V(lambda h=h, o0=o0: nc.vector.scalar_tensor_tensor(out=hg[:, h * P:(h + 1) * P], in0=ob(h)[:, o0:o0 + 128], scalar=sm2[:, 40 + h:41 + h],
                                                                            in1=goj[:, h * P:(h + 1) * P], op0=ALU.mult, op1=ALU.mult), [ob(h), sm2, goj], [hg])
                        A(lambda h=h: nc.scalar.activation(out=junk[:, h * P:(h + 1) * P], in_=hg[:, h * P:(h + 1) * P], func=AF.Square, accum_out=sm2[:, 44 + h:45 + h]),
                          [hg], [junk, sm2])
                    A(lambda: nc.scalar.activation(out=sm2[:, 48:52], in_=sm2[:, 44:48], func=AF.Ln, bias=EPS, scale=1.0 / 128), [sm2], [sm2])
                    A(lambda: nc.scalar.activation(out=sm2[:, 52:56], in_=sm2[:, 48:52], func=AF.Exp, scale=-0.5), [sm2], [sm2])
                    V(lambda: nc.vector.tensor_tensor(out=y[:, 512:1024].rearrange("p (h e) -> p h e", h=4), in0=hg[:, :].rearrange("p (h e) -> p h e", h=4),
                                                      in1=sm2[:, 52:56].unsqueeze(2).broadcast_to([P, 4, P]), op=ALU.mult), [hg, sm2], [y])
                yield
                transpose8(y, yT)
                for half in range(2):
                    bank = b3 if half == 0 else b5
                    for k in range(8):
                        T(lambda k=k, bank=bank, half=half: nc.tensor.matmul(bank[:, :], lhsT=yT[:, k, :], rhs=Wout[:, k, half * 512:(half + 1) * 512],
                                                                             start=(k == 0), stop=(k == 7)), [yT, Wout], [bank], 512)
                V(lambda: nc.vector.tensor_tensor(out=h1t[:, 0:512], in0=b3[:, :], in1=xt[:, 0:512], op=ALU.add), [b3, xt], [h1t], 512)
                V(lambda: nc.vector.tensor_tensor(out=h1t[:, 512:1024], in0=b5[:, :], in1=xt[:, 512:1024], op=ALU.add), [b5, xt], [h1t], 512)
                SP(lambda: nc.sync.dma_start(out=h1s.t[n * P:(n + 1) * P, :], in_=h1t[:, :]), [h1t], [h1s])

            def back2(gi):
                xsrc, n, main = seq[gi]
                h1t = h1t2[gi % 2]
                A(lambda: nc.scalar.activation(out=junk[:, :], in_=h1t[:, :], func=AF.Square, accum_out=sm2[:, 56:57]), [h1t], [junk, sm2])
                A(lambda: nc.scalar.activation(out=sm2[:, 57:58], in_=sm2[:, 56:57], func=AF.Ln, bias=EPS, scale=1.0 / D), [sm2], [sm2])
                A(lambda: nc.scalar.activation(out=sm2[:, 58:59], in_=sm2[:, 57:58], func=AF.Exp, scale=-0.5), [sm2], [sm2])
                V(lambda: nc.vector.scalar_tensor_tensor(out=xn2[:, :], in0=h1t[:, :], scalar=sm2[:, 58:59], in1=bmoe[:, :], op0=ALU.mult, op1=ALU.mult),
                  [h1t, sm2, bmoe], [xn2])
                transpose8(xn2, xn2T)
                for k in range(8):
                    T(lambda k=k: nc.tensor.matmul(ps_lg[:, 16:52], lhsT=xn2T[:, k, :], rhs=Wr[:, k, :], start=(k == 0), stop=(k == 7)), [xn2T, Wr], [ps_lg])
                yield
                V(lambda: nc.vector.tensor_tensor(out=rl[:, 0:36], in0=ps_lg[:, 16:52], in1=bcs[:, 0:36], op=ALU.add), [ps_lg, bcs], [rl])
                V(lambda: nc.vector.reduce_max(out=rl[:, 36:37], in_=rl[:, 0:4], axis=AX.X), [rl], [rl])
                V(lambda: nc.vector.tensor_scalar(out=rl[:, 37:38], in0=rl[:, 36:37], scalar1=-1.0, scalar2=None, op0=ALU.mult), [rl], [rl])
                A(lambda: nc.scalar.activation(out=rl[:, 44:48], in_=rl[:, 0:4], func=AF.Exp, bias=rl[:, 37:38], scale=1.0, accum_out=rl[:, 38:39]), [rl], [rl])
                V(lambda: nc.vector.reciprocal(out=rl[:, 39:40], in_=rl[:, 38:39]), [rl], [rl])
                V(lambda: nc.vector.tensor_scalar(out=rl[:, 40:44], in0=rl[:, 0:4], scalar1=rl[:, 36:37], scalar2=None, op0=ALU.is_ge), [rl], [rl])
                V(lambda: nc.vector.tensor_tensor(out=rl2[:, 0:32].rearrange("p (g e) -> p g e", g=4), in0=rl[:, 4:36].rearrange("p (g e) -> p g e", g=4),
                                                  in1=rl[:, 40:44].unsqueeze(2).broadcast_to([P, 4, 8]), op=ALU.mult), [rl], [rl2])
                V(lambda: nc.vector.reduce_sum(out=rl2[:, 32:40], in_=rl2[:, 0:32].rearrange("p (g e) -> p e g", g=4), axis=AX.X), [rl2], [rl2])
                V(lambda: nc.vector.max(out=m8[:, :], in_=rl2[:, 32:40]), [rl2], [m8])
                V(lambda: nc.vector.max_index(out=i8[:, :], in_max=m8[:, :], in_values=rl2[:, 32:40]), [m8, rl2], [i8])
                V(lambda: nc.vector.tensor_tensor(out=rl2[:, 40:44], in0=rl[:, 40:44], in1=cst[:, C_IOTA4:C_IOTA4 + 4], op=ALU.mult), [rl, cst], [rl2])
                V(lambda: nc.vector.reduce_sum(out=rl[:, 48:49], in_=rl2[:, 40:44], axis=AX.X), [rl2], [rl])
                V(lambda: nc.vector.tensor_tensor(out=rl[:, 49:50], in0=m8[:, 0:1], in1=m8[:, 1:2], op=ALU.subtract), [m8], [rl])
                A(lambda: nc.scalar.activation(out=rl[:, 50:51], in_=rl[:, 49:50], func=AF.Exp, scale=-1.0), [rl], [rl], 1)
                V(lambda: nc.vector.tensor_scalar(out=rl[:, 50:51], in0=rl[:, 50:51], scalar1=1.0, scalar2=None, op0=ALU.add), [rl], [rl], 1)
                V(lambda: nc.vector.reciprocal(out=rl[:, 50:51], in_=rl[:, 50:51]), [rl], [rl], 1)
                V(lambda: nc.vector.tensor_copy(out=rl2[:, 44:46], in_=i8[:, 0:2]), [i8], [rl2])
                V(lambda: nc.vector.scalar_tensor_tensor(out=rl[:, 51:53], in0=rl[:, 48:49].broadcast_to([P, 2]), scalar=8.0, in1=rl2[:, 44:46], op0=ALU.mult, op1=ALU.add),
                  [rl, rl2], [rl])
                yield
                for k_ in range(2):
                    V(lambda k_=k_: nc.vector.tensor_scalar(out=oh[:, k_, :], in0=cst[:, C_IOTA32:C_IOTA32 + 32], scalar1=rl[:, 51 + k_:52 + k_], scalar2=None, op0=ALU.is_equal),
                      [cst, rl], [oh])
                V(lambda: nc.vector.tensor_tensor(out=ohb[:, :], in0=oh[:, 0, :], in1=oh[:, 1, :], op=ALU.add), [oh], [ohb])
                T(lambda: nc.tensor.matmul(ps_pos[:, 64:96], lhsT=ustrb[:, :], rhs=ohb[:, :], start=True, stop=True), [ustrb, ohb], [ps_pos])
                T(lambda: nc.tensor.matmul(ps_tot[:, 96:128], lhsT=onesb[:, :], rhs=ohb[:, :], start=True, stop=True), [onesb, ohb], [ps_tot])
                V(lambda: nc.vector.tensor_tensor(out=pos_s[:, :], in0=ps_pos[:, 64:96], in1=CNT[:, :], op=ALU.add), [ps_pos, CNT], [pos_s])
                V(lambda: nc.vector.tensor_tensor(out=CNT[:, :], in0=ps_tot[:, 96:128], in1=CNT[:, :], op=ALU.add), [ps_tot, CNT], [CNT])
                for k_ in range(2):
                    V(lambda k_=k_: nc.vector.tensor_tensor(out=oh[:, 2, :], in0=oh[:, k_, :], in1=pos_s[:, :], op=ALU.mult), [oh, pos_s], [oh])
                    V(lambda k_=k_: nc.vector.reduce_sum(out=rl[:, 53 + k_:54 + k_], in_=oh[:, 2, :], axis=AX.X), [oh], [rl])
                V(lambda: nc.vector.tensor_scalar(out=rl[:, 55:57], in0=rl[:, 53:55], scalar1=float(CAP), scalar2=None, op0=ALU.is_lt), [rl], [rl])
                V(lambda: nc.vector.scalar_tensor_tensor(out=rl[:, 57:59], in0=rl[:, 51:53], scalar=float(CAP), in1=rl[:, 53:55], op0=ALU.mult, op1=ALU.add), [rl], [rl])
                V(lambda: nc.vector.tensor_scalar(out=rl2[:, 46:48], in0=rl[:, 55:57], scalar1=-4.0e6, scalar2=4.0e6, op0=ALU.mult, op1=ALU.add), [rl], [rl2])
                V(lambda: nc.vector.tensor_tensor(out=rl[:, 57:59], in0=rl[:, 57:59], in1=rl2[:, 46:48], op=ALU.add), [rl, rl2], [rl])
                V(lambda: nc.vector.tensor_copy(out=dest_i[:, n, :], in_=rl[:, 57:59]), [rl], [dest_i])
                V(lambda: nc.vector.tensor_tensor(out=rl2[:, 48:49], in0=rl[:, 39:40], in1=rl[:, 50:51], op=ALU.mult), [rl], [rl2])
                V(lambda: nc.vector.tensor_tensor(out=rl2[:, 49:50], in0=rl[:, 39:40], in1=rl2[:, 48:49], op=ALU.subtract), [rl, rl2], [rl2])
                V(lambda: nc.vector.tensor_tensor(out=gates[:, n, :], in0=rl2[:, 48:50], in1=rl[:, 55:57], op=ALU.mult), [rl2, rl], [gates])
                for k_ in range(2 if scatter else 0):
                    GD(lambda k_=k_: nc.gpsimd.indirect_dma_start(out=xbuf.t[:, :], out_offset=bass.IndirectOffsetOnAxis(ap=dest_i[:, n, k_:k_ + 1], axis=0),
                                                                   in_=xn2[:, :], in_offset=None, bounds_check=bc_reg, oob_is_err=False),
                       [xn2, dest_i, xbuf_z], [xbuf])

            def interleave(gens):
                gens = [g for g in gens if g is not None]
                while gens:
                    for g in list(gens):
                        try:
                            next(g)
                        except StopIteration:
                            gens.remove(g)

            def apply_carry():
                cm = cst[:, C_CARRY:C_CARRY + 1]
                V(lambda: nc.vector.tensor_scalar(out=Tst[:, :, :], in0=Tst[:, :, :], scalar1=cm, scalar2=None, op0=ALU.mult), [Tst, cst], [Tst])
                V(lambda: nc.vector.tensor_scalar(out=Sbf[:, :, :], in0=Sbf[:, :, :], scalar1=cm, scalar2=None, op0=ALU.mult), [Sbf, cst], [Sbf])
                V(lambda: nc.vector.tensor_scalar(out=Rst[:, :, :], in0=Rst[:, :, :], scalar1=cm, scalar2=None, op0=ALU.mult), [Rst, cst], [Rst])
                V(lambda: nc.vector.tensor_scalar(out=Rbf[:, :, :], in0=Rbf[:, :, :], scalar1=cm, scalar2=None, op0=ALU.mult), [Rbf, cst], [Rbf])
                for i in range(2):
                    V(lambda i=i: nc.vector.tensor_scalar(out=cv[i][:, :, 0:3], in0=cv[i][:, :, 0:3], scalar1=cm, scalar2=None, op0=ALU.mult), [cv[i], cst], [cv[i]])

            seq = [(x_pre, n, False) for n in range(PT)] + [(x_main, n, True) for n in range(NT)]
            NS = len(seq)
            load_x(0)
            if NS > 1:
                load_x(1)
            if PT > 0:
                make_trig(pos_pre, PT, xs[2])
                kb.merge([kb.record(pre(0))])
                pr1 = kb.record(pre(1)) if NS > 1 else []
                kb.merge([kb.record(front(0)), pr1])
                if NS > 2:
                    load_x(2)
                for gi in range(PT):
                    nxt = kb.record(front(gi + 1)) if gi + 1 < PT else []
                    prn = kb.record(pre(gi + 2)) if gi + 2 < NS else []
                    kb.merge([kb.record(back(gi)), nxt, prn])
                    if gi + 3 < NS:
                        load_x(gi + 3)
                    zfill()
                while zpending:
                    zfill()
                apply_carry()
                make_trig(pos_main, NT, h1t2[1])
                kb.merge([kb.record(front(PT))])
            else:
                while zpending:
                    zfill()
                make_trig(pos_main, NT, xs[2])
                kb.merge([kb.record(pre(0))])
                pr1 = kb.record(pre(1)) if NS > 1 else []
                kb.merge([kb.record(front(0)), pr1])
                if NS > 2:
                    load_x(2)
            for gi in range(PT, NS):
                nxt = kb.record(front(gi + 1)) if gi + 1 < NS else []
                prv = kb.record(back2(gi - 1)) if gi - 1 >= PT else []
                prn = kb.record(pre(gi + 2)) if gi + 2 < NS else []
                kb.merge([kb.record(back(gi)), prv, nxt, prn])
                if gi + 3 < NS:
                    load_x(gi + 3)
            kb.merge([kb.record(back2(NS - 1))])
            kb.barrier()
            if stop <= 3:
                return nc

        MODEL.update(MODEL_BC)
        with ExitStack() as bst:
            w1s = kb.sb(bst, "w1s", [P, 8, DEXP], F32)
            w3s = kb.sb(bst, "w3s", [P, 8, DEXP], F32)
            w2s = kb.sb(bst, "w2s", [P, 4, D], F32)
            w1b = [kb.sb(bst, "w1b%d" % i, [P, 8, DEXP], BF16) for i in range(2)]
            w3b = [kb.sb(bst, "w3b%d" % i, [P, 8, DEXP], BF16) for i in range(2)]
            w2b = [kb.sb(bst, "w2b%d" % i, [P, 4, D], BF16) for i in range(2)]
            xg = [kb.sb(bst, "xg%d" % i, [P, NB, D], BF16) for i in range(2)]
            xgT = [kb.sb(bst, "xgT%d" % i, [P, 8, CAP], BF16) for i in range(2)]
            sil = [kb.sb(bst, "sil%d" % i, [P, CAP], F32) for i in range(2)]
            hT = [kb.sb(bst, "hT%d" % i, [P, 4, CAP], BF16) for i in range(2)]
            yo = [kb.sb(bst, "yo%d" % i, [P, D], BF16) for i in range(3)]

            def load_expert(e):
                SP(lambda: nc.sync.dma_start(out=w1s[:, :, :], in_=w1.t[e, :, :].rearrange("(k p) n -> p k n", p=P)), [w1], [w1s])
                SP(lambda: nc.sync.dma_start(out=w3s[:, :, :], in_=w3.t[e, :, :].rearrange("(k p) n -> p k n", p=P)), [w3], [w3s])
                SP(lambda: nc.sync.dma_start(out=w2s[:, :, :], in_=w2.t[e, :, :].rearrange("(k p) n -> p k n", p=P)), [w2], [w2s])
                SP(lambda: nc.sync.dma_start(out=xg[e % 2][:, :, :], in_=xbuf.t[e * CAP:(e + 1) * CAP, :].rearrange("(a p) f -> p a f", p=P)), [xbuf], [xg[e % 2]])

            def head(e):
                j = e % 2
                for k in range(0, 8, 2):
                    G(lambda k=k: nc.gpsimd.tensor_copy(out=w1b[j][:, k:k + 2, :], in_=w1s[:, k:k + 2, :]), [w1s], [w1b[j]], 1024)
                for k in range(0, 8, 2):
                    A(lambda k=k: nc.scalar.copy(out=w3b[j][:, k:k + 2, :], in_=w3s[:, k:k + 2, :]), [w3s], [w3b[j]], 1024)
                for k in range(4):
                    V(lambda k=k: nc.vector.tensor_copy(out=w2b[j][:, k, :], in_=w2s[:, k, :]), [w2s], [w2b[j]], 1024)
                if e + 1 < NE:
                    load_expert(e + 1)
                yield
                for a in range(NB):
                    kb.lock(bkT)
                    for k in range(8):
                        T(lambda a=a, k=k: nc.tensor.transpose(out=bkT[:, k * P:(k + 1) * P], in_=xg[j][:, a, k * P:(k + 1) * P], identity=identb[:, :]), [xg[j], identb], [bkT])
                    if a % 2 == 0:
                        A(lambda a=a: nc.scalar.copy(out=xgT[j][:, :, a * P:(a + 1) * P], in_=bkT[:, :].rearrange("p (k t) -> p k t", k=8)), [bkT], [xgT[j]])
                    else:
                        V(lambda a=a: nc.vector.tensor_copy(out=xgT[j][:, :, a * P:(a + 1) * P], in_=bkT[:, :].rearrange("p (k t) -> p k t", k=8)), [bkT], [xgT[j]])
                    kb.unlock(bkT)
                yield
                for m in range(4):
                    pa, pb_ = (b0, b1) if m % 2 == 0 else (b2, b3)
                    for k in range(8):
                        T(lambda k=k, m=m, pa=pa: nc.tensor.matmul(pa[:, 0:CAP], lhsT=w1b[j][:, k, m * P:(m + 1) * P], rhs=xgT[j][:, k, :], start=(k == 0), stop=(k == 7)), [w1b[j], xgT[j]], [pa], CAP)
                    for k in range(8):
                        T(lambda k=k, m=m, pb_=pb_: nc.tensor.matmul(pb_[:, 0:CAP], lhsT=w3b[j][:, k, m * P:(m + 1) * P], rhs=xgT[j][:, k, :], start=(k == 0), stop=(k == 7)), [w3b[j], xgT[j]], [pb_], CAP)
                    s_ = sil[m % 2]
                    A(lambda pa=pa, s_=s_: nc.scalar.activation(out=s_[:, :], in_=pa[:, 0:CAP], func=AF.Silu), [pa], [s_], CAP)
                    V(lambda m=m, pb_=pb_, s_=s_: nc.vector.tensor_tensor(out=hT[j][:, m, :], in0=pb_[:, 0:CAP], in1=s_[:, :], op=ALU.mult), [pb_, s_], [hT[j]], CAP)
                    yield

            def tail(e):
                j = e % 2
                for a in range(NB):
                    yt = yo[(e * NB + a) % 3]
                    for half in range(2):
                        bank = b5 if half == 0 else b6
                        for m in range(4):
                            T(lambda a=a, m=m, half=half, bank=bank: nc.tensor.matmul(bank[:, :], lhsT=hT[j][:, m, a * P:(a + 1) * P], rhs=w2b[j][:, m, half * 512:(half + 1) * 512],
                                                                                      start=(m == 0), stop=(m == 3)), [hT[j], w2b[j]], [bank], 512)
                        if half == 0:
                            A(lambda yt=yt, bank=bank: nc.scalar.copy(out=yt[:, 0:512], in_=bank[:, :]), [bank], [yt], 512)
                        else:
                            V(lambda yt=yt, bank=bank: nc.vector.tensor_copy(out=yt[:, 512:1024], in_=bank[:, :]), [bank], [yt], 512)
                    r0 = e * CAP + a * P
                    SP(lambda yt=yt, r0=r0: nc.sync.dma_start(out=ybuf.t[r0:r0 + P, :], in_=yt[:, :]), [yt], [ybuf])
                    yield

            load_expert(0)
            kb.merge([kb.record(head(0))])
            for e in range(1, NE):
                kb.merge([kb.record(tail(e - 1)), kb.record(head(e))])
            kb.merge([kb.record(tail(NE - 1))])
            kb.barrier()
            if stop <= 4:
                return nc

        with ExitStack() as cstk:
            Wg = kb.sb(cstk, "Wg", [P, 8, D], BF16)
            Wu = kb.sb(cstk, "Wu", [P, 2, D], BF16)
            gkt2 = kb.sb(cstk, "gkt2", [P, 24], F32)
            bple = kb.sb(cstk, "bple", [P, D], F32)
            bfin = kb.sb(cstk, "bfin", [P, D], F32)
            SP(lambda: nc.sync.dma_start(out=gkt2[:, :], in_=gk[:, :]), [gk], [gkt2])
            SP(lambda: nc.sync.dma_start(out=bple[:, :], in_=bc_ple[:, :]), [bc_ple], [bple])
            SP(lambda: nc.sync.dma_start(out=bfin[:, :], in_=bc_fin[:, :]), [bc_fin], [bfin])
            with ExitStack() as lst:
                load_weight(lst, Wg, lambda c0, cw: w_gate.t[:, c0:c0 + cw].rearrange("(k p) n -> p k n", p=P), 8, D,
                            (lambda k: gkt2[:, 16 + k:17 + k], gkt2), "wg")
                load_weight(lst, Wu, lambda c0, cw: w_up.t[:, c0:c0 + cw].rearrange("(k p) n -> p k n", p=P), 2, D, None, "wu")
                kb.barrier()
            h1c = [kb.sb(cstk, "h1c%d" % i, [P, D], F32) for i in range(4)]
            Y1 = [kb.sb(cstk, "Y1_%d" % i, [P, D], BF16) for i in range(4)]
            Y2 = [kb.sb(cstk, "Y2_%d" % i, [P, D], BF16) for i in range(4)]
            pt = [kb.sb(cstk, "pt%d" % i, [P, PLE], F32) for i in range(4)]
            ptb = [kb.sb(cstk, "ptb%d" % i, [P, PLE], BF16) for i in range(2)]
            pT = [kb.sb(cstk, "pT%d" % i, [P, 2, P], BF16) for i in range(2)]
            h2 = [kb.sb(cstk, "h2_%d" % i, [P, D], F32) for i in range(2)]
            hb = [kb.sb(cstk, "hb%d" % i, [P, D], BF16) for i in range(2)]
            hbT = [kb.sb(cstk, "hbT%d" % i, [P, 8, P], BF16) for i in range(2)]
            gt = [kb.sb(cstk, "gt%d" % i, [P, D], F32) for i in range(2)]
            et = [kb.sb(cstk, "et%d" % i, [P, D], F32) for i in range(2)]
            junk2 = kb.sb(cstk, "junk2", [P, D], BF16)
            ot = [kb.sb(cstk, "ot%d" % i, [P, D], F32) for i in range(2)]
            sc = [kb.sb(cstk, "sc%d" % i, [P, 16], F32) for i in range(2)]
            for i in range(4):
                G(lambda i=i: nc.gpsimd.memset(Y1[i][:, :], 0.0), [], [Y1[i]])
                G(lambda i=i: nc.gpsimd.memset(Y2[i][:, :], 0.0), [], [Y2[i]])

            def load_C(n):
                j = n % 4
                SP(lambda: nc.sync.dma_start(out=h1c[j][:, :], in_=h1s.t[n * P:(n + 1) * P, :]), [h1s], [h1c[j]])
                SP(lambda: nc.sync.dma_start(out=pt[j][:, :], in_=p_main.t[n * P:(n + 1) * P, :]), [p_main], [pt[j]])
                for (k_, Yk) in ((0, Y1[j]), (1, Y2[j])):
                    GD(lambda k_=k_, Yk=Yk: nc.gpsimd.indirect_dma_start(out=Yk[:, :], out_offset=None, in_=ybuf.t[:, :],
                                                                          in_offset=bass.IndirectOffsetOnAxis(ap=dest_i[:, n, k_:k_ + 1], axis=0),
                                                                          bounds_check=bc_reg, oob_is_err=False), [ybuf, dest_i], [Yk])

            def tile_C(n):
                j = n % 2
                j4 = n % 4
                gb, pb0, pb1 = (b0, b1, b2) if j == 0 else (b3, b5, b6)
                h2j, hbj, hbTj, gtj, etj, scj, ptbj, pTj, o_ = h2[j], hb[j], hbT[j], gt[j], et[j], sc[j], ptb[j], pT[j], ot[j]
                V(lambda: nc.vector.scalar_tensor_tensor(out=h2j[:, :], in0=Y1[j4][:, :], scalar=gates[:, n, 0:1], in1=h1c[j4][:, :], op0=ALU.mult, op1=ALU.add),
                  [Y1[j4], gates, h1c[j4]], [h2j])
                V(lambda: nc.vector.scalar_tensor_tensor(out=h2j[:, :], in0=Y2[j4][:, :], scalar=gates[:, n, 1:2], in1=h2j[:, :], op0=ALU.mult, op1=ALU.add),
                  [Y2[j4], gates, h2j], [h2j])
                A(lambda: nc.scalar.activation(out=junk2[:, :], in_=h2j[:, :], func=AF.Square, accum_out=scj[:, 0:1]), [h2j], [junk2, scj])
                A(lambda: nc.scalar.activation(out=scj[:, 1:2], in_=scj[:, 0:1], func=AF.Ln, bias=EPS, scale=1.0 / D), [scj], [scj], 1)
                A(lambda: nc.scalar.activation(out=scj[:, 2:3], in_=scj[:, 1:2], func=AF.Exp, scale=-0.5), [scj], [scj], 1)
                V(lambda: nc.vector.tensor_scalar(out=hbj[:, :], in0=h2j[:, :], scalar1=scj[:, 2:3], scalar2=None, op0=ALU.mult), [h2j, scj], [hbj])
                yield
                G(lambda: nc.gpsimd.tensor_copy(out=ptbj[:, :], in_=pt[j4][:, :]), [pt[j4]], [ptbj], 256)
                kb.lock(bkT)
                for k in range(2):
                    T(lambda k=k: nc.tensor.transpose(out=bkT[:, k * P:(k + 1) * P], in_=ptbj[:, k * P:(k + 1) * P], identity=identb[:, :]), [ptbj, identb], [bkT])
                A(lambda: nc.scalar.copy(out=pTj[:, :, :], in_=bkT[:, 0:2 * P].rearrange("p (k t) -> p k t", k=2)), [bkT], [pTj], 256)
                kb.unlock(bkT)
                for half in range(2):
                    bank = pb0 if half == 0 else pb1
                    for k in range(2):
                        T(lambda k=k, bank=bank, half=half: nc.tensor.matmul(bank[:, :], lhsT=pTj[:, k, :], rhs=Wu[:, k, half * 512:(half + 1) * 512], start=(k == 0), stop=(k == 1)),
                          [pTj, Wu], [bank], 512)
                    A(lambda bank=bank, half=half: nc.scalar.activation(out=junk2[:, half * 512:(half + 1) * 512], in_=bank[:, :], func=AF.Square, accum_out=scj[:, 4 + half:5 + half]),
                      [bank], [junk2, scj], 512)
                V(lambda: nc.vector.tensor_tensor(out=scj[:, 6:7], in0=scj[:, 4:5], in1=scj[:, 5:6], op=ALU.add), [scj], [scj], 1)
                A(lambda: nc.scalar.activation(out=scj[:, 7:8], in_=scj[:, 6:7], func=AF.Ln, bias=EPS, scale=1.0 / D), [scj], [scj], 1)
                A(lambda: nc.scalar.activation(out=scj[:, 8:9], in_=scj[:, 7:8], func=AF.Exp, scale=-0.5), [scj], [scj], 1)
                for half in range(2):
                    bank = pb0 if half == 0 else pb1
                    sl = slice(half * 512, (half + 1) * 512)
                    V(lambda bank=bank, sl=sl: nc.vector.scalar_tensor_tensor(out=etj[:, sl], in0=bank[:, :], scalar=scj[:, 8:9], in1=bple[:, sl], op0=ALU.mult, op1=ALU.mult),
                      [bank, scj, bple], [etj], 512)
                yield
                kb.mark()
                kb.lock(bkT)
                for k in range(8):
                    T(lambda k=k: nc.tensor.transpose(out=bkT[:, k * P:(k + 1) * P], in_=hbj[:, k * P:(k + 1) * P], identity=identb[:, :]), [hbj, identb], [bkT])
                V(lambda: nc.vector.tensor_copy(out=hbTj[:, :, :], in_=bkT[:, :].rearrange("p (k t) -> p k t", k=8)), [bkT], [hbTj])
                kb.unlock(bkT)
                gbs = (gb, pb0)
                for half in range(2):
                    for k in range(8):
                        T(lambda k=k, half=half: nc.tensor.matmul(gbs[half][:, :], lhsT=hbTj[:, k, :], rhs=Wg[:, k, half * 512:(half + 1) * 512], start=(k == 0), stop=(k == 7)),
                          [hbTj, Wg], [gbs[half]], 512)
                kb.lock_engine("act")
                for half in range(2):
                    A(lambda half=half: nc.scalar.activation(out=gtj[:, half * 512:(half + 1) * 512], in_=gbs[half][:, :], func=AF.Sigmoid), [gbs[half]], [gtj], 512)
                kb.unlock_engine("act")
                yield
                G(lambda: nc.gpsimd.tensor_tensor(out=etj[:, :], in0=etj[:, :], in1=gtj[:, :], op=ALU.mult), [etj, gtj], [etj])
                V(lambda: nc.vector.tensor_tensor(out=h2j[:, :], in0=h2j[:, :], in1=etj[:, :], op=ALU.add), [h2j, etj], [h2j])
                A(lambda: nc.scalar.activation(out=junk2[:, :], in_=h2j[:, :], func=AF.Square, accum_out=scj[:, 10:11]), [h2j], [junk2, scj])
                A(lambda: nc.scalar.activation(out=scj[:, 11:12], in_=scj[:, 10:11], func=AF.Ln, bias=EPS, scale=1.0 / D), [scj], [scj], 1)
                A(lambda: nc.scalar.activation(out=scj[:, 12:13], in_=scj[:, 11:12], func=AF.Exp, scale=-0.5), [scj], [scj], 1)
                V(lambda: nc.vector.scalar_tensor_tensor(out=o_[:, :], in0=h2j[:, :], scalar=scj[:, 12:13], in1=bfin[:, :], op0=ALU.mult, op1=ALU.mult),
                  [h2j, scj, bfin], [o_])
                SP(lambda: nc.sync.dma_start(out=out_d.t[n * P:(n + 1) * P, :], in_=o_[:, :]), [o_], [out_d])

            load_C(0)
            if NT > 1:
                load_C(1)
            prev2 = []
            for n in range(NT):
                if n + 2 < NT:
                    load_C(n + 2)
                st_ = kb.record(tile_C(n))
                mk = st_.index(("mark",))
                kb.merge([prev2, st_[:mk]])
                prev2 = st_[mk + 1:]
            kb.merge([prev2])
            kb.barrier()
        build_program.stats = (kb.n_inst, kb.n_wait)
    return nc


def make_consts(carry):
    c = np.zeros((P, CST_W), np.float32)
    idx = np.arange(P)
    c[:, C_ID:C_ID + P] = np.eye(P, dtype=np.float32)
    c[:, C_MASK:C_MASK + P] = (idx[None, :] >= idx[:, None]).astype(np.float32)
    c[:, C_USTR:C_USTR + P] = (idx[:, None] < idx[None, :]).astype(np.float32)
    c[:, C_ONES:C_ONES + P] = 1.0
    h = np.arange(8, dtype=np.float64)
    lg = np.log1p(-(2.0 ** (-5.0 - h)))
    t = idx.astype(np.float64)
    gq = np.exp(lg[None, :] * t[:, None])
    gkk = np.exp(-lg[None, :] * t[:, None]) * (64.0 ** -0.5)
    c[:, C_GQ:C_GQ + 8] = gq
    c[:, C_GK:C_GK + 8] = gkk
    gC = np.exp(lg * 128.0)
    c[:, C_GC:C_GC + 8] = gC[None, :]
    freqs = (10000.0 ** (-np.arange(32, dtype=np.float32) / 32)).astype(np.float32)
    c[:, C_FREQ:C_FREQ + 32] = freqs[None, :]
    c[:, C_IOTA32:C_IOTA32 + 32] = np.arange(32, dtype=np.float32)[None, :]
    c[:, C_IOTA4:C_IOTA4 + 4] = np.arange(4, dtype=np.float32)[None, :]
    c[:, C_CARRY] = carry
    return c


def arr_pk(v):
    return np.ascontiguousarray(v.reshape(-1, P).T)


def rep(v):
    return np.ascontiguousarray(np.broadcast_to(v.reshape(1, -1), (P, v.size))).astype(np.float32)


def shared_maps(inp):
    f = lambda a: np.ascontiguousarray(np.asarray(a, dtype=np.float32))
    m = {}
    m["w_in"] = f(inp["w_in"][0])
    m["w_out"] = f(inp["w_out"][0])
    m["w1"] = f(inp["w1"][0])
    m["w3"] = f(inp["w3"][0])
    m["w2"] = f(inp["w2"][0])
    m["w_up"] = f(inp["w_ple_up"][0])
    m["w_gate"] = f(inp["w_ple_gate"][0])
    m["wr"] = np.ascontiguousarray(np.concatenate([f(inp["w_group"][0]), f(inp["w_router"][0])], axis=1))
    gout = np.concatenate([f(inp["ret_gn"][0]), f(inp["ml_gn"][0])])
    m["gk"] = np.ascontiguousarray(np.concatenate([arr_pk(f(inp["attn_norm"][0])), arr_pk(gout), arr_pk(f(inp["ple_gate_norm"][0]))], axis=1))
    m["bc_moe"] = rep(f(inp["moe_norm"][0]))
    m["bc_ple"] = rep(f(inp["ple_norm"][0]))
    m["bc_fin"] = rep(f(inp["final_norm"]))
    m["bc_small"] = rep(np.concatenate([f(inp["b_group"][0]), f(inp["b_router"][0]), f(inp["b_igate"][0]), f(inp["b_fgate"][0])]))
    cw = f(inp["conv_w"][0])
    cb = f(inp["conv_b"][0])
    cwa = np.zeros((P, 8, 4), np.float32)
    for c in range(8):
        cwa[:, c, :] = cw[:, c * P:(c + 1) * P].T
    m["convw"] = np.ascontiguousarray(np.concatenate([cwa.reshape(P, 32), arr_pk(cb)], axis=1))
    m["convb_row"] = np.ascontiguousarray(cb.reshape(1, 1024))
    return m


_CACHE = {}


def kernel(**inputs):
    x = np.asarray(inputs["x"], dtype=np.float32)
    p = np.asarray(inputs["p"], dtype=np.float32)[0]
    pos = np.asarray(inputs["positions"]).astype(np.int32)
    B, S, _ = x.shape
    half = S // 2
    NT = half // P
    PT = NT
    key = (NT, PT)
    if key not in _CACHE:
        _CACHE[key] = build_program(NT, PT)
    nc = _CACHE[key]
    sh = shared_maps(inputs)
    zeros_src = np.zeros((1024, 512), np.float32)
    in_maps = []
    for core in range(8):
        b, s = core // 2, core % 2
        m = dict(sh)
        lo = s * half
        m["x_main"] = np.ascontiguousarray(x[b, lo:lo + half])
        m["pos_main"] = np.ascontiguousarray(pos[b, lo:lo + half].reshape(NT, P).T)
        m["p_main"] = np.ascontiguousarray(p[b, lo:lo + half])
        if s == 0:
            m["x_pre"] = np.zeros((half, D), np.float32)
            m["pos_pre"] = np.zeros((P, PT), np.int32)
        else:
            m["x_pre"] = np.ascontiguousarray(x[b, 0:half])
            m["pos_pre"] = np.ascontiguousarray(pos[b, 0:half].reshape(PT, P).T)
        m["cst"] = make_consts(float(s))
        m["zsrc"] = zeros_src
        in_maps.append(m)
    res = run_bass_kernel_spmd(nc, in_maps, core_ids=list(range(8)))
    out = np.empty((B, S, D), np.float32)
    for core in range(8):
        b, s = core // 2, core % 2
        out[b, s * half:(s + 1) * half] = res.results[core]["out"]
    return out
```
